# Optimizing a Trainium2 kernel written in Bass

```python
import functools
import jax
import jax.numpy as jnp
from jax import lax

D_MODEL = 1024
BATCH = 16
SEQ = 2048
DEPTH = 2

GRID_W = 64
CTX_LEN = 256
N_GROUPS = 4
GROUP_W = D_MODEL // 4
MIX_W = 4 * GROUP_W
SUB_W = GROUP_W // N_GROUPS
CHUNK = 128
POOL_WINDOWS = (2, 4, 8, 16)
MLA_HEADS = 4
QK_NOPE = 64
QK_ROPE = 32
V_HEAD = 64
QK_HEAD = QK_NOPE + QK_ROPE
Q_LORA = 3 * D_MODEL // 16
KV_LORA = D_MODEL // 8
ROPE_BASE = 10000.0
Q_BLOCK = 128
D_FF = 7 * D_MODEL // 2
N_EXPERTS = 8
TOP_K = 2
N_ADA = 6
EPS = 1e-6
N_DENSE = (DEPTH + 1) // 2
N_MOE = DEPTH // 2

OFF_A = 0
OFF_B = OFF_A + 2 * GROUP_W
OFF_CQ = OFF_B + GROUP_W
OFF_CKV = OFF_CQ + Q_LORA
OFF_CKR = OFF_CKV + KV_LORA
OFF_D = OFF_CKR + QK_ROPE
IN_W = OFF_D + GROUP_W

kernel_name = 'hybrid_parallel_groups_dit_block'


def rmsnorm(x, g):
    xf = x.astype(jnp.float32)
    y = xf * lax.rsqrt(jnp.mean(xf * xf, axis=-1, keepdims=True) + EPS)
    return (y * g.astype(jnp.float32)).astype(x.dtype)


def modulate(x, g, shift, scale):
    return rmsnorm(x, g) * (1 + scale) + shift


def axial_rope_tables(length):
    rows = length // GRID_W
    row = jnp.repeat(jnp.arange(rows, dtype=jnp.float32), GRID_W)
    col = jnp.tile(jnp.arange(GRID_W, dtype=jnp.float32), rows)
    n_freq = QK_ROPE // 4
    inv_freq = ROPE_BASE ** (-jnp.arange(n_freq, dtype=jnp.float32) / n_freq)
    ang = jnp.stack([row[:, None] * inv_freq, col[:, None] * inv_freq], axis=1)
    return jnp.cos(ang), jnp.sin(ang)


def apply_axial_rope(x, cos, sin):
    b, l, h, _ = x.shape
    xr = x.astype(jnp.float32).reshape(b, l, h, 2, 2, QK_ROPE // 4)
    x1, x2 = xr[..., 0, :], xr[..., 1, :]
    cs, sn = cos[:, None], sin[:, None]
    out = jnp.stack([x1 * cs - x2 * sn, x2 * cs + x1 * sn], axis=-2)
    return out.reshape(b, l, h, QK_ROPE).astype(x.dtype)


def chunk_spatial_gate(z, norm_g, w_s, b_s):
    b, l, _ = z.shape
    uv = jax.nn.gelu(z)
    u = uv[..., :GROUP_W]
    v = rmsnorm(uv[..., GROUP_W:], norm_g).reshape(b, l // CHUNK, CHUNK, N_GROUPS, SUB_W)
    mixed = jnp.einsum('gts,bnsgc->bntgc', w_s, v) + b_s.T[None, None, :, :, None]
    return u * mixed.reshape(b, l, GROUP_W)


def multiscale_pool(z, w_p, scale):
    b, l, _ = z.shape
    zf = z.astype(jnp.float32).reshape(b, l, N_GROUPS, SUB_W)
    cs = jnp.concatenate([jnp.zeros((b, 1, N_GROUPS, SUB_W), jnp.float32), jnp.cumsum(zf, axis=1)], axis=1)
    t = jnp.arange(l)
    pooled = []
    for gi, w in enumerate(POOL_WINDOWS):
        lo = jnp.clip(t - w // 2, 0, l)
        hi = jnp.clip(t - w // 2 + w, 0, l)
        csg = cs[:, :, gi]
        s = jnp.take(csg, hi, axis=1) - jnp.take(csg, lo, axis=1)
        pooled.append(s / (hi - lo).astype(jnp.float32)[:, None])
    diff = jnp.stack(pooled, axis=2) - zf
    y = jnp.einsum('blgc,gcd->blgd', diff, w_p.astype(jnp.float32)).reshape(b, l, GROUP_W)
    return (y * scale).astype(z.dtype)


def fourier_mix(z, w_f):
    b, l, _ = z.shape
    zf = z.astype(jnp.float32).reshape(b, l, N_GROUPS, SUB_W)
    y = jnp.fft.fft2(zf, axes=(1, 3), norm='ortho').real.reshape(b, l, GROUP_W)
    return y.astype(z.dtype) @ w_f


def mla_q(p, q_norm_g, w_uq, qk_q_g, rope):
    b, l, _ = p.shape
    q = (rmsnorm(p[..., OFF_CQ:OFF_CKV], q_norm_g) @ w_uq).reshape(b, l, MLA_HEADS, QK_HEAD)
    q = rmsnorm(q, qk_q_g)
    if rope is not None:
        q = jnp.concatenate([q[..., :QK_NOPE], apply_axial_rope(q[..., QK_NOPE:], *rope)], axis=-1)
    return q.transpose(0, 2, 1, 3)


def mla_kv(p, kv_norm_g, w_ukv, qk_k_g, rope):
    b, l, _ = p.shape
    kv = (rmsnorm(p[..., OFF_CKV:OFF_CKR], kv_norm_g) @ w_ukv).reshape(b, l, MLA_HEADS, QK_NOPE + V_HEAD)
    k_rope = jnp.broadcast_to(p[:, :, None, OFF_CKR:OFF_D], (b, l, MLA_HEADS, QK_ROPE))
    k = rmsnorm(jnp.concatenate([kv[..., :QK_NOPE], k_rope], axis=-1), qk_k_g)
    if rope is not None:
        k = jnp.concatenate([k[..., :QK_NOPE], apply_axial_rope(k[..., QK_NOPE:], *rope)], axis=-1)
    return k.transpose(0, 2, 1, 3), kv[..., QK_NOPE:].transpose(0, 2, 1, 3)


def attend(q, k, v):
    s = jnp.einsum('bhqd,bhkd->bhqk', q, k).astype(jnp.float32) * (QK_HEAD ** -0.5)
    p = jax.nn.softmax(s, axis=-1).astype(v.dtype)
    return jnp.einsum('bhqk,bhkd->bhqd', p, v)


def heads_to_channels(o):
    b, h, l, d = o.shape
    return o.transpose(0, 2, 1, 3).reshape(b, l, h * d)


def blocked_attention(q, k, v):
    b, h, l, d = q.shape
    qb = q.reshape(b, h, l // Q_BLOCK, Q_BLOCK, d).transpose(2, 0, 1, 3, 4)
    out = lax.map(lambda qi: attend(qi, k, v), qb)
    return out.transpose(1, 2, 0, 3, 4).reshape(b, h, l, V_HEAD)


def merge_groups(p, attn, gmlp_norm_g, spatial_w, spatial_b, pool_w, pool_scale, fourier_w, w_out):
    ya = chunk_spatial_gate(p[..., OFF_A:OFF_B], gmlp_norm_g, spatial_w, spatial_b)
    yb = multiscale_pool(p[..., OFF_B:OFF_CQ], pool_w, pool_scale)
    yd = fourier_mix(p[..., OFF_D:IN_W], fourier_w)
    return jnp.concatenate([ya, yb, attn, yd], axis=-1) @ w_out


def swiglu(h, w_gate, w_up, w_down):
    return (jax.nn.silu(h @ w_gate) * (h @ w_up)) @ w_down


def moe_swiglu(h, router_w, w_gate, w_up, w_down):
    b, l, d = h.shape
    t = h.reshape(b * l, d)
    logits = (t @ router_w).astype(jnp.float32)
    top_val, top_idx = lax.top_k(logits, TOP_K)
    top_w = jax.nn.softmax(top_val, axis=-1)
    gate = jnp.sum(jax.nn.one_hot(top_idx, N_EXPERTS, dtype=jnp.float32) * top_w[..., None], axis=1)
    out = jnp.zeros((b * l, d), jnp.float32)
    for e in range(N_EXPERTS):
        out = out + gate[:, e:e + 1] * swiglu(t, w_gate[e], w_up[e], w_down[e]).astype(jnp.float32)
    return out.astype(h.dtype).reshape(b, l, d)


def setup_inputs(seed: int = 0) -> dict:
    key = jax.random.key(seed)
    ks = iter(jax.random.split(key, 40))

    def nrm(shape, scale):
        return jax.random.normal(next(ks), shape, jnp.float32) * scale

    def gain(shape, noise=0.02):
        return 1.0 + nrm(shape, noise)

    D = D_MODEL
    return {
        'x': nrm((BATCH, SEQ, D), 1.0),
        'c': nrm((BATCH, D), 1.0),
        'ctx': nrm((BATCH, CTX_LEN, D), 1.0),
        'c_ctx': nrm((D,), 1.0),
        'ada_w': nrm((DEPTH, D, N_ADA * D), 0.5 * D ** -0.5),
        'ada_b': nrm((DEPTH, N_ADA * D), 0.02),
        'norm1_g': gain((DEPTH, D)),
        'w_in': nrm((DEPTH, D, IN_W), D ** -0.5),
        'gmlp_norm_g': gain((DEPTH, GROUP_W)),
        'spatial_w': nrm((DEPTH, N_GROUPS, CHUNK, CHUNK), CHUNK ** -0.5),
        'spatial_b': gain((DEPTH, N_GROUPS, CHUNK)),
        'pool_w': nrm((DEPTH, N_GROUPS, SUB_W, SUB_W), SUB_W ** -0.5),
        'pool_scale': gain((DEPTH, GROUP_W), 0.1),
        'q_norm_g': gain((DEPTH, Q_LORA)),
        'w_uq': nrm((DEPTH, Q_LORA, MLA_HEADS * QK_HEAD), Q_LORA ** -0.5),
        'kv_norm_g': gain((DEPTH, KV_LORA)),
        'w_ukv': nrm((DEPTH, KV_LORA, MLA_HEADS * (QK_NOPE + V_HEAD)), KV_LORA ** -0.5),
        'qk_q_g': gain((DEPTH, QK_HEAD)),
        'qk_k_g': gain((DEPTH, QK_HEAD)),
        'fourier_w': nrm((DEPTH, GROUP_W, GROUP_W), GROUP_W ** -0.5),
        'w_out': nrm((DEPTH, MIX_W, D), MIX_W ** -0.5),
        'norm2_g': gain((DEPTH, D)),
        'ffn_w_gate': nrm((N_DENSE, D, D_FF), D ** -0.5),
        'ffn_w_up': nrm((N_DENSE, D, D_FF), D ** -0.5),
        'ffn_w_down': nrm((N_DENSE, D_FF, D), D_FF ** -0.5),
        'router_w': nrm((N_MOE, D, N_EXPERTS), D ** -0.5),
        'moe_w_gate': nrm((N_MOE, N_EXPERTS, D, D_FF), D ** -0.5),
        'moe_w_up': nrm((N_MOE, N_EXPERTS, D, D_FF), D ** -0.5),
        'moe_w_down': nrm((N_MOE, N_EXPERTS, D_FF, D), D_FF ** -0.5),
    }


def reference(x, c, ctx, c_ctx, ada_w, ada_b, norm1_g, w_in, gmlp_norm_g, spatial_w, spatial_b,
              pool_w, pool_scale, q_norm_g, w_uq, kv_norm_g, w_ukv, qk_q_g, qk_k_g, fourier_w, w_out,
              norm2_g, ffn_w_gate, ffn_w_up, ffn_w_down, router_w, moe_w_gate, moe_w_up, moe_w_down):
    rope = axial_rope_tables(x.shape[1])
    c_act = jax.nn.silu(c)
    cc_act = jax.nn.silu(c_ctx)
    xc = ctx
    for i in range(DEPTH):
        last = i == DEPTH - 1
        sh1, sc1, g1, sh2, sc2, g2 = jnp.split((c_act @ ada_w[i] + ada_b[i])[:, None, :], N_ADA, axis=-1)
        csh1, csc1, cg1, csh2, csc2, cg2 = jnp.split(cc_act @ ada_w[i] + ada_b[i], N_ADA, axis=-1)
        group_w = (gmlp_norm_g[i], spatial_w[i], spatial_b[i], pool_w[i], pool_scale[i], fourier_w[i], w_out[i])
        if i % 2 == 0:
            j = i // 2
            ffn = functools.partial(swiglu, w_gate=ffn_w_gate[j], w_up=ffn_w_up[j], w_down=ffn_w_down[j])
        else:
            j = i // 2
            ffn = functools.partial(moe_swiglu, router_w=router_w[j], w_gate=moe_w_gate[j],
                                    w_up=moe_w_up[j], w_down=moe_w_down[j])

        p = modulate(x, norm1_g[i], sh1, sc1) @ w_in[i]
        pc = modulate(xc, norm1_g[i], csh1, csc1) @ w_in[i]
        kc, vc = mla_kv(pc, kv_norm_g[i], w_ukv[i], qk_k_g[i], None)
        k, v = mla_kv(p, kv_norm_g[i], w_ukv[i], qk_k_g[i], rope)
        q = mla_q(p, q_norm_g[i], w_uq[i], qk_q_g[i], rope)
        attn = heads_to_channels(blocked_attention(q, jnp.concatenate([kc, k], axis=2),
                                                   jnp.concatenate([vc, v], axis=2)))
        x_new = x + g1 * merge_groups(p, attn, *group_w)
        x_new = x_new + g2 * ffn(modulate(x_new, norm2_g[i], sh2, sc2))

        if not last:
            qc = mla_q(pc, q_norm_g[i], w_uq[i], qk_q_g[i], None)
            attn_c = heads_to_channels(attend(qc, kc, vc))
            xc = xc + cg1 * merge_groups(pc, attn_c, *group_w)
            xc = xc + cg2 * ffn(modulate(xc, norm2_g[i], csh2, csc2))
        x = x_new
    return x
```

```python
import numpy as np
import ml_dtypes
from contextlib import ExitStack
import concourse.bass as bass
import concourse.mybir as mybir
from concourse.bass_utils import run_bass_kernel_spmd

F32 = mybir.dt.float32
BF16 = mybir.dt.bfloat16
U8 = mybir.dt.uint8
AF = mybir.ActivationFunctionType
ALU = mybir.AluOpType
AX = mybir.AxisListType

D = 1024
T = 2048
TC = 256
NK = T + TC
DFF = 3584
NE = 8
EPS = 1e-6
OFF_A, OFF_B, OFF_CQ, OFF_CKV, OFF_CKR, OFF_D, IN_W = 0, 512, 768, 960, 1088, 1120, 1376
PADW = 8
SEM_LIMIT = 8000


class Buf:
    __slots__ = ("w", "r", "name", "dsem", "dcount")

    def __init__(self, name=""):
        self.w = None
        self.r = {}
        self.name = name
        self.dsem = None
        self.dcount = 0


class Eng:
    def __init__(self, name, h):
        self.name = name
        self.h = h
        self.sem = None
        self.count = 0
        self.waited = {}
        self.own = set()


class Ctx:
    def __init__(self, nc, stack):
        self.nc = nc
        self.stack = stack
        self.nsem = 0
        self.engs = {n: Eng(n, getattr(nc, n)) for n in ("tensor", "vector", "scalar", "gpsimd", "sync")}
        self.dma_tokens = {}
        self.ninst = 0

    def new_sem(self, name):
        self.nsem += 1
        return self.stack.enter_context(self.nc.semaphore(f"s{self.nsem}_{name}"))

    def _wait(self, e, deps):
        need = {}
        for d in deps:
            if d is None:
                continue
            s, v = d
            if e.name == "tensor" and s in e.own:
                continue
            if need.get(s, 0) < v:
                need[s] = v
        for s, v in need.items():
            if e.waited.get(s, 0) < v:
                e.h.wait_ge(s, v)
                e.waited[s] = v

    def _deps(self, reads, writes):
        deps = []
        for b in list(reads) + list(writes):
            if isinstance(b.w, list):
                deps.extend(b.w)
            else:
                deps.append(b.w)
        for b in writes:
            deps.extend(b.r.items())
        return deps

    def _commit(self, tok, reads, writes):
        for b in writes:
            b.w = tok
            b.r = {}
        s, v = tok
        for b in reads:
            if b in writes:
                continue
            if b.r.get(s, 0) < v:
                b.r[s] = v

    def _tok(self, e, ins):
        if e.sem is None or e.count >= SEM_LIMIT:
            e.sem = self.new_sem(e.name)
            e.own.add(e.sem)
            e.count = 0
        e.count += 1
        ins.then_inc(e.sem, 1)
        return (e.sem, e.count)

    def op(self, eng, emit, reads=(), writes=()):
        e = self.engs[eng]
        self._wait(e, self._deps(reads, writes))
        ins = emit(e.h)
        tok = self._tok(e, ins)
        self._commit(tok, reads, writes)
        self.ninst += 1
        return tok

    def dma(self, eng, out, in_, reads=(), writes=()):
        e = self.engs[eng]
        self._wait(e, self._deps(reads, writes))
        owner = writes[0] if writes else reads[0]
        kind = "sw" if eng == "gpsimd" else "hw"
        if owner.dsem is None:
            owner.dsem = {}
        if kind not in owner.dsem:
            owner.dsem[kind] = [self.new_sem("d" + kind + owner.name), 0]
        ent = owner.dsem[kind]
        ent[1] += 16
        e.h.dma_start(out=out, in_=in_).then_inc(ent[0], 16)
        tok = (ent[0], ent[1])
        self._commit(tok, reads, writes)
        if writes:
            others = [(v[0], v[1]) for k, v in owner.dsem.items() if k != kind and v[1] > 0]
            if others:
                owner.w = [tok] + others
        self.dma_tokens[ent[0]] = ent[1]
        return tok

    def barrier(self):
        sp = self.engs["sync"]
        deps = list(self.dma_tokens.items())
        for e in self.engs.values():
            if e.sem is not None:
                deps.append((e.sem, e.count))
        self._wait(sp, deps)
        ins = sp.h.nop()
        tok = self._tok(sp, ins)
        for e in self.engs.values():
            if e is not sp:
                self._wait(e, [tok])

    def finish(self):
        self.barrier()


def _bf(a):
    return np.ascontiguousarray(a.astype(ml_dtypes.bfloat16))


def make_consts():
    c = {}
    c["ident"] = np.eye(128, dtype=np.float32)
    t = np.arange(T)
    row = (t // 64).astype(np.float32)
    col = (t % 64).astype(np.float32)
    inv = (np.float32(10000.0) ** (-np.arange(8, dtype=np.float32) / np.float32(8))).astype(np.float32)
    a0 = row[:, None] * inv[None, :]
    a1 = col[:, None] * inv[None, :]
    cos32 = np.concatenate([np.cos(a0), np.cos(a0), np.cos(a1), np.cos(a1)], axis=1).astype(np.float32)
    sin32 = np.concatenate([-np.sin(a0), np.sin(a0), -np.sin(a1), np.sin(a1)], axis=1).astype(np.float32)
    c["rope_cos"] = np.ascontiguousarray(cos32.reshape(16, 128, 32).transpose(1, 0, 2))
    c["rope_sin"] = np.ascontiguousarray(sin32.reshape(16, 128, 32).transpose(1, 0, 2))
    def dft(L):
        lk = (np.arange(L)[:, None] * np.arange(L)[None, :]) % L
        ang = 2.0 * np.pi * lk / L
        return np.cos(ang) / np.sqrt(L), -np.sin(ang) / np.sqrt(L)
    CL, SLn = dft(T)
    def lay(M):
        return _bf(M.reshape(16, 128, 4, 512).transpose(2, 1, 0, 3))
    c["dft_c"] = lay(CL)
    c["dft_s"] = lay(SLn)
    C2, S2n = dft(TC)
    c["dftc_c"] = _bf(C2.reshape(2, 128, TC).transpose(1, 0, 2))
    c["dftc_s"] = _bf(S2n.reshape(2, 128, TC).transpose(1, 0, 2))
    cm = (np.arange(64)[:, None] * np.arange(64)[None, :]) % 64
    C64 = np.cos(2 * np.pi * cm / 64) / 8.0
    S64 = np.sin(2 * np.pi * cm / 64) / 8.0
    bd = np.zeros((2, 128, 128))
    for h in range(2):
        bd[0, h * 64:(h + 1) * 64, h * 64:(h + 1) * 64] = C64
        bd[1, h * 64:(h + 1) * 64, h * 64:(h + 1) * 64] = S64
    c["bd64"] = _bf(bd.transpose(1, 0, 2))
    E = np.zeros((128, 2, 16), np.float32)
    for cch in range(2):
        for p in range(128):
            w = (2, 4, 8, 16)[2 * cch + p // 64]
            for i in range(8):
                E[p, cch, i] = 1.0 / min(i + w // 2, w)
                tt = 8 - i
                E[p, cch, 8 + i] = 1.0 / min(tt + w // 2, w)
    c["pool_e"] = E
    sel = np.zeros((8, 8, 128), np.float32)
    for e in range(8):
        sel[e, e, :] = 1.0
    c["sel8"] = sel
    return c


class _Stop(Exception):
    pass


def build_program(dbg=None, stop=None):
    nc = bass.Bass("TRN2", target_bir_lowering=False)
    dbg = dbg or {}

    def din(name, shape, dt=F32):
        return nc.dram_tensor(name, list(shape), dt, kind="ExternalInput").ap()

    xT_d = din("xT", [2, D, T])
    ctxT_d = din("ctxT", [2, D, TC])
    cT_d = din("cT", [128, 8, 3])
    ada_w_d = din("ada_w", [2, D, 6 * D])
    ada_b_d = din("ada_b", [2, 128, 48])
    n1g_d = din("norm1_g", [2, 128, 8])
    n2g_d = din("norm2_g", [2, 128, 8])
    w_in_d = din("w_in", [2, D, IN_W])
    gmlp_g_d = din("gmlp_g", [2, 128, 256])
    spw_d = din("spatial_wT", [2, 128, 4, 128])
    spb_d = din("spatial_bT", [2, 128, 4])
    bdwp_d = din("bdwp", [2, 128, 2, 128])
    pscale_d = din("pool_scale", [2, 128, 2])
    qng_d = din("q_norm_g", [2, 128, 192])
    kvng_d = din("kv_norm_g", [2, 128, 128])
    qkq_d = din("qk_q_g", [2, 128, 384])
    qkk_d = din("qk_k_g", [2, 128, 384])
    w_uq_d = din("w_uq", [2, 192, 384])
    w_ukv_d = din("w_ukv", [2, 128, 512])
    wf_d = din("fourier_w", [2, 256, 256])
    w_out_d = din("w_out", [2, D, D])
    fg_d = din("ffn_w_gate", [1, D, DFF])
    fu_d = din("ffn_w_up", [1, D, DFF])
    fd_d = din("ffn_w_down", [1, DFF, D])
    rw_d = din("router_w", [128, 8, 8])
    mg_d = din("moe_w_gate", [1, NE, D, DFF])
    mu_d = din("moe_w_up", [1, NE, D, DFF])
    md_d = din("moe_w_down", [1, NE, DFF, D])
    ident_d = din("ident", [128, 128])
    rcos_d = din("rope_cos", [128, 16, 32])
    rsin_d = din("rope_sin", [128, 16, 32])
    dftc_d = din("dft_c", [4, 128, 16, 512], BF16)
    dfts_d = din("dft_s", [4, 128, 16, 512], BF16)
    dftcc_d = din("dftc_c", [128, 2, TC], BF16)
    dftcs_d = din("dftc_s", [128, 2, TC], BF16)
    bd64_d = din("bd64", [128, 2, 128], BF16)
    poole_d = din("pool_e", [128, 2, 16])
    sel8_d = din("sel8", [8, 8, 128])
    outT_d = nc.dram_tensor("outT", [2, D, T], F32, kind="ExternalOutput").ap()
    dbg_d = {k: nc.dram_tensor("dbg_" + k, list(shp), F32, kind="ExternalOutput").ap() for k, shp in dbg.items()}

    with ExitStack() as st:
        cx = Ctx(nc, st)

        def sb(name, shape, dt):
            return st.enter_context(nc.sbuf_tensor("sb_" + name, list(shape), dt))

        XT = sb("XT", [128, 8, T], F32)
        XC = sb("XC", [128, 8, TC], F32)
        HT = sb("HT", [128, 8, T], BF16)
        HC = sb("HC", [128, 8, TC], BF16)
        ARENA_BYTES = 72 * 1024
        arena = sb("arena", [128, ARENA_BYTES], U8)
        ident = sb("ident", [128, 128], F32)
        identb = sb("identb", [128, 128], BF16)
        onesb = sb("onesb", [128, 128], BF16)
        onesf = sb("onesf", [128, 128], F32)
        epsb = sb("epsb", [128, 1], F32)
        cact = sb("cact", [128, 8, 3], F32)
        MOD = sb("MOD", [128, 2, 6, 8, 3], F32)
        GM1 = sb("GM1", [128, 2, 8, 3], F32)
        GM2 = sb("GM2", [128, 2, 8, 3], F32)
        adab = sb("adab", [128, 2, 48], F32)
        n1g = sb("n1g", [128, 2, 8], F32)
        n2g = sb("n2g", [128, 2, 8], F32)
        rcos = sb("rcos", [128, 16, 32], F32)
        rsin = sb("rsin", [128, 16, 32], F32)
        bd64 = sb("bd64", [128, 2, 128], BF16)
        poole = sb("poole", [128, 2, 16], F32)
        sel8 = sb("sel8", [8, 8, 128], F32)
        rwf = sb("rwf", [128, 8, 8], F32)
        gmlp_g = sb("gmlp_g", [128, 256], F32)
        spw = sb("spw", [128, 4, 128], BF16)
        spb = sb("spb", [128, 4], F32)
        bdwp = sb("bdwp", [128, 2, 128], BF16)
        pscale = sb("pscale", [128, 2], F32)
        qng = sb("qng", [128, 192], F32)
        kvng = sb("kvng", [128, 128], F32)
        qkq = sb("qkq", [128, 384], F32)
        qkk = sb("qkk", [128, 384], F32)
        wuq = sb("wuq", [128, 2, 384], BF16)
        wukv = sb("wukv", [128, 512], BF16)
        wfb = sb("wfb", [128, 2, 256], BF16)
        WCS = sb("WCS", [128, 2, 2, 256], BF16)
        small = sb("small", [128, 64], F32)
        small2 = [sb("small_a", [128, 32], F32), sb("small_b", [128, 32], F32)]

        PS = [st.enter_context(nc.psum_tensor(f"ps{i}", [128, 512], F32)) for i in range(8)]
        PB = [Buf(f"ps{i}") for i in range(8)]

        class AA:
            def __init__(self):
                self.off = 0

            def reset(self):
                cx.barrier()
                self.off = 0

            def get(self, shape, dt):
                esz = 4 if dt == F32 else 2
                n = int(np.prod(shape[1:]))
                nbytes = n * esz
                self.off = (self.off + 31) // 32 * 32
                assert self.off + nbytes <= ARENA_BYTES, (self.off, nbytes)
                ap = arena[:, self.off:self.off + nbytes]
                if dt != U8:
                    ap = ap.bitcast(dt)
                self.off += nbytes
                if len(shape) == 2:
                    return ap
                names = " ".join(f"d{i}" for i in range(len(shape) - 1))
                kw = {f"d{i}": int(shape[i + 1]) for i in range(len(shape) - 2)}
                return ap.rearrange(f"p ({names}) -> p {names}", **kw)

        aa = AA()

        def chk(name):
            if stop == name:
                raise _Stop()
        B_const = Buf("const")
        B_mod = Buf("mod")
        B_lw = Buf("lw")
        B_small = Buf("small")

        def V(fn, reads=(), writes=()):
            return cx.op("vector", fn, reads, writes)

        def A(fn, reads=(), writes=()):
            return cx.op("scalar", fn, reads, writes)

        def G(fn, reads=(), writes=()):
            return cx.op("gpsimd", fn, reads, writes)

        def PE(fn, reads=(), writes=()):
            return cx.op("tensor", fn, reads, writes)

        def mm_group(out_ap, pairs, reads, writes):
            def emit(pe):
                n = len(pairs)
                last = None
                for i, (l, r) in enumerate(pairs):
                    last = pe.matmul(out_ap, l, r, start=(i == 0), stop=(i == n - 1))
                return last
            return PE(emit, reads, writes)

        def debug_dump(name, ap, buf):
            if name in dbg_d:
                cx.dma("gpsimd", dbg_d[name], ap, reads=[buf])

        V(lambda v: v.memset(onesb[:], 1.0), writes=[B_const])
        V(lambda v: v.memset(onesf[:], 1.0), writes=[B_const])
        V(lambda v: v.memset(epsb[:], EPS), writes=[B_const])
        B_ld = Buf("cload")
        for dst, src in ((ident[:], ident_d), (cact[:], cT_d), (rcos[:], rcos_d), (rsin[:], rsin_d),
                         (poole[:], poole_d), (sel8[:], sel8_d), (rwf[:], rw_d), (bd64[:], bd64_d),
                         (adab[:], ada_b_d.rearrange("l p j -> p l j")),
                         (n1g[:], n1g_d.rearrange("l p j -> p l j")), (n2g[:], n2g_d.rearrange("l p j -> p l j"))):
            cx.dma("sync", dst, src, writes=[B_ld])
        cx.dma("gpsimd", identb[:], ident_d, writes=[B_ld])
        A(lambda a: a.activation(out=cact[:], in_=cact[:], func=AF.Silu), reads=[B_ld], writes=[B_ld])

        aa.reset()
        AW = [aa.get([128, 8, 1024], F32) for _ in range(2)]
        AWB = [Buf("aw0"), Buf("aw1")]
        k = 0
        for l in range(2):
            for which in range(6):
                s = k % 2
                k += 1
                cx.dma("sync", AW[s], ada_w_d[l, :, which * 1024:(which + 1) * 1024].rearrange("(j p) n -> p j n", p=128),
                       writes=[AWB[s]])
                pb = PB[s]
                ps = PS[s]

                def emit(pe, s=s, ps=ps):
                    last = None
                    for j in range(8):
                        for kk in range(8):
                            last = pe.matmul(ps[:, j * 3:(j + 1) * 3], AW[s][:, kk, j * 128:(j + 1) * 128], cact[:, kk, :],
                                             start=(kk == 0), stop=(kk == 7))
                    return last
                PE(emit, reads=[AWB[s], B_ld], writes=[pb])
                V(lambda v, l=l, which=which, ps=ps: v.tensor_tensor(
                    out=MOD[:, l, which], in0=ps[:, 0:24].rearrange("p (j n) -> p j n", n=3),
                    in1=adab[:, l, which * 8:(which + 1) * 8].unsqueeze(2).to_broadcast([128, 8, 3]), op=ALU.add),
                  reads=[pb, B_ld], writes=[B_mod])
        for l in range(2):
            for (GM, ng, which) in ((GM1, n1g, 1), (GM2, n2g, 4)):
                V(lambda v, GM=GM, l=l, which=which: v.tensor_scalar(out=GM[:, l], in0=MOD[:, l, which], scalar1=1.0, scalar2=None, op0=ALU.add),
                  reads=[B_mod], writes=[B_mod])
                V(lambda v, GM=GM, l=l, ng=ng: v.tensor_tensor(out=GM[:, l], in0=GM[:, l], in1=ng[:, l].unsqueeze(2).to_broadcast([128, 8, 3]), op=ALU.mult),
                  reads=[B_mod, B_ld], writes=[B_mod])

        class Seq:
            pass

        lat = Seq()
        lat.name = "lat"; lat.T = T; lat.X = XT; lat.H = HT; lat.blocks = [(i * 512, 512) for i in range(4)]
        lat.XB = [Buf(f"xb{i}") for i in range(4)]; lat.HB = [Buf(f"hb{i}") for i in range(4)]
        lat.ntile = 16; lat.tok0 = TC; lat.rope = True
        cs = Seq()
        cs.name = "ctx"; cs.T = TC; cs.X = XC; cs.H = HC; cs.blocks = [(0, TC)]
        cs.XB = [Buf("xcb")]; cs.HB = [Buf("hcb")]
        cs.ntile = 2; cs.tok0 = 0; cs.rope = False

        def blk_of(seq, t0):
            return t0 // 512

        def norm_phase(seqs, l, GM, shift_which, router=False, gateT=None, B_gateT=None):
            aa.reset()
            SQ2 = [aa.get([128, 8, 512], BF16) for _ in range(2)]
            RS2 = [aa.get([128, 512], F32) for _ in range(2)]
            NTMP = 8
            TMP = [aa.get([128, 512], F32) for _ in range(NTMP)]
            B_sq2, B_rs2 = [Buf("sq0"), Buf("sq1")], [Buf("rs0"), Buf("rs1")]
            nblk = 0
            B_tmp = [Buf(f"tmp{i}") for i in range(NTMP)]
            if router:
                H32 = aa.get([128, 8, 512], F32)
                B_h32 = Buf("h32")
                LG = aa.get([128, 16], F32)
                B_lg = Buf("lg")
            it = 0
            allblk = [(seq, bi, t0, nt) for seq in seqs for bi, (t0, nt) in enumerate(seq.blocks)]

            def squares(k):
                seq, bi, t0, nt = allblk[k]
                SQ, B_sq = SQ2[k % 2], B_sq2[k % 2]
                for j in range(8):
                    if j % 2 == 1 and not router:
                        G(lambda g, j=j: g.tensor_tensor(out=SQ[:, j, 0:nt], in0=seq.X[:, j, t0:t0 + nt], in1=seq.X[:, j, t0:t0 + nt], op=ALU.mult),
                          reads=[seq.XB[bi]], writes=[B_sq])
                    else:
                        A(lambda a, j=j: a.activation(out=SQ[:, j, 0:nt], in_=seq.X[:, j, t0:t0 + nt], func=AF.Square),
                          reads=[seq.XB[bi]], writes=[B_sq])
                mm_group(PS[k % 2][:, 0:nt], [(onesb[:], SQ[:, j, 0:nt]) for j in range(8)], reads=[B_sq, B_const], writes=[PB[k % 2]])

            squares(0)
            for k, (seq, bi, t0, nt) in enumerate(allblk):
                    n = seq.n
                    xb, hb = seq.XB[bi], seq.HB[bi]
                    RS, B_rs = RS2[k % 2], B_rs2[k % 2]
                    pbn = k % 2
                    if k + 1 < len(allblk):
                        squares(k + 1)
                    A(lambda a: a.activation(out=RS[:, 0:nt], in_=PS[pbn][:, 0:nt], func=AF.Sqrt, scale=1.0 / D, bias=epsb[:, 0:1]),
                      reads=[PB[pbn], B_const], writes=[B_rs])
                    V(lambda v: v.reciprocal(out=RS[:, 0:nt], in_=RS[:, 0:nt]), reads=[B_rs], writes=[B_rs])
                    for j in range(8):
                        s = it % NTMP
                        it += 1
                        V(lambda v, j=j, s=s: v.tensor_tensor(out=TMP[s][:, 0:nt], in0=seq.X[:, j, t0:t0 + nt], in1=RS[:, 0:nt], op=ALU.mult),
                          reads=[xb, B_rs], writes=[B_tmp[s]])
                        if router:
                            A(lambda a, j=j, s=s: a.activation(out=H32[:, j, 0:nt], in_=TMP[s][:, 0:nt], func=AF.Identity,
                                                               scale=GM[:, l, j, n:n + 1], bias=MOD[:, l, shift_which, j, n:n + 1]),
                              reads=[B_tmp[s], B_mod], writes=[B_h32])
                            G(lambda g, j=j: g.tensor_copy(out=seq.H[:, j, t0:t0 + nt], in_=H32[:, j, 0:nt]), reads=[B_h32], writes=[hb])
                        else:
                            A(lambda a, j=j, s=s: a.activation(out=seq.H[:, j, t0:t0 + nt], in_=TMP[s][:, 0:nt], func=AF.Identity,
                                                               scale=GM[:, l, j, n:n + 1], bias=MOD[:, l, shift_which, j, n:n + 1]),
                              reads=[B_tmp[s], B_mod], writes=[hb])
                    if router:
                        for ti in range(nt // 128):
                            c0 = ti * 128
                            mm_group(PS[3][:, 0:8], [(H32[:, j, c0:c0 + 128], rwf[:, j, :]) for j in range(8)],
                                     reads=[B_h32, B_ld], writes=[PB[3]])
                            lg = LG[:, 0:8]
                            w2 = LG[:, 8:16]
                            sm = small
                            V(lambda v: v.tensor_copy(out=lg, in_=PS[3][:, 0:8]), reads=[PB[3]], writes=[B_lg])
                            V(lambda v: v.reduce_max(out=sm[:, 0:1], in_=lg, axis=AX.X), reads=[B_lg], writes=[B_small])
                            V(lambda v: v.tensor_scalar(out=w2, in0=lg, scalar1=sm[:, 0:1], scalar2=-1e30, op0=ALU.is_equal, op1=ALU.mult),
                              reads=[B_lg, B_small], writes=[B_lg])
                            V(lambda v: v.tensor_tensor(out=w2, in0=w2, in1=lg, op=ALU.add), reads=[B_lg], writes=[B_lg])
                            V(lambda v: v.reduce_max(out=sm[:, 1:2], in_=w2, axis=AX.X), reads=[B_lg], writes=[B_small])
                            V(lambda v: v.tensor_scalar(out=w2, in0=lg, scalar1=sm[:, 1:2], scalar2=None, op0=ALU.is_ge),
                              reads=[B_lg, B_small], writes=[B_lg])
                            V(lambda v: v.tensor_scalar(out=sm[:, 2:3], in0=sm[:, 0:1], scalar1=-1.0, scalar2=None, op0=ALU.mult),
                              reads=[B_small], writes=[B_small])
                            A(lambda a: a.activation(out=lg, in_=lg, func=AF.Exp, bias=sm[:, 2:3], scale=1.0), reads=[B_lg, B_small], writes=[B_lg])
                            V(lambda v: v.tensor_tensor(out=lg, in0=lg, in1=w2, op=ALU.mult), reads=[B_lg], writes=[B_lg])
                            V(lambda v: v.reduce_sum(out=sm[:, 3:4], in_=lg, axis=AX.X), reads=[B_lg], writes=[B_small])
                            V(lambda v: v.reciprocal(out=sm[:, 3:4], in_=sm[:, 3:4]), reads=[B_small], writes=[B_small])
                            V(lambda v: v.tensor_scalar(out=lg, in0=lg, scalar1=sm[:, 3:4], scalar2=None, op0=ALU.mult),
                              reads=[B_lg, B_small], writes=[B_lg])
                            PE(lambda pe: pe.transpose(PS[2][0:8, 0:128], lg, ident[:]), reads=[B_lg, B_ld], writes=[PB[2]])
                            V(lambda v, c0=c0: v.tensor_copy(out=gateT[0:8, t0 + c0:t0 + c0 + 128], in_=PS[2][0:8, 0:128]),
                              reads=[PB[2]], writes=[B_gateT])

        def wout_update(seq, l, t0, nt, YT, B_y, WO, B_wo, bank0):
            xb = seq.XB[blk_of(seq, t0)]
            n = seq.n
            for dj in range(8):
                bk = bank0 + dj % 2
                mm_group(PS[bk][:, 0:nt], [(WO[:, c, dj * 128:(dj + 1) * 128], YT[:, c, 0:nt]) for c in range(2)],
                         reads=[B_y, B_wo], writes=[PB[bk]])
                V(lambda v, dj=dj, bk=bk: v.scalar_tensor_tensor(
                    out=seq.X[:, dj, t0:t0 + nt], in0=PS[bk][:, 0:nt], scalar=MOD[:, l, 2, dj, n:n + 1],
                    in1=seq.X[:, dj, t0:t0 + nt], op0=ALU.mult, op1=ALU.add),
                  reads=[PB[bk], B_mod, xb], writes=[xb])

        def load_wout(l, m, WO, B_wo):
            cx.dma("gpsimd", WO, w_out_d[l, m * 256:(m + 1) * 256, :].rearrange("(c p) n -> p c n", p=128), writes=[B_wo])

        def load_win(l, c0, c1, W, B_w):
            cx.dma("gpsimd", W, w_in_d[l, :, c0:c1].rearrange("(j p) n -> p j n", p=128), writes=[B_w])

        def load_layer_small(l):
            for dst, src, q in ((gmlp_g[:], gmlp_g_d[l], "sync"), (spw[:], spw_d[l], "gpsimd"), (spb[:], spb_d[l], "sync"),
                                (bdwp[:], bdwp_d[l], "gpsimd"), (pscale[:], pscale_d[l], "sync"), (qng[:], qng_d[l], "sync"),
                                (kvng[:], kvng_d[l], "sync"), (qkq[:], qkq_d[l], "sync"), (qkk[:], qkk_d[l], "sync"),
                                (wuq[:, 0, :], w_uq_d[l, 0:128, :], "gpsimd"), (wuq[0:64, 1, :], w_uq_d[l, 128:192, :], "gpsimd"),
                                (wukv[:], w_ukv_d[l], "gpsimd"),
                                (wfb[:], wf_d[l].rearrange("(c p) n -> p c n", p=128), "gpsimd")):
                cx.dma(q, dst, src, writes=[B_lw])
            for csi in range(2):
                for c in range(2):
                    mm_group(PS[0][:, 0:256], [(bd64[:, csi, :], wfb[:, c, :])], reads=[B_ld, B_lw], writes=[PB[0]])
                    V(lambda v, csi=csi, c=c: v.tensor_copy(out=WCS[:, csi, c, :], in_=PS[0][:, 0:256]), reads=[PB[0]], writes=[B_lw])

        def mixer_attention(l, seqs_q, do_ctx_q):
            aa.reset()
            WINC = aa.get([128, 8, 352], BF16); B_w = Buf("winc")
            WO = aa.get([128, 2, 1024], BF16); B_wo = Buf("woc")
            KT = aa.get([128, 4, NK], BF16); B_kt = Buf("kt")
            VP = aa.get([128, 18, 2, 192], BF16); B_vp = Buf("vp")

            class _S:
                pass

            def mk(i):
                o = _S()
                o.TM = aa.get([128, 352], F32); o.B_tm = Buf(f"tm{i}")
                o.CN = aa.get([128, 320], BF16); o.B_cn = Buf(f"cn{i}")
                o.CNT = aa.get([128, 3, 128], BF16); o.B_cnt = Buf(f"cnt{i}")
                o.QF = aa.get([128, 4, 96], F32); o.B_qf = Buf(f"qf{i}")
                o.KF = o.QF; o.B_kf = o.B_qf
                o.QB = aa.get([128, 4, 96], BF16); o.B_qb = Buf(f"qb{i}")
                o.KB = o.QB; o.B_kb = o.B_qb
                if i < 2:
                    o.sm = small2[i]
                else:
                    o.sm = aa.get([128, 32], F32)
                o.B_small = Buf(f"small{i}")
                o.pb = i
                o.ub = 4 + i
                return o
            SS = [mk(0), mk(1)]
            R1 = aa.get([128, 4, 32], F32); B_r1 = Buf("r1")
            SQS = aa.get([128, 384], F32); B_sqs = Buf("sqs")
            SQA = aa.get([128, 192], BF16); B_sqa = Buf("sqa")
            off_q = aa.off
            SS += [mk(2), mk(3)]
            end_extra = aa.off
            aa.off = off_q
            QT2 = [aa.get([128, 4, 512], BF16) for _ in range(2)]; B_qt2 = [Buf("qt0"), Buf("qt1")]
            PT = [aa.get([128, 512], BF16) for _ in range(3)]; B_pt = [Buf(f"pt{i}") for i in range(3)]
            RD = aa.get([128, 512], F32); B_rd = Buf("rd")
            BC = aa.get([128, 512], F32); B_bc = Buf("bc")
            AT = aa.get([128, 2, 512], BF16); B_at = Buf("at")
            aa.off = max(aa.off, end_extra)
            for o in SS:
                o.R1 = R1; o.B_r1 = B_r1; o.SQS = SQS; o.B_sqs = B_sqs
            load_win(l, OFF_CQ, OFF_D, WINC, B_w)
            load_wout(l, 2, WO, B_wo)
            V(lambda v: v.memset(VP.rearrange("p a b c -> p (a b c)"), 0.0), writes=[B_vp])
            for pr_ in range(2):
                V(lambda v, pr_=pr_: v.memset(VP[:, :, pr_, 64:65], 1.0), writes=[B_vp])

            def rms_scale(S, src_ap, width, col):
                A(lambda a: a.activation(out=SQA[:, 0:width], in_=src_ap, func=AF.Square, accum_out=S.sm[:, col:col + 1]),
                  reads=[S.B_tm], writes=[B_sqa, S.B_small])
                A(lambda a: a.activation(out=S.sm[:, col:col + 1], in_=S.sm[:, col:col + 1], func=AF.Ln, scale=1.0 / width, bias=epsb[:, 0:1]),
                  reads=[S.B_small, B_const], writes=[S.B_small])
                A(lambda a: a.activation(out=S.sm[:, col:col + 1], in_=S.sm[:, col:col + 1], func=AF.Exp, scale=-0.5),
                  reads=[S.B_small], writes=[S.B_small])

            def head_norm(S, src, B_src, gains, dst, B_dst, colbase):
                V(lambda v: v.tensor_tensor(out=S.SQS[:].rearrange("p (h d) -> p h d", h=4), in0=src, in1=src, op=ALU.mult),
                  reads=[B_src], writes=[S.B_sqs])
                V(lambda v: v.reduce_sum(out=S.sm[:, colbase:colbase + 4], in_=S.SQS[:].rearrange("p (h d) -> p h d", h=4), axis=AX.X),
                  reads=[S.B_sqs], writes=[S.B_small])
                yield
                A(lambda a: a.activation(out=S.sm[:, colbase:colbase + 4], in_=S.sm[:, colbase:colbase + 4], func=AF.Ln, scale=1.0 / 96, bias=epsb[:, 0:1]),
                  reads=[S.B_small, B_const], writes=[S.B_small])
                A(lambda a: a.activation(out=S.sm[:, colbase:colbase + 4], in_=S.sm[:, colbase:colbase + 4], func=AF.Exp, scale=-0.5),
                  reads=[S.B_small], writes=[S.B_small])
                yield
                V(lambda v: v.tensor_tensor(out=dst, in0=src, in1=S.sm[:, colbase:colbase + 4].unsqueeze(2).to_broadcast([128, 4, 96]), op=ALU.mult),
                  reads=[B_src, S.B_small], writes=[B_dst])
                V(lambda v: v.tensor_tensor(out=dst, in0=dst, in1=gains.rearrange("p (h d) -> p h d", h=4), op=ALU.mult),
                  reads=[B_dst, B_lw], writes=[B_dst])

            def rope(S, src, B_src, ti):
                xr = src[:, :, 64:96]
                cosb = rcos[:, ti, :].unsqueeze(1).to_broadcast([128, 4, 32])
                x5 = xr.rearrange("p h (a b f) -> p h a b f", a=2, b=2)
                r5 = S.R1[:].rearrange("p h (a b f) -> p h a b f", a=2, b=2)
                s5 = rsin[:, ti, :].rearrange("p (a b f) -> p a b f", a=2, b=2)
                for bsel in range(2):
                    V(lambda v, bsel=bsel: v.tensor_tensor(
                        out=r5[:, :, :, bsel, :], in0=x5[:, :, :, 1 - bsel, :],
                        in1=s5[:, :, bsel, :].unsqueeze(1).to_broadcast([128, 4, 2, 8]), op=ALU.mult),
                      reads=[B_src, B_ld], writes=[S.B_r1])
                V(lambda v: v.tensor_tensor(out=xr, in0=xr, in1=cosb, op=ALU.mult), reads=[B_src, B_ld], writes=[B_src])
                V(lambda v: v.tensor_tensor(out=xr, in0=xr, in1=S.R1[:], op=ALU.add), reads=[B_src, S.B_r1], writes=[B_src])

            def run_window(jobs, sets, make):
                jobs = list(jobs)
                free = list(sets)
                active = []
                while jobs or active:
                    while jobs and free:
                        si = free.pop(0)
                        active.append((make(jobs.pop(0), si), si))
                    for item in list(active):
                        g, si = item
                        try:
                            next(g)
                        except StopIteration:
                            active.remove(item)
                            free.append(si)
                        yield

            def kv_tile(seq, ti, si):
                S = SS[si]
                P, PBp = PS[S.pb], PB[S.pb]
                U, PBu = PS[S.ub], PB[S.ub]
                pbt = P[:].bitcast(BF16)
                t0 = ti * 128
                g0 = seq.tok0 + t0
                kt_i = g0 // 128
                hb = seq.HB[blk_of(seq, t0)]
                mm_group(P[:, 192:352], [(seq.H[:, j, t0:t0 + 128], WINC[:, j, 192:352]) for j in range(8)], reads=[hb, B_w], writes=[PBp])
                yield
                A(lambda a: a.copy(out=S.TM[:, 192:352], in_=P[:, 192:352]), reads=[PBp], writes=[S.B_tm])
                rms_scale(S, S.TM[:, 192:320], 128, 1)
                yield
                V(lambda v: v.scalar_tensor_tensor(out=S.CN[:, 192:320], in0=S.TM[:, 192:320], scalar=S.sm[:, 1:2], in1=kvng[:], op0=ALU.mult, op1=ALU.mult),
                  reads=[S.B_tm, S.B_small, B_lw], writes=[S.B_cn])
                yield
                PE(lambda pe: pe.transpose(pbt[:, 0:128], S.CN[:, 192:320], identb[:]), reads=[S.B_cn, B_ld], writes=[PBp])
                yield
                V(lambda v: v.tensor_copy(out=S.CNT[:, 2, :], in_=pbt[:, 0:128]), reads=[PBp], writes=[S.B_cnt])
                yield
                mm_group(U[:, 0:512], [(S.CNT[:, 2, :], wukv[:])], reads=[S.B_cnt, B_lw], writes=[PBu])
                yield
                kv4 = U[:, 0:512].rearrange("p (h d) -> p h d", h=4)
                for e in range(2):
                    A(lambda a, e=e: a.copy(out=VP[:, kt_i, :, e * 128:e * 128 + 64], in_=kv4[:, e:4:2, 64:128]),
                      reads=[PBu], writes=[B_vp])
                A(lambda a: a.copy(out=S.KF[:, :, 0:64], in_=kv4[:, :, 0:64]), reads=[PBu], writes=[S.B_kf])
                V(lambda v: v.tensor_copy(out=S.KF[:, :, 64:96], in_=S.TM[:, 320:352].unsqueeze(1).to_broadcast([128, 4, 32])),
                  reads=[S.B_tm], writes=[S.B_kf])
                yield
                for _ in head_norm(S, S.KF[:], S.B_kf, qkk[:], S.KF[:], S.B_kf, 8):
                    yield
                if seq.rope:
                    rope(S, S.KF[:], S.B_kf, ti)
                V(lambda v: v.tensor_copy(out=S.KB[:], in_=S.KF[:]), reads=[S.B_kf], writes=[S.B_kb])
                yield

                def trk(pe):
                    last = None
                    for h in range(4):
                        last = pe.transpose(pbt[0:96, h * 128:(h + 1) * 128], S.KB[:, h, :], identb[:])
                    return last
                PE(trk, reads=[S.B_kb, B_ld], writes=[PBp])
                yield
                V(lambda v: v.tensor_copy(out=KT[0:96, :, g0:g0 + 128], in_=pbt[0:96, 0:512].rearrange("p (h t) -> p h t", h=4)),
                  reads=[PBp], writes=[B_kt])

            def q_tile(seq, ti, qc, qs, si):
                S = SS[si]
                P, PBp = PS[S.pb], PB[S.pb]
                pbt = P[:].bitcast(BF16)
                t0 = ti * 128
                hb = seq.HB[blk_of(seq, t0)]
                QT = QT2[qs]; B_qt = B_qt2[qs]
                mm_group(P[:, 0:192], [(seq.H[:, j, t0:t0 + 128], WINC[:, j, 0:192]) for j in range(8)], reads=[hb, B_w], writes=[PBp])
                yield
                A(lambda a: a.copy(out=S.TM[:, 0:192], in_=P[:, 0:192]), reads=[PBp], writes=[S.B_tm])
                rms_scale(S, S.TM[:, 0:192], 192, 0)
                yield
                V(lambda v: v.scalar_tensor_tensor(out=S.CN[:, 0:192], in0=S.TM[:, 0:192], scalar=S.sm[:, 0:1], in1=qng[:], op0=ALU.mult, op1=ALU.mult),
                  reads=[S.B_tm, S.B_small, B_lw], writes=[S.B_cn])
                yield
                yield

                def tr(pe):
                    pe.transpose(pbt[:, 0:128], S.CN[:, 0:128], identb[:])
                    return pe.transpose(pbt[0:64, 128:256], S.CN[:, 128:192], identb[:])
                PE(tr, reads=[S.B_cn, B_ld], writes=[PBp])
                yield
                V(lambda v: v.tensor_copy(out=S.CNT[:, 0, :], in_=pbt[:, 0:128]), reads=[PBp], writes=[S.B_cnt])
                V(lambda v: v.tensor_copy(out=S.CNT[0:64, 1, :], in_=pbt[0:64, 128:256]), reads=[PBp], writes=[S.B_cnt])
                yield
                yield
                mm_group(P[:, 128:512], [(S.CNT[:, 0, :], wuq[:, 0, :]), (S.CNT[0:64, 1, :], wuq[0:64, 1, :])],
                         reads=[S.B_cnt, B_lw], writes=[PBp])
                yield
                A(lambda a: a.copy(out=S.QF[:].rearrange("p h d -> p (h d)"), in_=P[:, 128:512]), reads=[PBp], writes=[S.B_qf])
                yield
                for _ in head_norm(S, S.QF[:], S.B_qf, qkq[:], S.QF[:], S.B_qf, 12):
                    yield
                if seq.rope:
                    rope(S, S.QF[:], S.B_qf, ti)
                V(lambda v: v.tensor_copy(out=S.QB[:], in_=S.QF[:]), reads=[S.B_qf], writes=[S.B_qb])
                yield
                yield
                yield

                def trq(pe):
                    last = None
                    for h in range(4):
                        last = pe.transpose(pbt[0:96, h * 128:(h + 1) * 128], S.QB[:, h, :], identb[:])
                    return last
                PE(trq, reads=[S.B_qb, B_ld], writes=[PBp])
                yield
                V(lambda v: v.tensor_copy(out=QT[0:96, :, qc:qc + 128], in_=pbt[0:96, 0:512].rearrange("p (h t) -> p h t", h=4)),
                  reads=[PBp], writes=[B_qt])

            kv_jobs = [(seq, ti) for seq in (cs, lat) for ti in range(seq.ntile)]
            for _ in run_window(kv_jobs, [0, 1, 2, 3], lambda job, si: kv_tile(job[0], job[1], si)):
                pass
            chk("attn_kv")
            cx.barrier()
            SS[1].pb = 3

            scale = 96 ** -0.5
            it = [0]
            blks = [(seq, t0, nt) for seq in seqs_q for (t0, nt) in seq.blocks]

            def q_jobs(bi):
                seq, t0, nt = blks[bi]
                jobs = [(seq, t0 // 128 + tq, tq * 128, bi % 2) for tq in range(nt // 128)]
                return run_window(jobs, [0, 1], lambda job, si: q_tile(job[0], job[1], job[2], job[3], si))

            for _ in q_jobs(0):
                pass
            for bi, (seq, t0, nt) in enumerate(blks):
                    nkt = 18 if seq is lat else 2
                    QT = QT2[bi % 2]; B_qt = B_qt2[bi % 2]
                    nxt = q_jobs(bi + 1) if bi + 1 < len(blks) else iter(())
                    items = [(h, kt_i) for h in range(4) for kt_i in range(nkt)]
                    slot_of = {}

                    def do_S(i):
                        h, kt_i = items[i]
                        s = it[0] % 3
                        sbk = 1 + it[0] % 2
                        it[0] += 1
                        slot_of[i] = s
                        mm_group(PS[sbk][:, 0:nt], [(KT[0:96, h, kt_i * 128:(kt_i + 1) * 128], QT[0:96, h, 0:nt])],
                                 reads=[B_kt, B_qt], writes=[PB[sbk]])
                        A(lambda a: a.activation(out=PT[s][:, 0:nt], in_=PS[sbk][:, 0:nt], func=AF.Exp, scale=scale),
                          reads=[PB[sbk]], writes=[B_pt[s]])

                    def do_PV(i):
                        h, kt_i = items[i]
                        s = slot_of[i]
                        pr, odd = h // 2, h % 2
                        accb = 4 + h % 2
                        acc = PS[accb]
                        lhs = VP[:, kt_i, pr, 64:192] if odd else VP[:, kt_i, pr, 0:65]
                        M = 128 if odd else 65
                        PE(lambda pe: pe.matmul(acc[0:M, 0:nt], lhs, PT[s][:, 0:nt], start=(kt_i == 0), stop=(kt_i == nkt - 1)),
                           reads=[B_vp, B_pt[s]], writes=[PB[accb]])
                        if kt_i == nkt - 1:
                            dr = 0 if odd else 64
                            V(lambda v: v.reciprocal(out=RD[dr:dr + 1, 0:nt], in_=acc[dr:dr + 1, 0:nt]), reads=[PB[accb]], writes=[B_rd])

                    def do_norm(h, stage):
                        pr, odd = h // 2, h % 2
                        accb = 4 + h % 2
                        acc = PS[accb]
                        dr = 0 if odd else 64
                        if stage == 0:
                            mm_group(PS[6][:, 0:nt], [(onesf[dr:dr + 1, :], RD[dr:dr + 1, 0:nt])], reads=[B_rd, B_const], writes=[PB[6]])
                        elif stage == 1:
                            A(lambda a: a.copy(out=BC[:, 0:nt], in_=PS[6][:, 0:nt]), reads=[PB[6]], writes=[B_bc])
                        else:
                            r0 = 64 if odd else 0
                            V(lambda v: v.tensor_tensor(out=AT[r0:r0 + 64, pr, 0:nt], in0=acc[r0:r0 + 64, 0:nt], in1=BC[r0:r0 + 64, 0:nt], op=ALU.mult),
                              reads=[PB[accb], B_bc], writes=[B_at])

                    n_it = len(items)
                    LA = 2
                    last_step = n_it + LA - 1
                    offs = (8, 10, 12) if nkt >= 16 else (2, 2, 2)
                    due = {}
                    for i in range(n_it + LA):
                        if i < n_it:
                            do_S(i)
                        j = i - LA
                        if j >= 0:
                            do_PV(j)
                            h, kt_i = items[j]
                            if kt_i == nkt - 1:
                                for stage in range(3):
                                    due.setdefault(min(i + offs[stage], last_step), []).append((h, stage))
                        for (h, stage) in due.pop(i, []):
                            do_norm(h, stage)
                        next(nxt, None)
                    assert not due
                    for _ in nxt:
                        pass
                    wout_update(seq, l, t0, nt, AT, B_at, WO, B_wo, 6)

        def mixer_spatial(l, seqs):
            aa.reset()
            WINA = aa.get([128, 8, 512], BF16); B_w = Buf("wina")
            WO = aa.get([128, 2, 1024], BF16); B_wo = Buf("woa")
            UV = [aa.get([128, 512], F32) for _ in range(4)]; B_uv = [Buf(f"uv{i}") for i in range(4)]
            VN = [aa.get([128, 256], BF16) for _ in range(2)]; B_vn = [Buf(f"vn{i}") for i in range(2)]
            SQS = aa.get([128, 256], F32); B_sqs = Buf("sqsa")
            YTK = [aa.get([128, 256], BF16) for _ in range(2)]; B_ytk = [Buf(f"ytk{i}") for i in range(2)]
            MX = [aa.get([128, 256], F32) for _ in range(2)]; B_mx = [Buf(f"mx{i}") for i in range(2)]
            YA = aa.get([128, 2, 512], BF16); B_ya = Buf("ya")
            load_win(l, OFF_A, OFF_B, WINA, B_w)
            load_wout(l, 0, WO, B_wo)
            sm = small
            B_sm = Buf("sm_sp")
            pbt = PS[7][:].bitcast(BF16)
            for seq in seqs:
                for (b0, nt) in seq.blocks:
                    ntile = nt // 128
                    for ti in range(ntile):
                        t0 = b0 + ti * 128
                        hb = seq.HB[blk_of(seq, t0)]
                        bk = ti % 2
                        mm_group(PS[bk][:, 0:512], [(seq.H[:, j, t0:t0 + 128], WINA[:, j, :]) for j in range(8)], reads=[hb, B_w], writes=[PB[bk]])
                        A(lambda a, ti=ti, bk=bk: a.activation(out=UV[ti][:], in_=PS[bk][:, 0:512], func=AF.Gelu), reads=[PB[bk]], writes=[B_uv[ti]])
                        V(lambda v, ti=ti: v.tensor_tensor(out=SQS[:], in0=UV[ti][:, 256:512], in1=UV[ti][:, 256:512], op=ALU.mult),
                          reads=[B_uv[ti]], writes=[B_sqs])
                        V(lambda v, ti=ti: v.reduce_sum(out=sm[:, 20 + ti:21 + ti], in_=SQS[:], axis=AX.X), reads=[B_sqs], writes=[B_sm])
                    A(lambda a: a.activation(out=sm[:, 20:20 + ntile], in_=sm[:, 20:20 + ntile], func=AF.Sqrt, scale=1.0 / 256, bias=epsb[:, 0:1]),
                      reads=[B_sm, B_const], writes=[B_sm])
                    V(lambda v: v.reciprocal(out=sm[:, 20:20 + ntile], in_=sm[:, 20:20 + ntile]), reads=[B_sm], writes=[B_sm])
                    for ti in range(ntile):
                        p2 = ti % 2
                        V(lambda v, ti=ti, p2=p2: v.scalar_tensor_tensor(out=VN[p2][:], in0=UV[ti][:, 256:512], scalar=sm[:, 20 + ti:21 + ti], in1=gmlp_g[:],
                                                                         op0=ALU.mult, op1=ALU.mult),
                          reads=[B_uv[ti], B_sm, B_lw], writes=[B_vn[p2]])
                        mbk = 2 + p2

                        def mix(pe, p2=p2, mbk=mbk):
                            last = None
                            for g in range(4):
                                last = pe.matmul(PS[mbk][:, g * 64:(g + 1) * 64], spw[:, g, :], VN[p2][:, g * 64:(g + 1) * 64], start=True, stop=True)
                            return last
                        PE(mix, reads=[B_vn[p2], B_lw], writes=[PB[mbk]])
                        V(lambda v, p2=p2, mbk=mbk: v.tensor_tensor(out=MX[p2][:].rearrange("p (g c) -> p g c", g=4),
                                                                    in0=PS[mbk][:, 0:256].rearrange("p (g c) -> p g c", g=4),
                                                                    in1=spb[:].unsqueeze(2).to_broadcast([128, 4, 64]), op=ALU.add),
                          reads=[PB[mbk], B_lw], writes=[B_mx[p2]])
                        G(lambda g, p2=p2, ti=ti: g.tensor_tensor(out=YTK[p2][:], in0=MX[p2][:], in1=UV[ti][:, 0:256], op=ALU.mult),
                          reads=[B_mx[p2], B_uv[ti]], writes=[B_ytk[p2]])

                        def tr(pe, p2=p2):
                            pe.transpose(pbt[:, p2 * 256:p2 * 256 + 128], YTK[p2][:, 0:128], identb[:])
                            return pe.transpose(pbt[:, p2 * 256 + 128:p2 * 256 + 256], YTK[p2][:, 128:256], identb[:])
                        PE(tr, reads=[B_ytk[p2], B_ld], writes=[PB[7]])
                        A(lambda a, ti=ti, p2=p2: a.copy(out=YA[:, :, ti * 128:(ti + 1) * 128],
                                                         in_=pbt[:, p2 * 256:p2 * 256 + 256].rearrange("p (c t) -> p c t", c=2)),
                          reads=[PB[7]], writes=[B_ya])
                    wout_update(seq, l, b0, nt, YA, B_ya, WO, B_wo, 4)

        def mixer_pool(l, seqs):
            for seq in seqs:
                aa.reset()
                L = seq.T
                W = L + 2 * PADW
                WINB = aa.get([128, 8, 256], BF16); B_w = Buf("winb")
                WO = aa.get([128, 2, 1024], BF16); B_wo = Buf("wob")
                ZB = aa.get([128, 2, W], F32); B_zb = Buf("zb")
                T1 = aa.get([128, W], F32); B_t1 = Buf("t1")
                T2 = aa.get([128, W], F32); B_t2 = Buf("t2")
                DT = aa.get([128, 2, L], BF16); B_dt = Buf("dt")
                ET = aa.get([128, 16], F32); B_et = Buf("et")
                YB = aa.get([128, 2, 512], BF16); B_yb = Buf("yb")
                load_win(l, OFF_B, OFF_CQ, WINB, B_w)
                load_wout(l, 1, WO, B_wo)
                G(lambda g: g.memset(ZB[:, :, 0:PADW], 0.0), writes=[B_zb])
                G(lambda g: g.memset(ZB[:, :, PADW + L:W], 0.0), writes=[B_zb])
                ib = 0
                for (b0, nt) in seq.blocks:
                    hb = seq.HB[blk_of(seq, b0)]
                    for c in range(2):
                        bk = ib % 2
                        ib += 1
                        mm_group(PS[bk][:, 0:nt], [(WINB[:, j, c * 128:(c + 1) * 128], seq.H[:, j, b0:b0 + nt]) for j in range(8)],
                                 reads=[hb, B_w], writes=[PB[bk]])
                        A(lambda a, c=c, bk=bk: a.copy(out=ZB[:, c, PADW + b0:PADW + b0 + nt], in_=PS[bk][:, 0:nt]), reads=[PB[bk]], writes=[B_zb])
                for c in range(2):
                    z = ZB[:, c, :]
                    V(lambda v: v.tensor_tensor(out=T1[:, 1:W], in0=z[:, 0:W - 1], in1=z[:, 1:W], op=ALU.add), reads=[B_zb], writes=[B_t1])
                    if c == 0:
                        lo_src, lo_w = T1, 2
                        G(lambda g: g.tensor_tensor(out=T2[64:128, 2:W - 1], in0=T1[64:128, 1:W - 2], in1=T1[64:128, 3:W], op=ALU.add),
                          reads=[B_t1], writes=[B_t2])
                        hi_w = 4
                    else:
                        V(lambda v: v.tensor_tensor(out=T2[:, 2:W - 1], in0=T1[:, 1:W - 2], in1=T1[:, 3:W], op=ALU.add), reads=[B_t1], writes=[B_t2])
                        V(lambda v: v.tensor_tensor(out=T1[:, 4:W - 3], in0=T2[:, 2:W - 5], in1=T2[:, 6:W - 1], op=ALU.add), reads=[B_t2], writes=[B_t1])
                        G(lambda g: g.tensor_tensor(out=T2[64:128, 8:W - 7], in0=T1[64:128, 4:W - 11], in1=T1[64:128, 12:W - 3], op=ALU.add),
                          reads=[B_t1], writes=[B_t2])
                        lo_w, hi_w = 8, 16
                    for (r0, S, B_s, w) in ((0, T1, B_t1, lo_w), (64, T2, B_t2, hi_w)):
                        rs = slice(r0, r0 + 64)
                        V(lambda v, rs=rs, S=S, w=w, c=c: v.scalar_tensor_tensor(
                            out=DT[rs, c, :], in0=S[rs, PADW:PADW + L], scalar=1.0 / w, in1=ZB[rs, c, PADW:PADW + L], op0=ALU.mult, op1=ALU.subtract),
                          reads=[B_s, B_zb], writes=[B_dt])
                        for (e0, tcol) in ((0, 0), (8, L - 8)):
                            V(lambda v, rs=rs, S=S, e0=e0, tcol=tcol, c=c: v.tensor_tensor(
                                out=ET[rs, e0:e0 + 8], in0=S[rs, PADW + tcol:PADW + tcol + 8], in1=poole[rs, c, e0:e0 + 8], op=ALU.mult),
                              reads=[B_s, B_ld], writes=[B_et])
                            V(lambda v, rs=rs, e0=e0, tcol=tcol, c=c: v.tensor_tensor(
                                out=DT[rs, c, tcol:tcol + 8], in0=ET[rs, e0:e0 + 8], in1=ZB[rs, c, PADW + tcol:PADW + tcol + 8], op=ALU.subtract),
                              reads=[B_et, B_zb], writes=[B_dt])
                for (b0, nt) in seq.blocks:
                    for c in range(2):
                        bk = c
                        mm_group(PS[bk][:, 0:nt], [(bdwp[:, c, :], DT[:, c, b0:b0 + nt])], reads=[B_dt, B_lw], writes=[PB[bk]])
                        A(lambda a, c=c, bk=bk: a.activation(out=YB[:, c, 0:nt], in_=PS[bk][:, 0:nt], func=AF.Identity, scale=pscale[:, c:c + 1]),
                          reads=[PB[bk], B_lw], writes=[B_yb])
                    wout_update(seq, l, b0, nt, YB, B_yb, WO, B_wo, 2)

        def mixer_fourier(l, seqs):
            for seq in seqs:
                aa.reset()
                L = seq.T
                ntile = seq.ntile
                kbw = 512 if seq is lat else TC
                WIND = aa.get([128, 8, 256], BF16); B_w = Buf("wind")
                WO = aa.get([128, 2, 1024], BF16); B_wo = Buf("wod")
                ZD = aa.get([128, ntile, 256], BF16); B_zd = Buf("zd")
                TAB = aa.get([128, 2, ntile, kbw], BF16); B_tab = Buf("tab")
                UT = aa.get([128, 2, 2, 512], BF16); B_ut = Buf("ut")
                YD = aa.get([128, 2, 512], BF16); B_yd = Buf("yd")
                load_win(l, OFF_D, IN_W, WIND, B_w)
                load_wout(l, 3, WO, B_wo)
                for ti in range(ntile):
                    t0 = ti * 128
                    hb = seq.HB[blk_of(seq, t0)]
                    bk = ti % 2
                    mm_group(PS[bk][:, 0:256], [(seq.H[:, j, t0:t0 + 128], WIND[:, j, :]) for j in range(8)], reads=[hb, B_w], writes=[PB[bk]])
                    A(lambda a, ti=ti, bk=bk: a.copy(out=ZD[:, ti, :], in_=PS[bk][:, 0:256]), reads=[PB[bk]], writes=[B_zd])
                for kb, (b0, nt) in enumerate(seq.blocks):
                    if seq is lat:
                        cx.dma("sync", TAB[:, 0], dftc_d[kb], writes=[B_tab])
                        cx.dma("sync", TAB[:, 1], dfts_d[kb], writes=[B_tab])
                    else:
                        cx.dma("sync", TAB[:, 0], dftcc_d, writes=[B_tab])
                        cx.dma("sync", TAB[:, 1], dftcs_d, writes=[B_tab])
                    for csi in range(2):
                        for c in range(2):
                            bk = csi * 2 + c
                            mm_group(PS[bk][:, 0:nt], [(ZD[:, ti, c * 128:(c + 1) * 128], TAB[:, csi, ti, 0:nt]) for ti in range(ntile)],
                                     reads=[B_zd, B_tab], writes=[PB[bk]])
                            if bk % 2 == 0:
                                A(lambda a, csi=csi, c=c, bk=bk: a.copy(out=UT[:, csi, c, 0:nt], in_=PS[bk][:, 0:nt]), reads=[PB[bk]], writes=[B_ut])
                            else:
                                V(lambda v, csi=csi, c=c, bk=bk: v.tensor_copy(out=UT[:, csi, c, 0:nt], in_=PS[bk][:, 0:nt]), reads=[PB[bk]], writes=[B_ut])
                    for dj in range(2):
                        bk = 4 + dj
                        mm_group(PS[bk][:, 0:nt], [(WCS[:, csi, c, dj * 128:(dj + 1) * 128], UT[:, csi, c, 0:nt]) for csi in range(2) for c in range(2)],
                                 reads=[B_ut, B_lw], writes=[PB[bk]])
                        A(lambda a, dj=dj, bk=bk: a.copy(out=YD[:, dj, 0:nt], in_=PS[bk][:, 0:nt]), reads=[PB[bk]], writes=[B_yd])
                    wout_update(seq, l, b0, nt, YD, B_yd, WO, B_wo, 6)

        def ffn_phase(l, seqs, experts):
            moe = experts[0][3] is not None
            SL = 512
            nsl = DFF // SL
            WG = [aa.get([128, 8, SL], BF16) for _ in range(2)]
            WU = [aa.get([128, 8, SL], BF16) for _ in range(2)]
            WD = [aa.get([128, 4, D], BF16) for _ in range(2)]
            B_wgu = [Buf("wgu0"), Buf("wgu1")]
            B_wd = [Buf("wd0"), Buf("wd1")]
            ACT = [aa.get([128, 4, 512], BF16) for _ in range(2)]
            B_act = [Buf("act0"), Buf("act1")]
            SG = [aa.get([128, 512], F32) for _ in range(2)]
            B_sg = [Buf("sg0"), Buf("sg1")]
            PAB = [Buf("pab0"), Buf("pab1")]
            PC = [Buf("pc0"), Buf("pc1")]
            if moe:
                GBC = aa.get([128, T], F32)
                B_gbc = Buf("gbc")
            work = [(ei, si) for ei in range(len(experts)) for si in range(nsl)]

            def issue_load(idx):
                ei, si = work[idx]
                wg, wu, wd, ge = experts[ei]
                s = idx % 2
                f0 = si * SL
                cx.dma("gpsimd", WG[s], wg[:, f0:f0 + SL].rearrange("(j p) n -> p j n", p=128), writes=[B_wgu[s]])
                cx.dma("gpsimd", WU[s], wu[:, f0:f0 + SL].rearrange("(j p) n -> p j n", p=128), writes=[B_wgu[s]])
                cx.dma("gpsimd", WD[s], wd[f0:f0 + SL, :].rearrange("(c p) n -> p c n", p=128), writes=[B_wd[s]])

            cnt = {"fc": 0, "blk": 0, "c": 0}

            def s1_begin():
                a_s = cnt["blk"] % 2
                cnt["blk"] += 1
                return a_s

            def s1_fc(idx, seq, bi, a_s, fc):
                s = idx % 2
                b0, nt = seq.blocks[bi]
                hb = seq.HB[bi]
                ba = cnt["fc"] % 2
                cnt["fc"] += 1
                pa, pbk = PS[2 * ba], PS[2 * ba + 1]

                def gu(pe):
                    last = None
                    for j in range(8):
                        last = pe.matmul(pa[:, 0:nt], WG[s][:, j, fc * 128:(fc + 1) * 128], seq.H[:, j, b0:b0 + nt], start=(j == 0), stop=(j == 7))
                    for j in range(8):
                        last = pe.matmul(pbk[:, 0:nt], WU[s][:, j, fc * 128:(fc + 1) * 128], seq.H[:, j, b0:b0 + nt], start=(j == 0), stop=(j == 7))
                    return last
                PE(gu, reads=[B_wgu[s], hb], writes=[PAB[ba]])
                A(lambda a: a.activation(out=SG[ba][:, 0:nt], in_=pa[:, 0:nt], func=AF.Silu), reads=[PAB[ba]], writes=[B_sg[ba]])
                if moe:
                    G(lambda g: g.tensor_tensor(out=SG[ba][:, 0:nt], in0=SG[ba][:, 0:nt], in1=GBC[:, b0:b0 + nt], op=ALU.mult),
                      reads=[B_sg[ba], B_gbc], writes=[B_sg[ba]])
                V(lambda v: v.tensor_tensor(out=ACT[a_s][:, fc, 0:nt], in0=SG[ba][:, 0:nt], in1=pbk[:, 0:nt], op=ALU.mult),
                  reads=[B_sg[ba], PAB[ba]], writes=[B_act[a_s]])

            def s2_dp(idx, seq, bi, a_s, dp):
                s = idx % 2
                b0, nt = seq.blocks[bi]
                xb = seq.XB[bi]
                n = seq.n
                pc = cnt["c"] % 2
                cnt["c"] += 1
                banks = (PS[4 + 2 * pc], PS[5 + 2 * pc])

                def dn(pe):
                    last = None
                    for k2 in range(2):
                        dj = dp * 2 + k2
                        for fc in range(4):
                            last = pe.matmul(banks[k2][:, 0:nt], WD[s][:, fc, dj * 128:(dj + 1) * 128], ACT[a_s][:, fc, 0:nt],
                                             start=(fc == 0), stop=(fc == 3))
                    return last
                PE(dn, reads=[B_wd[s], B_act[a_s]], writes=[PC[pc]])
                for k2 in range(2):
                    dj = dp * 2 + k2
                    V(lambda v, dj=dj, bank=banks[k2]: v.scalar_tensor_tensor(
                        out=seq.X[:, dj, b0:b0 + nt], in0=bank[:, 0:nt], scalar=MOD[:, l, 5, dj, n:n + 1],
                        in1=seq.X[:, dj, b0:b0 + nt], op0=ALU.mult, op1=ALU.add),
                      reads=[PC[pc], B_mod, xb], writes=[xb])

            def stage2(idx, seq, bi, a_s):
                for dp in range(4):
                    s2_dp(idx, seq, bi, a_s, dp)

            issue_load(0)
            pending = None
            for idx, (ei, si) in enumerate(work):
                ge = experts[ei][3]
                if moe and si == 0:
                    for (b0, nt) in lat.blocks:
                        mm_group(PS[6][:, 0:nt], [(sel8[0:8, ge, :], gateT_g[0][0:8, b0:b0 + nt])], reads=[B_ld, gateT_g[1]], writes=[PC[1]])
                        V(lambda v, b0=b0: v.tensor_copy(out=GBC[:, b0:b0 + nt], in_=PS[6][:, 0:nt]), reads=[PC[1]], writes=[B_gbc])
                first = True
                for seq in seqs:
                    for bi in range(len(seq.blocks)):
                        a_s = s1_begin()
                        for q4 in range(4):
                            s1_fc(idx, seq, bi, a_s, q4)
                            if pending is not None:
                                s2_dp(*pending, q4)
                        pending = (idx, seq, bi, a_s)
                        if first and idx + 1 < len(work):
                            issue_load(idx + 1)
                        first = False
            stage2(*pending)

        gateT_g = [None, None]

        def main_schedule():
          for b in range(2):
              lat.n = b
              cs.n = 2
              aa.reset()
              chk("ada")
              for bi, (t0, nt) in enumerate(lat.blocks):
                  cx.dma("sync", XT[:, :, t0:t0 + nt], xT_d[b, :, t0:t0 + nt].rearrange("(j p) t -> p j t", p=128), writes=[lat.XB[bi]])
              cx.dma("sync", XC[:], ctxT_d[b].rearrange("(j p) t -> p j t", p=128), writes=[cs.XB[0]])
              for l in range(2):
                  last = (l == 1)
                  load_layer_small(l)
                  norm_phase([cs, lat], l, GM1, 0)
                  if b == 0 and l == 0:
                      debug_dump("h0", HT[:, :, 0:512], lat.HB[0])
                  chk("norm1")
                  both = [lat] if last else [cs, lat]
                  mixer_attention(l, both, do_ctx_q=not last)
                  chk("attn")
                  if b == 0 and l == 0:
                      cx.barrier()
                      debug_dump("x_att", XT[:, :, 0:512], lat.XB[0])
                  mixer_spatial(l, both)
                  chk("spatial")
                  if b == 0 and l == 0:
                      cx.barrier()
                      debug_dump("x_sp", XT[:, :, 0:512], lat.XB[0])
                  mixer_pool(l, both)
                  chk("pool")
                  if b == 0 and l == 0:
                      cx.barrier()
                      debug_dump("x_pool", XT[:, :, 0:512], lat.XB[0])
                  mixer_fourier(l, both)
                  chk("fourier")
                  if b == 0 and l == 0:
                      cx.barrier()
                      debug_dump("x_mix", XT[:, :, 0:512], lat.XB[0])
                      debug_dump("xc_mix", XC[:], cs.XB[0])
                  if not last:
                      norm_phase([cs, lat], l, GM2, 3)
                      aa.reset()
                      ffn_phase(l, [cs, lat], [(fg_d[0], fu_d[0], fd_d[0], None)])
                      chk("ffn0")
                      if b == 0:
                          cx.barrier()
                          debug_dump("x_l0", XT[:, :, 0:512], lat.XB[0])
                          debug_dump("xc_l0", XC[:], cs.XB[0])
                  else:
                      gateT = XC[:].rearrange("p j t -> p (j t)")
                      B_gateT = cs.XB[0]
                      gateT_g[0], gateT_g[1] = gateT, B_gateT
                      norm_phase([lat], l, GM2, 3, router=True, gateT=gateT, B_gateT=B_gateT)
                      if b == 0:
                          debug_dump("gateT", gateT[0:8, 0:512], B_gateT)
                      aa.reset()
                      ffn_phase(l, [lat], [(mg_d[0, e], mu_d[0, e], md_d[0, e], e) for e in range(NE)])
              cx.barrier()
              for bi, (t0, nt) in enumerate(lat.blocks):
                  cx.dma("sync", outT_d[b, :, t0:t0 + nt].rearrange("(j p) t -> p j t", p=128), XT[:, :, t0:t0 + nt], reads=[lat.XB[bi]])

        try:
            main_schedule()
        except _Stop:
            pass
        cx.finish()
        print("instructions (tracked ops):", cx.ninst, "semaphores:", cx.nsem)
    return nc


_CACHE = {}


def _pmajor(v):
    return np.ascontiguousarray(v.reshape(*v.shape[:-1], 8, 128).swapaxes(-1, -2))


def prepare_inputs(inp):
    f = lambda a: np.ascontiguousarray(np.asarray(a, dtype=np.float32))
    L = 2
    shared = dict(make_consts())
    shared["ada_w"] = f(inp["ada_w"])
    shared["ada_b"] = np.ascontiguousarray(f(inp["ada_b"]).reshape(L, 48, 128).transpose(0, 2, 1))
    shared["norm1_g"] = _pmajor(f(inp["norm1_g"]))
    shared["norm2_g"] = _pmajor(f(inp["norm2_g"]))
    shared["w_in"] = f(inp["w_in"])
    bc = lambda v: np.ascontiguousarray(np.broadcast_to(v[:, None, :], (L, 128, v.shape[-1])))
    shared["gmlp_g"] = bc(f(inp["gmlp_norm_g"]))
    shared["spatial_wT"] = np.ascontiguousarray(f(inp["spatial_w"]).transpose(0, 3, 1, 2))
    shared["spatial_bT"] = np.ascontiguousarray(f(inp["spatial_b"]).transpose(0, 2, 1))
    pw = f(inp["pool_w"])
    bd = np.zeros((L, 128, 2, 128), np.float32)
    for c in range(2):
        for h in range(2):
            bd[:, h * 64:(h + 1) * 64, c, h * 64:(h + 1) * 64] = pw[:, 2 * c + h]
    shared["bdwp"] = bd
    shared["pool_scale"] = np.ascontiguousarray(f(inp["pool_scale"]).reshape(L, 2, 128).transpose(0, 2, 1))
    shared["q_norm_g"] = bc(f(inp["q_norm_g"]))
    shared["kv_norm_g"] = bc(f(inp["kv_norm_g"]))
    shared["qk_q_g"] = bc(np.tile(f(inp["qk_q_g"]), (1, 4)))
    shared["qk_k_g"] = bc(np.tile(f(inp["qk_k_g"]), (1, 4)))
    shared["w_uq"] = f(inp["w_uq"])
    shared["w_ukv"] = f(inp["w_ukv"])
    shared["fourier_w"] = f(inp["fourier_w"])
    shared["w_out"] = f(inp["w_out"])
    shared["ffn_w_gate"] = f(inp["ffn_w_gate"])
    shared["ffn_w_up"] = f(inp["ffn_w_up"])
    shared["ffn_w_down"] = f(inp["ffn_w_down"])
    shared["router_w"] = np.ascontiguousarray(f(inp["router_w"])[0].reshape(8, 128, 8).transpose(1, 0, 2))
    shared["moe_w_gate"] = f(inp["moe_w_gate"])
    shared["moe_w_up"] = f(inp["moe_w_up"])
    shared["moe_w_down"] = f(inp["moe_w_down"])
    x = f(inp["x"])
    ctx = f(inp["ctx"])
    c = f(inp["c"])
    cc = f(inp["c_ctx"])
    maps = []
    for i in range(8):
        m = dict(shared)
        m["xT"] = np.ascontiguousarray(x[2 * i:2 * i + 2].transpose(0, 2, 1))
        m["ctxT"] = np.ascontiguousarray(ctx[2 * i:2 * i + 2].transpose(0, 2, 1))
        c3 = np.stack([c[2 * i], c[2 * i + 1], cc], axis=-1)
        m["cT"] = np.ascontiguousarray(c3.reshape(8, 128, 3).transpose(1, 0, 2))
        maps.append(m)
    return maps


def kernel(**inputs):
    if "nc" not in _CACHE:
        _CACHE["nc"] = build_program()
    nc = _CACHE["nc"]
    maps = prepare_inputs(inputs)
    res = run_bass_kernel_spmd(nc, maps, core_ids=list(range(8)))
    out = np.empty((16, T, D), np.float32)
    for i in range(8):
        o = res.results[i]["outT"]
        out[2 * i:2 * i + 2] = o.transpose(0, 2, 1)
    return out
```

```python
import numpy as np
import ml_dtypes
from contextlib import ExitStack
import concourse.bass as bass
import concourse.mybir as mybir
from concourse.bass_utils import run_bass_kernel_spmd

F32 = mybir.dt.float32
BF16 = mybir.dt.bfloat16
U8 = mybir.dt.uint8
AF = mybir.ActivationFunctionType
ALU = mybir.AluOpType
AX = mybir.AxisListType

D = 1024
T = 2048
TC = 256
NK = T + TC
DFF = 3584
NE = 8
EPS = 1e-6
OFF_A, OFF_B, OFF_CQ, OFF_CKV, OFF_CKR, OFF_D, IN_W = 0, 512, 768, 960, 1088, 1120, 1376
PADW = 8
SEM_LIMIT = 8000


class Buf:
    __slots__ = ("w", "r", "name", "dsem", "dcount")

    def __init__(self, name=""):
        self.w = None
        self.r = {}
        self.name = name
        self.dsem = None
        self.dcount = 0


class Eng:
    def __init__(self, name, h):
        self.name = name
        self.h = h
        self.sem = None
        self.count = 0
        self.waited = {}
        self.own = set()


class Ctx:
    def __init__(self, nc, stack):
        self.nc = nc
        self.stack = stack
        self.nsem = 0
        self.engs = {n: Eng(n, getattr(nc, n)) for n in ("tensor", "vector", "scalar", "gpsimd", "sync")}
        self.dma_tokens = {}
        self.ninst = 0

    def new_sem(self, name):
        self.nsem += 1
        return self.stack.enter_context(self.nc.semaphore(f"s{self.nsem}_{name}"))

    def _wait(self, e, deps):
        need = {}
        for d in deps:
            if d is None:
                continue
            s, v = d
            if e.name == "tensor" and s in e.own:
                continue
            if need.get(s, 0) < v:
                need[s] = v
        for s, v in need.items():
            if e.waited.get(s, 0) < v:
                e.h.wait_ge(s, v)
                e.waited[s] = v

    def _deps(self, reads, writes):
        deps = []
        for b in list(reads) + list(writes):
            if isinstance(b.w, list):
                deps.extend(b.w)
            else:
                deps.append(b.w)
        for b in writes:
            deps.extend(b.r.items())
        return deps

    def _commit(self, tok, reads, writes):
        for b in writes:
            b.w = tok
            b.r = {}
        s, v = tok
        for b in reads:
            if b in writes:
                continue
            if b.r.get(s, 0) < v:
                b.r[s] = v

    def _tok(self, e, ins):
        if e.sem is None or e.count >= SEM_LIMIT:
            e.sem = self.new_sem(e.name)
            e.own.add(e.sem)
            e.count = 0
        e.count += 1
        ins.then_inc(e.sem, 1)
        return (e.sem, e.count)

    def op(self, eng, emit, reads=(), writes=()):
        e = self.engs[eng]
        self._wait(e, self._deps(reads, writes))
        ins = emit(e.h)
        tok = self._tok(e, ins)
        self._commit(tok, reads, writes)
        self.ninst += 1
        return tok

    def dma(self, eng, out, in_, reads=(), writes=()):
        e = self.engs[eng]
        self._wait(e, self._deps(reads, writes))
        owner = writes[0] if writes else reads[0]
        kind = "sw" if eng == "gpsimd" else "hw"
        if owner.dsem is None:
            owner.dsem = {}
        if kind not in owner.dsem:
            owner.dsem[kind] = [self.new_sem("d" + kind + owner.name), 0]
        ent = owner.dsem[kind]
        ent[1] += 16
        e.h.dma_start(out=out, in_=in_).then_inc(ent[0], 16)
        tok = (ent[0], ent[1])
        self._commit(tok, reads, writes)
        if writes:
            others = [(v[0], v[1]) for k, v in owner.dsem.items() if k != kind and v[1] > 0]
            if others:
                owner.w = [tok] + others
        self.dma_tokens[ent[0]] = ent[1]
        return tok

    def barrier(self):
        sp = self.engs["sync"]
        deps = list(self.dma_tokens.items())
        for e in self.engs.values():
            if e.sem is not None:
                deps.append((e.sem, e.count))
        self._wait(sp, deps)
        ins = sp.h.nop()
        tok = self._tok(sp, ins)
        for e in self.engs.values():
            if e is not sp:
                self._wait(e, [tok])

    def finish(self):
        self.barrier()


def _bf(a):
    return np.ascontiguousarray(a.astype(ml_dtypes.bfloat16))


def make_consts():
    c = {}
    c["ident"] = np.eye(128, dtype=np.float32)
    t = np.arange(T)
    row = (t // 64).astype(np.float32)
    col = (t % 64).astype(np.float32)
    inv = (np.float32(10000.0) ** (-np.arange(8, dtype=np.float32) / np.float32(8))).astype(np.float32)
    a0 = row[:, None] * inv[None, :]
    a1 = col[:, None] * inv[None, :]
    cos32 = np.concatenate([np.cos(a0), np.cos(a0), np.cos(a1), np.cos(a1)], axis=1).astype(np.float32)
    sin32 = np.concatenate([-np.sin(a0), np.sin(a0), -np.sin(a1), np.sin(a1)], axis=1).astype(np.float32)
    c["rope_cos"] = np.ascontiguousarray(cos32.reshape(16, 128, 32).transpose(1, 0, 2))
    c["rope_sin"] = np.ascontiguousarray(sin32.reshape(16, 128, 32).transpose(1, 0, 2))
    def dft(L):
        lk = (np.arange(L)[:, None] * np.arange(L)[None, :]) % L
        ang = 2.0 * np.pi * lk / L
        return np.cos(ang) / np.sqrt(L), -np.sin(ang) / np.sqrt(L)
    CL, SLn = dft(T)
    def lay(M):
        return _bf(M.reshape(16, 128, 4, 512).transpose(2, 1, 0, 3))
    c["dft_c"] = lay(CL)
    c["dft_s"] = lay(SLn)
    C2, S2n = dft(TC)
    c["dftc_c"] = _bf(C2.reshape(2, 128, TC).transpose(1, 0, 2))
    c["dftc_s"] = _bf(S2n.reshape(2, 128, TC).transpose(1, 0, 2))
    cm = (np.arange(64)[:, None] * np.arange(64)[None, :]) % 64
    C64 = np.cos(2 * np.pi * cm / 64) / 8.0
    S64 = np.sin(2 * np.pi * cm / 64) / 8.0
    bd = np.zeros((2, 128, 128))
    for h in range(2):
        bd[0, h * 64:(h + 1) * 64, h * 64:(h + 1) * 64] = C64
        bd[1, h * 64:(h + 1) * 64, h * 64:(h + 1) * 64] = S64
    c["bd64"] = _bf(bd.transpose(1, 0, 2))
    E = np.zeros((128, 2, 16), np.float32)
    for cch in range(2):
        for p in range(128):
            w = (2, 4, 8, 16)[2 * cch + p // 64]
            for i in range(8):
                E[p, cch, i] = 1.0 / min(i + w // 2, w)
                tt = 8 - i
                E[p, cch, 8 + i] = 1.0 / min(tt + w // 2, w)
    c["pool_e"] = E
    sel = np.zeros((8, 8, 128), np.float32)
    for e in range(8):
        sel[e, e, :] = 1.0
    c["sel8"] = sel
    return c


class _Stop(Exception):
    pass


def build_program(dbg=None, stop=None):
    nc = bass.Bass("TRN2", target_bir_lowering=False)
    dbg = dbg or {}

    def din(name, shape, dt=F32):
        return nc.dram_tensor(name, list(shape), dt, kind="ExternalInput").ap()

    xT_d = din("xT", [2, D, T])
    ctxT_d = din("ctxT", [2, D, TC])
    cT_d = din("cT", [128, 8, 3])
    ada_w_d = din("ada_w", [2, D, 6 * D])
    ada_b_d = din("ada_b", [2, 128, 48])
    n1g_d = din("norm1_g", [2, 128, 8])
    n2g_d = din("norm2_g", [2, 128, 8])
    w_in_d = din("w_in", [2, D, IN_W])
    gmlp_g_d = din("gmlp_g", [2, 128, 256])
    spw_d = din("spatial_wT", [2, 128, 4, 128])
    spb_d = din("spatial_bT", [2, 128, 4])
    bdwp_d = din("bdwp", [2, 128, 2, 128])
    pscale_d = din("pool_scale", [2, 128, 2])
    qng_d = din("q_norm_g", [2, 128, 192])
    kvng_d = din("kv_norm_g", [2, 128, 128])
    qkq_d = din("qk_q_g", [2, 128, 384])
    qkk_d = din("qk_k_g", [2, 128, 384])
    w_uq_d = din("w_uq", [2, 192, 384])
    w_ukv_d = din("w_ukv", [2, 128, 512])
    wf_d = din("fourier_w", [2, 256, 256])
    w_out_d = din("w_out", [2, D, D])
    fg_d = din("ffn_w_gate", [1, D, DFF])
    fu_d = din("ffn_w_up", [1, D, DFF])
    fd_d = din("ffn_w_down", [1, DFF, D])
    rw_d = din("router_w", [128, 8, 8])
    mg_d = din("moe_w_gate", [1, NE, D, DFF])
    mu_d = din("moe_w_up", [1, NE, D, DFF])
    md_d = din("moe_w_down", [1, NE, DFF, D])
    ident_d = din("ident", [128, 128])
    rcos_d = din("rope_cos", [128, 16, 32])
    rsin_d = din("rope_sin", [128, 16, 32])
    dftc_d = din("dft_c", [4, 128, 16, 512], BF16)
    dfts_d = din("dft_s", [4, 128, 16, 512], BF16)
    dftcc_d = din("dftc_c", [128, 2, TC], BF16)
    dftcs_d = din("dftc_s", [128, 2, TC], BF16)
    bd64_d = din("bd64", [128, 2, 128], BF16)
    poole_d = din("pool_e", [128, 2, 16])
    sel8_d = din("sel8", [8, 8, 128])
    outT_d = nc.dram_tensor("outT", [2, D, T], F32, kind="ExternalOutput").ap()
    dbg_d = {k: nc.dram_tensor("dbg_" + k, list(shp), F32, kind="ExternalOutput").ap() for k, shp in dbg.items()}

    with ExitStack() as st:
        cx = Ctx(nc, st)

        def sb(name, shape, dt):
            return st.enter_context(nc.sbuf_tensor("sb_" + name, list(shape), dt))

        XT = sb("XT", [128, 8, T], F32)
        XC = sb("XC", [128, 8, TC], F32)
        HT = sb("HT", [128, 8, T], BF16)
        HC = sb("HC", [128, 8, TC], BF16)
        ARENA_BYTES = 72 * 1024
        arena = sb("arena", [128, ARENA_BYTES], U8)
        ident = sb("ident", [128, 128], F32)
        identb = sb("identb", [128, 128], BF16)
        onesb = sb("onesb", [128, 128], BF16)
        onesf = sb("onesf", [128, 128], F32)
        epsb = sb("epsb", [128, 1], F32)
        cact = sb("cact", [128, 8, 3], F32)
        MOD = sb("MOD", [128, 2, 6, 8, 3], F32)
        GM1 = sb("GM1", [128, 2, 8, 3], F32)
        GM2 = sb("GM2", [128, 2, 8, 3], F32)
        adab = sb("adab", [128, 2, 48], F32)
        n1g = sb("n1g", [128, 2, 8], F32)
        n2g = sb("n2g", [128, 2, 8], F32)
        rcos = sb("rcos", [128, 16, 32], F32)
        rsin = sb("rsin", [128, 16, 32], F32)
        bd64 = sb("bd64", [128, 2, 128], BF16)
        poole = sb("poole", [128, 2, 16], F32)
        sel8 = sb("sel8", [8, 8, 128], F32)
        rwf = sb("rwf", [128, 8, 8], F32)
        gmlp_g = sb("gmlp_g", [128, 256], F32)
        spw = sb("spw", [128, 4, 128], BF16)
        spb = sb("spb", [128, 4], F32)
        bdwp = sb("bdwp", [128, 2, 128], BF16)
        pscale = sb("pscale", [128, 2], F32)
        qng = sb("qng", [128, 192], F32)
        kvng = sb("kvng", [128, 128], F32)
        qkq = sb("qkq", [128, 384], F32)
        qkk = sb("qkk", [128, 384], F32)
        wuq = sb("wuq", [128, 2, 384], BF16)
        wukv = sb("wukv", [128, 512], BF16)
        wfb = sb("wfb", [128, 2, 256], BF16)
        WCS = sb("WCS", [128, 2, 2, 256], BF16)
        small = sb("small", [128, 64], F32)
        small2 = [sb("small_a", [128, 32], F32), sb("small_b", [128, 32], F32)]

        PS = [st.enter_context(nc.psum_tensor(f"ps{i}", [128, 512], F32)) for i in range(8)]
        PB = [Buf(f"ps{i}") for i in range(8)]

        class AA:
            def __init__(self):
                self.off = 0

            def reset(self):
                cx.barrier()
                self.off = 0

            def get(self, shape, dt):
                esz = 4 if dt == F32 else 2
                n = int(np.prod(shape[1:]))
                nbytes = n * esz
                self.off = (self.off + 31) // 32 * 32
                assert self.off + nbytes <= ARENA_BYTES, (self.off, nbytes)
                ap = arena[:, self.off:self.off + nbytes]
                if dt != U8:
                    ap = ap.bitcast(dt)
                self.off += nbytes
                if len(shape) == 2:
                    return ap
                names = " ".join(f"d{i}" for i in range(len(shape) - 1))
                kw = {f"d{i}": int(shape[i + 1]) for i in range(len(shape) - 2)}
                return ap.rearrange(f"p ({names}) -> p {names}", **kw)

        aa = AA()

        def chk(name):
            if stop == name:
                raise _Stop()
        B_const = Buf("const")
        B_mod = Buf("mod")
        B_lw = Buf("lw")
        B_small = Buf("small")

        def V(fn, reads=(), writes=()):
            return cx.op("vector", fn, reads, writes)

        def A(fn, reads=(), writes=()):
            return cx.op("scalar", fn, reads, writes)

        def G(fn, reads=(), writes=()):
            return cx.op("gpsimd", fn, reads, writes)

        def PE(fn, reads=(), writes=()):
            return cx.op("tensor", fn, reads, writes)

        def mm_group(out_ap, pairs, reads, writes):
            def emit(pe):
                n = len(pairs)
                last = None
                for i, (l, r) in enumerate(pairs):
                    last = pe.matmul(out_ap, l, r, start=(i == 0), stop=(i == n - 1))
                return last
            return PE(emit, reads, writes)

        def debug_dump(name, ap, buf):
            if name in dbg_d:
                cx.dma("gpsimd", dbg_d[name], ap, reads=[buf])

        V(lambda v: v.memset(onesb[:], 1.0), writes=[B_const])
        V(lambda v: v.memset(onesf[:], 1.0), writes=[B_const])
        V(lambda v: v.memset(epsb[:], EPS), writes=[B_const])
        B_ld = Buf("cload")
        for dst, src in ((ident[:], ident_d), (cact[:], cT_d), (rcos[:], rcos_d), (rsin[:], rsin_d),
                         (poole[:], poole_d), (sel8[:], sel8_d), (rwf[:], rw_d), (bd64[:], bd64_d),
                         (adab[:], ada_b_d.rearrange("l p j -> p l j")),
                         (n1g[:], n1g_d.rearrange("l p j -> p l j")), (n2g[:], n2g_d.rearrange("l p j -> p l j"))):
            cx.dma("sync", dst, src, writes=[B_ld])
        cx.dma("gpsimd", identb[:], ident_d, writes=[B_ld])
        A(lambda a: a.activation(out=cact[:], in_=cact[:], func=AF.Silu), reads=[B_ld], writes=[B_ld])

        aa.reset()
        AW = [aa.get([128, 8, 1024], F32) for _ in range(2)]
        AWB = [Buf("aw0"), Buf("aw1")]
        k = 0
        for l in range(2):
            for which in range(6):
                s = k % 2
                k += 1
                cx.dma("sync", AW[s], ada_w_d[l, :, which * 1024:(which + 1) * 1024].rearrange("(j p) n -> p j n", p=128),
                       writes=[AWB[s]])
                pb = PB[s]
                ps = PS[s]

                def emit(pe, s=s, ps=ps):
                    last = None
                    for j in range(8):
                        for kk in range(8):
                            last = pe.matmul(ps[:, j * 3:(j + 1) * 3], AW[s][:, kk, j * 128:(j + 1) * 128], cact[:, kk, :],
                                             start=(kk == 0), stop=(kk == 7))
                    return last
                PE(emit, reads=[AWB[s], B_ld], writes=[pb])
                V(lambda v, l=l, which=which, ps=ps: v.tensor_tensor(
                    out=MOD[:, l, which], in0=ps[:, 0:24].rearrange("p (j n) -> p j n", n=3),
                    in1=adab[:, l, which * 8:(which + 1) * 8].unsqueeze(2).to_broadcast([128, 8, 3]), op=ALU.add),
                  reads=[pb, B_ld], writes=[B_mod])
        for l in range(2):
            for (GM, ng, which) in ((GM1, n1g, 1), (GM2, n2g, 4)):
                V(lambda v, GM=GM, l=l, which=which: v.tensor_scalar(out=GM[:, l], in0=MOD[:, l, which], scalar1=1.0, scalar2=None, op0=ALU.add),
                  reads=[B_mod], writes=[B_mod])
                V(lambda v, GM=GM, l=l, ng=ng: v.tensor_tensor(out=GM[:, l], in0=GM[:, l], in1=ng[:, l].unsqueeze(2).to_broadcast([128, 8, 3]), op=ALU.mult),
                  reads=[B_mod, B_ld], writes=[B_mod])

        class Seq:
            pass

        lat = Seq()
        lat.name = "lat"; lat.T = T; lat.X = XT; lat.H = HT; lat.blocks = [(i * 512, 512) for i in range(4)]
        lat.XB = [Buf(f"xb{i}") for i in range(4)]; lat.HB = [Buf(f"hb{i}") for i in range(4)]
        lat.ntile = 16; lat.tok0 = TC; lat.rope = True
        cs = Seq()
        cs.name = "ctx"; cs.T = TC; cs.X = XC; cs.H = HC; cs.blocks = [(0, TC)]
        cs.XB = [Buf("xcb")]; cs.HB = [Buf("hcb")]
        cs.ntile = 2; cs.tok0 = 0; cs.rope = False

        def blk_of(seq, t0):
            return t0 // 512

        def norm_phase(seqs, l, GM, shift_which, router=False, gateT=None, B_gateT=None):
            aa.reset()
            SQ2 = [aa.get([128, 8, 512], BF16) for _ in range(2)]
            RS2 = [aa.get([128, 512], F32) for _ in range(2)]
            NTMP = 8
            TMP = [aa.get([128, 512], F32) for _ in range(NTMP)]
            B_sq2, B_rs2 = [Buf("sq0"), Buf("sq1")], [Buf("rs0"), Buf("rs1")]
            nblk = 0
            B_tmp = [Buf(f"tmp{i}") for i in range(NTMP)]
            if router:
                H32 = aa.get([128, 8, 512], F32)
                B_h32 = Buf("h32")
                LG = aa.get([128, 16], F32)
                B_lg = Buf("lg")
            it = 0
            allblk = [(seq, bi, t0, nt) for seq in seqs for bi, (t0, nt) in enumerate(seq.blocks)]

            def squares(k):
                seq, bi, t0, nt = allblk[k]
                SQ, B_sq = SQ2[k % 2], B_sq2[k % 2]
                for j in range(8):
                    if j % 2 == 1 and not router:
                        G(lambda g, j=j: g.tensor_tensor(out=SQ[:, j, 0:nt], in0=seq.X[:, j, t0:t0 + nt], in1=seq.X[:, j, t0:t0 + nt], op=ALU.mult),
                          reads=[seq.XB[bi]], writes=[B_sq])
                    else:
                        A(lambda a, j=j: a.activation(out=SQ[:, j, 0:nt], in_=seq.X[:, j, t0:t0 + nt], func=AF.Square),
                          reads=[seq.XB[bi]], writes=[B_sq])
                mm_group(PS[k % 2][:, 0:nt], [(onesb[:], SQ[:, j, 0:nt]) for j in range(8)], reads=[B_sq, B_const], writes=[PB[k % 2]])

            squares(0)
            for k, (seq, bi, t0, nt) in enumerate(allblk):
                    n = seq.n
                    xb, hb = seq.XB[bi], seq.HB[bi]
                    RS, B_rs = RS2[k % 2], B_rs2[k % 2]
                    pbn = k % 2
                    if k + 1 < len(allblk):
                        squares(k + 1)
                    A(lambda a: a.activation(out=RS[:, 0:nt], in_=PS[pbn][:, 0:nt], func=AF.Sqrt, scale=1.0 / D, bias=epsb[:, 0:1]),
                      reads=[PB[pbn], B_const], writes=[B_rs])
                    V(lambda v: v.reciprocal(out=RS[:, 0:nt], in_=RS[:, 0:nt]), reads=[B_rs], writes=[B_rs])
                    for j in range(8):
                        s = it % NTMP
                        it += 1
                        V(lambda v, j=j, s=s: v.tensor_tensor(out=TMP[s][:, 0:nt], in0=seq.X[:, j, t0:t0 + nt], in1=RS[:, 0:nt], op=ALU.mult),
                          reads=[xb, B_rs], writes=[B_tmp[s]])
                        if router:
                            A(lambda a, j=j, s=s: a.activation(out=H32[:, j, 0:nt], in_=TMP[s][:, 0:nt], func=AF.Identity,
                                                               scale=GM[:, l, j, n:n + 1], bias=MOD[:, l, shift_which, j, n:n + 1]),
                              reads=[B_tmp[s], B_mod], writes=[B_h32])
                            G(lambda g, j=j: g.tensor_copy(out=seq.H[:, j, t0:t0 + nt], in_=H32[:, j, 0:nt]), reads=[B_h32], writes=[hb])
                        else:
                            A(lambda a, j=j, s=s: a.activation(out=seq.H[:, j, t0:t0 + nt], in_=TMP[s][:, 0:nt], func=AF.Identity,
                                                               scale=GM[:, l, j, n:n + 1], bias=MOD[:, l, shift_which, j, n:n + 1]),
                              reads=[B_tmp[s], B_mod], writes=[hb])
                    if router:
                        for ti in range(nt // 128):
                            c0 = ti * 128
                            mm_group(PS[3][:, 0:8], [(H32[:, j, c0:c0 + 128], rwf[:, j, :]) for j in range(8)],
                                     reads=[B_h32, B_ld], writes=[PB[3]])
                            lg = LG[:, 0:8]
                            w2 = LG[:, 8:16]
                            sm = small
                            V(lambda v: v.tensor_copy(out=lg, in_=PS[3][:, 0:8]), reads=[PB[3]], writes=[B_lg])
                            V(lambda v: v.reduce_max(out=sm[:, 0:1], in_=lg, axis=AX.X), reads=[B_lg], writes=[B_small])
                            V(lambda v: v.tensor_scalar(out=w2, in0=lg, scalar1=sm[:, 0:1], scalar2=-1e30, op0=ALU.is_equal, op1=ALU.mult),
                              reads=[B_lg, B_small], writes=[B_lg])
                            V(lambda v: v.tensor_tensor(out=w2, in0=w2, in1=lg, op=ALU.add), reads=[B_lg], writes=[B_lg])
                            V(lambda v: v.reduce_max(out=sm[:, 1:2], in_=w2, axis=AX.X), reads=[B_lg], writes=[B_small])
                            V(lambda v: v.tensor_scalar(out=w2, in0=lg, scalar1=sm[:, 1:2], scalar2=None, op0=ALU.is_ge),
                              reads=[B_lg, B_small], writes=[B_lg])
                            V(lambda v: v.tensor_scalar(out=sm[:, 2:3], in0=sm[:, 0:1], scalar1=-1.0, scalar2=None, op0=ALU.mult),
                              reads=[B_small], writes=[B_small])
                            A(lambda a: a.activation(out=lg, in_=lg, func=AF.Exp, bias=sm[:, 2:3], scale=1.0), reads=[B_lg, B_small], writes=[B_lg])
                            V(lambda v: v.tensor_tensor(out=lg, in0=lg, in1=w2, op=ALU.mult), reads=[B_lg], writes=[B_lg])
                            V(lambda v: v.reduce_sum(out=sm[:, 3:4], in_=lg, axis=AX.X), reads=[B_lg], writes=[B_small])
                            V(lambda v: v.reciprocal(out=sm[:, 3:4], in_=sm[:, 3:4]), reads=[B_small], writes=[B_small])
                            V(lambda v: v.tensor_scalar(out=lg, in0=lg, scalar1=sm[:, 3:4], scalar2=None, op0=ALU.mult),
                              reads=[B_lg, B_small], writes=[B_lg])
                            PE(lambda pe: pe.transpose(PS[2][0:8, 0:128], lg, ident[:]), reads=[B_lg, B_ld], writes=[PB[2]])
                            V(lambda v, c0=c0: v.tensor_copy(out=gateT[0:8, t0 + c0:t0 + c0 + 128], in_=PS[2][0:8, 0:128]),
                              reads=[PB[2]], writes=[B_gateT])

        def wout_update(seq, l, t0, nt, YT, B_y, WO, B_wo, bank0):
            xb = seq.XB[blk_of(seq, t0)]
            n = seq.n
            for dj in range(8):
                bk = bank0 + dj % 2
                mm_group(PS[bk][:, 0:nt], [(WO[:, c, dj * 128:(dj + 1) * 128], YT[:, c, 0:nt]) for c in range(2)],
                         reads=[B_y, B_wo], writes=[PB[bk]])
                V(lambda v, dj=dj, bk=bk: v.scalar_tensor_tensor(
                    out=seq.X[:, dj, t0:t0 + nt], in0=PS[bk][:, 0:nt], scalar=MOD[:, l, 2, dj, n:n + 1],
                    in1=seq.X[:, dj, t0:t0 + nt], op0=ALU.mult, op1=ALU.add),
                  reads=[PB[bk], B_mod, xb], writes=[xb])

        def load_wout(l, m, WO, B_wo):
            cx.dma("gpsimd", WO, w_out_d[l, m * 256:(m + 1) * 256, :].rearrange("(c p) n -> p c n", p=128), writes=[B_wo])

        def load_win(l, c0, c1, W, B_w):
            cx.dma("gpsimd", W, w_in_d[l, :, c0:c1].rearrange("(j p) n -> p j n", p=128), writes=[B_w])

        def load_layer_small(l):
            for dst, src, q in ((gmlp_g[:], gmlp_g_d[l], "sync"), (spw[:], spw_d[l], "gpsimd"), (spb[:], spb_d[l], "sync"),
                                (bdwp[:], bdwp_d[l], "gpsimd"), (pscale[:], pscale_d[l], "sync"), (qng[:], qng_d[l], "sync"),
                                (kvng[:], kvng_d[l], "sync"), (qkq[:], qkq_d[l], "sync"), (qkk[:], qkk_d[l], "sync"),
                                (wuq[:, 0, :], w_uq_d[l, 0:128, :], "gpsimd"), (wuq[0:64, 1, :], w_uq_d[l, 128:192, :], "gpsimd"),
                                (wukv[:], w_ukv_d[l], "gpsimd"),
                                (wfb[:], wf_d[l].rearrange("(c p) n -> p c n", p=128), "gpsimd")):
                cx.dma(q, dst, src, writes=[B_lw])
            for csi in range(2):
                for c in range(2):
                    mm_group(PS[0][:, 0:256], [(bd64[:, csi, :], wfb[:, c, :])], reads=[B_ld, B_lw], writes=[PB[0]])
                    V(lambda v, csi=csi, c=c: v.tensor_copy(out=WCS[:, csi, c, :], in_=PS[0][:, 0:256]), reads=[PB[0]], writes=[B_lw])

        def mixer_attention(l, seqs_q, do_ctx_q):
            aa.reset()
            WINC = aa.get([128, 8, 352], BF16); B_w = Buf("winc")
            WO = aa.get([128, 2, 1024], BF16); B_wo = Buf("woc")
            KT = aa.get([128, 4, NK], BF16); B_kt = Buf("kt")
            VP = aa.get([128, 18, 2, 192], BF16); B_vp = Buf("vp")

            class _S:
                pass

            def mk(i):
                o = _S()
                o.TM = aa.get([128, 352], F32); o.B_tm = Buf(f"tm{i}")
                o.CN = aa.get([128, 320], BF16); o.B_cn = Buf(f"cn{i}")
                o.CNT = aa.get([128, 3, 128], BF16); o.B_cnt = Buf(f"cnt{i}")
                o.QF = aa.get([128, 4, 96], F32); o.B_qf = Buf(f"qf{i}")
                o.KF = o.QF; o.B_kf = o.B_qf
                o.QB = aa.get([128, 4, 96], BF16); o.B_qb = Buf(f"qb{i}")
                o.KB = o.QB; o.B_kb = o.B_qb
                if i < 2:
                    o.sm = small2[i]
                else:
                    o.sm = aa.get([128, 32], F32)
                o.B_small = Buf(f"small{i}")
                o.pb = i
                o.ub = 4 + i
                return o
            SS = [mk(0), mk(1)]
            R1 = aa.get([128, 4, 32], F32); B_r1 = Buf("r1")
            SQS = aa.get([128, 384], F32); B_sqs = Buf("sqs")
            SQA = aa.get([128, 192], BF16); B_sqa = Buf("sqa")
            off_q = aa.off
            SS += [mk(2), mk(3)]
            end_extra = aa.off
            aa.off = off_q
            QT2 = [aa.get([128, 4, 512], BF16) for _ in range(2)]; B_qt2 = [Buf("qt0"), Buf("qt1")]
            PT = [aa.get([128, 512], BF16) for _ in range(3)]; B_pt = [Buf(f"pt{i}") for i in range(3)]
            RD = aa.get([128, 512], F32); B_rd = Buf("rd")
            BC = aa.get([128, 512], F32); B_bc = Buf("bc")
            AT = aa.get([128, 2, 512], BF16); B_at = Buf("at")
            aa.off = max(aa.off, end_extra)
            for o in SS:
                o.R1 = R1; o.B_r1 = B_r1; o.SQS = SQS; o.B_sqs = B_sqs
            load_win(l, OFF_CQ, OFF_D, WINC, B_w)
            load_wout(l, 2, WO, B_wo)
            V(lambda v: v.memset(VP.rearrange("p a b c -> p (a b c)"), 0.0), writes=[B_vp])
            for pr_ in range(2):
                V(lambda v, pr_=pr_: v.memset(VP[:, :, pr_, 64:65], 1.0), writes=[B_vp])

            def rms_scale(S, src_ap, width, col):
                A(lambda a: a.activation(out=SQA[:, 0:width], in_=src_ap, func=AF.Square, accum_out=S.sm[:, col:col + 1]),
                  reads=[S.B_tm], writes=[B_sqa, S.B_small])
                A(lambda a: a.activation(out=S.sm[:, col:col + 1], in_=S.sm[:, col:col + 1], func=AF.Ln, scale=1.0 / width, bias=epsb[:, 0:1]),
                  reads=[S.B_small, B_const], writes=[S.B_small])
                A(lambda a: a.activation(out=S.sm[:, col:col + 1], in_=S.sm[:, col:col + 1], func=AF.Exp, scale=-0.5),
                  reads=[S.B_small], writes=[S.B_small])

            def head_norm(S, src, B_src, gains, dst, B_dst, colbase):
                V(lambda v: v.tensor_tensor(out=S.SQS[:].rearrange("p (h d) -> p h d", h=4), in0=src, in1=src, op=ALU.mult),
                  reads=[B_src], writes=[S.B_sqs])
                V(lambda v: v.reduce_sum(out=S.sm[:, colbase:colbase + 4], in_=S.SQS[:].rearrange("p (h d) -> p h d", h=4), axis=AX.X),
                  reads=[S.B_sqs], writes=[S.B_small])
                yield
                A(lambda a: a.activation(out=S.sm[:, colbase:colbase + 4], in_=S.sm[:, colbase:colbase + 4], func=AF.Ln, scale=1.0 / 96, bias=epsb[:, 0:1]),
                  reads=[S.B_small, B_const], writes=[S.B_small])
                A(lambda a: a.activation(out=S.sm[:, colbase:colbase + 4], in_=S.sm[:, colbase:colbase + 4], func=AF.Exp, scale=-0.5),
                  reads=[S.B_small], writes=[S.B_small])
                yield
                V(lambda v: v.tensor_tensor(out=dst, in0=src, in1=S.sm[:, colbase:colbase + 4].unsqueeze(2).to_broadcast([128, 4, 96]), op=ALU.mult),
                  reads=[B_src, S.B_small], writes=[B_dst])
                V(lambda v: v.tensor_tensor(out=dst, in0=dst, in1=gains.rearrange("p (h d) -> p h d", h=4), op=ALU.mult),
                  reads=[B_dst, B_lw], writes=[B_dst])

            def rope(S, src, B_src, ti):
                xr = src[:, :, 64:96]
                cosb = rcos[:, ti, :].unsqueeze(1).to_broadcast([128, 4, 32])
                x5 = xr.rearrange("p h (a b f) -> p h a b f", a=2, b=2)
                r5 = S.R1[:].rearrange("p h (a b f) -> p h a b f", a=2, b=2)
                s5 = rsin[:, ti, :].rearrange("p (a b f) -> p a b f", a=2, b=2)
                for bsel in range(2):
                    V(lambda v, bsel=bsel: v.tensor_tensor(
                        out=r5[:, :, :, bsel, :], in0=x5[:, :, :, 1 - bsel, :],
                        in1=s5[:, :, bsel, :].unsqueeze(1).to_broadcast([128, 4, 2, 8]), op=ALU.mult),
                      reads=[B_src, B_ld], writes=[S.B_r1])
                V(lambda v: v.tensor_tensor(out=xr, in0=xr, in1=cosb, op=ALU.mult), reads=[B_src, B_ld], writes=[B_src])
                V(lambda v: v.tensor_tensor(out=xr, in0=xr, in1=S.R1[:], op=ALU.add), reads=[B_src, S.B_r1], writes=[B_src])

            def run_window(jobs, sets, make):
                jobs = list(jobs)
                free = list(sets)
                active = []
                while jobs or active:
                    while jobs and free:
                        si = free.pop(0)
                        active.append((make(jobs.pop(0), si), si))
                    for item in list(active):
                        g, si = item
                        try:
                            next(g)
                        except StopIteration:
                            active.remove(item)
                            free.append(si)
                        yield

            def kv_tile(seq, ti, si):
                S = SS[si]
                P, PBp = PS[S.pb], PB[S.pb]
                U, PBu = PS[S.ub], PB[S.ub]
                pbt = P[:].bitcast(BF16)
                t0 = ti * 128
                g0 = seq.tok0 + t0
                kt_i = g0 // 128
                hb = seq.HB[blk_of(seq, t0)]
                mm_group(P[:, 192:352], [(seq.H[:, j, t0:t0 + 128], WINC[:, j, 192:352]) for j in range(8)], reads=[hb, B_w], writes=[PBp])
                yield
                A(lambda a: a.copy(out=S.TM[:, 192:352], in_=P[:, 192:352]), reads=[PBp], writes=[S.B_tm])
                rms_scale(S, S.TM[:, 192:320], 128, 1)
                yield
                V(lambda v: v.scalar_tensor_tensor(out=S.CN[:, 192:320], in0=S.TM[:, 192:320], scalar=S.sm[:, 1:2], in1=kvng[:], op0=ALU.mult, op1=ALU.mult),
                  reads=[S.B_tm, S.B_small, B_lw], writes=[S.B_cn])
                yield
                PE(lambda pe: pe.transpose(pbt[:, 0:128], S.CN[:, 192:320], identb[:]), reads=[S.B_cn, B_ld], writes=[PBp])
                yield
                V(lambda v: v.tensor_copy(out=S.CNT[:, 2, :], in_=pbt[:, 0:128]), reads=[PBp], writes=[S.B_cnt])
                yield
                mm_group(U[:, 0:512], [(S.CNT[:, 2, :], wukv[:])], reads=[S.B_cnt, B_lw], writes=[PBu])
                yield
                kv4 = U[:, 0:512].rearrange("p (h d) -> p h d", h=4)
                for e in range(2):
                    A(lambda a, e=e: a.copy(out=VP[:, kt_i, :, e * 128:e * 128 + 64], in_=kv4[:, e:4:2, 64:128]),
                      reads=[PBu], writes=[B_vp])
                A(lambda a: a.copy(out=S.KF[:, :, 0:64], in_=kv4[:, :, 0:64]), reads=[PBu], writes=[S.B_kf])
                V(lambda v: v.tensor_copy(out=S.KF[:, :, 64:96], in_=S.TM[:, 320:352].unsqueeze(1).to_broadcast([128, 4, 32])),
                  reads=[S.B_tm], writes=[S.B_kf])
                yield
                for _ in head_norm(S, S.KF[:], S.B_kf, qkk[:], S.KF[:], S.B_kf, 8):
                    yield
                if seq.rope:
                    rope(S, S.KF[:], S.B_kf, ti)
                V(lambda v: v.tensor_copy(out=S.KB[:], in_=S.KF[:]), reads=[S.B_kf], writes=[S.B_kb])
                yield

                def trk(pe):
                    last = None
                    for h in range(4):
                        last = pe.transpose(pbt[0:96, h * 128:(h + 1) * 128], S.KB[:, h, :], identb[:])
                    return last
                PE(trk, reads=[S.B_kb, B_ld], writes=[PBp])
                yield
                V(lambda v: v.tensor_copy(out=KT[0:96, :, g0:g0 + 128], in_=pbt[0:96, 0:512].rearrange("p (h t) -> p h t", h=4)),
                  reads=[PBp], writes=[B_kt])

            def q_tile(seq, ti, qc, qs, si):
                S = SS[si]
                P, PBp = PS[S.pb], PB[S.pb]
                pbt = P[:].bitcast(BF16)
                t0 = ti * 128
                hb = seq.HB[blk_of(seq, t0)]
                QT = QT2[qs]; B_qt = B_qt2[qs]
                mm_group(P[:, 0:192], [(seq.H[:, j, t0:t0 + 128], WINC[:, j, 0:192]) for j in range(8)], reads=[hb, B_w], writes=[PBp])
                yield
                A(lambda a: a.copy(out=S.TM[:, 0:192], in_=P[:, 0:192]), reads=[PBp], writes=[S.B_tm])
                rms_scale(S, S.TM[:, 0:192], 192, 0)
                yield
                V(lambda v: v.scalar_tensor_tensor(out=S.CN[:, 0:192], in0=S.TM[:, 0:192], scalar=S.sm[:, 0:1], in1=qng[:], op0=ALU.mult, op1=ALU.mult),
                  reads=[S.B_tm, S.B_small, B_lw], writes=[S.B_cn])
                yield
                yield

                def tr(pe):
                    pe.transpose(pbt[:, 0:128], S.CN[:, 0:128], identb[:])
                    return pe.transpose(pbt[0:64, 128:256], S.CN[:, 128:192], identb[:])
                PE(tr, reads=[S.B_cn, B_ld], writes=[PBp])
                yield
                V(lambda v: v.tensor_copy(out=S.CNT[:, 0, :], in_=pbt[:, 0:128]), reads=[PBp], writes=[S.B_cnt])
                V(lambda v: v.tensor_copy(out=S.CNT[0:64, 1, :], in_=pbt[0:64, 128:256]), reads=[PBp], writes=[S.B_cnt])
                yield
                yield
                mm_group(P[:, 128:512], [(S.CNT[:, 0, :], wuq[:, 0, :]), (S.CNT[0:64, 1, :], wuq[0:64, 1, :])],
                         reads=[S.B_cnt, B_lw], writes=[PBp])
                yield
                A(lambda a: a.copy(out=S.QF[:].rearrange("p h d -> p (h d)"), in_=P[:, 128:512]), reads=[PBp], writes=[S.B_qf])
                yield
                for _ in head_norm(S, S.QF[:], S.B_qf, qkq[:], S.QF[:], S.B_qf, 12):
                    yield
                if seq.rope:
                    rope(S, S.QF[:], S.B_qf, ti)
                V(lambda v: v.tensor_copy(out=S.QB[:], in_=S.QF[:]), reads=[S.B_qf], writes=[S.B_qb])
                yield
                yield
                yield

                def trq(pe):
                    last = None
                    for h in range(4):
                        last = pe.transpose(pbt[0:96, h * 128:(h + 1) * 128], S.QB[:, h, :], identb[:])
                    return last
                PE(trq, reads=[S.B_qb, B_ld], writes=[PBp])
                yield
                V(lambda v: v.tensor_copy(out=QT[0:96, :, qc:qc + 128], in_=pbt[0:96, 0:512].rearrange("p (h t) -> p h t", h=4)),
                  reads=[PBp], writes=[B_qt])

            kv_jobs = [(seq, ti) for seq in (cs, lat) for ti in range(seq.ntile)]
            for _ in run_window(kv_jobs, [0, 1, 2, 3], lambda job, si: kv_tile(job[0], job[1], si)):
                pass
            chk("attn_kv")
            cx.barrier()
            SS[1].pb = 3

            scale = 96 ** -0.5
            it = [0]
            blks = [(seq, t0, nt) for seq in seqs_q for (t0, nt) in seq.blocks]

            def q_jobs(bi):
                seq, t0, nt = blks[bi]
                jobs = [(seq, t0 // 128 + tq, tq * 128, bi % 2) for tq in range(nt // 128)]
                return run_window(jobs, [0, 1], lambda job, si: q_tile(job[0], job[1], job[2], job[3], si))

            for _ in q_jobs(0):
                pass
            for bi, (seq, t0, nt) in enumerate(blks):
                    nkt = 18 if seq is lat else 2
                    QT = QT2[bi % 2]; B_qt = B_qt2[bi % 2]
                    nxt = q_jobs(bi + 1) if bi + 1 < len(blks) else iter(())
                    items = [(h, kt_i) for h in range(4) for kt_i in range(nkt)]
                    slot_of = {}

                    def do_S(i):
                        h, kt_i = items[i]
                        s = it[0] % 3
                        sbk = 1 + it[0] % 2
                        it[0] += 1
                        slot_of[i] = s
                        mm_group(PS[sbk][:, 0:nt], [(KT[0:96, h, kt_i * 128:(kt_i + 1) * 128], QT[0:96, h, 0:nt])],
                                 reads=[B_kt, B_qt], writes=[PB[sbk]])
                        A(lambda a: a.activation(out=PT[s][:, 0:nt], in_=PS[sbk][:, 0:nt], func=AF.Exp, scale=scale),
                          reads=[PB[sbk]], writes=[B_pt[s]])

                    def do_PV(i):
                        h, kt_i = items[i]
                        s = slot_of[i]
                        pr, odd = h // 2, h % 2
                        accb = 4 + h % 2
                        acc = PS[accb]
                        lhs = VP[:, kt_i, pr, 64:192] if odd else VP[:, kt_i, pr, 0:65]
                        M = 128 if odd else 65
                        PE(lambda pe: pe.matmul(acc[0:M, 0:nt], lhs, PT[s][:, 0:nt], start=(kt_i == 0), stop=(kt_i == nkt - 1)),
                           reads=[B_vp, B_pt[s]], writes=[PB[accb]])
                        if kt_i == nkt - 1:
                            dr = 0 if odd else 64
                            V(lambda v: v.reciprocal(out=RD[dr:dr + 1, 0:nt], in_=acc[dr:dr + 1, 0:nt]), reads=[PB[accb]], writes=[B_rd])

                    def do_norm(h, stage):
                        pr, odd = h // 2, h % 2
                        accb = 4 + h % 2
                        acc = PS[accb]
                        dr = 0 if odd else 64
                        if stage == 0:
                            mm_group(PS[6][:, 0:nt], [(onesf[dr:dr + 1, :], RD[dr:dr + 1, 0:nt])], reads=[B_rd, B_const], writes=[PB[6]])
                        elif stage == 1:
                            A(lambda a: a.copy(out=BC[:, 0:nt], in_=PS[6][:, 0:nt]), reads=[PB[6]], writes=[B_bc])
                        else:
                            r0 = 64 if odd else 0
                            V(lambda v: v.tensor_tensor(out=AT[r0:r0 + 64, pr, 0:nt], in0=acc[r0:r0 + 64, 0:nt], in1=BC[r0:r0 + 64, 0:nt], op=ALU.mult),
                              reads=[PB[accb], B_bc], writes=[B_at])

                    n_it = len(items)
                    LA = 2
                    last_step = n_it + LA - 1
                    offs = (8, 10, 12) if nkt >= 16 else (2, 2, 2)
                    due = {}
                    for i in range(n_it + LA):
                        if i < n_it:
                            do_S(i)
                        j = i - LA
                        if j >= 0:
                            do_PV(j)
                            h, kt_i = items[j]
                            if kt_i == nkt - 1:
                                for stage in range(3):
                                    due.setdefault(min(i + offs[stage], last_step), []).append((h, stage))
                        for (h, stage) in due.pop(i, []):
                            do_norm(h, stage)
                        next(nxt, None)
                    assert not due
                    for _ in nxt:
                        pass
                    wout_update(seq, l, t0, nt, AT, B_at, WO, B_wo, 6)

        def mixer_spatial(l, seqs):
            aa.reset()
            WINA = aa.get([128, 8, 512], BF16); B_w = Buf("wina")
            WO = aa.get([128, 2, 1024], BF16); B_wo = Buf("woa")
            UV = [aa.get([128, 512], F32) for _ in range(4)]; B_uv = [Buf(f"uv{i}") for i in range(4)]
            VN = [aa.get([128, 256], BF16) for _ in range(2)]; B_vn = [Buf(f"vn{i}") for i in range(2)]
            SQS = aa.get([128, 256], F32); B_sqs = Buf("sqsa")
            YTK = [aa.get([128, 256], BF16) for _ in range(2)]; B_ytk = [Buf(f"ytk{i}") for i in range(2)]
            MX = [aa.get([128, 256], F32) for _ in range(2)]; B_mx = [Buf(f"mx{i}") for i in range(2)]
            YA = aa.get([128, 2, 512], BF16); B_ya = Buf("ya")
            load_win(l, OFF_A, OFF_B, WINA, B_w)
            load_wout(l, 0, WO, B_wo)
            sm = small
            B_sm = Buf("sm_sp")
            pbt = PS[7][:].bitcast(BF16)
            for seq in seqs:
                for (b0, nt) in seq.blocks:
                    ntile = nt // 128
                    for ti in range(ntile):
                        t0 = b0 + ti * 128
                        hb = seq.HB[blk_of(seq, t0)]
                        bk = ti % 2
                        mm_group(PS[bk][:, 0:512], [(seq.H[:, j, t0:t0 + 128], WINA[:, j, :]) for j in range(8)], reads=[hb, B_w], writes=[PB[bk]])
                        A(lambda a, ti=ti, bk=bk: a.activation(out=UV[ti][:], in_=PS[bk][:, 0:512], func=AF.Gelu), reads=[PB[bk]], writes=[B_uv[ti]])
                        V(lambda v, ti=ti: v.tensor_tensor(out=SQS[:], in0=UV[ti][:, 256:512], in1=UV[ti][:, 256:512], op=ALU.mult),
                          reads=[B_uv[ti]], writes=[B_sqs])
                        V(lambda v, ti=ti: v.reduce_sum(out=sm[:, 20 + ti:21 + ti], in_=SQS[:], axis=AX.X), reads=[B_sqs], writes=[B_sm])
                    A(lambda a: a.activation(out=sm[:, 20:20 + ntile], in_=sm[:, 20:20 + ntile], func=AF.Sqrt, scale=1.0 / 256, bias=epsb[:, 0:1]),
                      reads=[B_sm, B_const], writes=[B_sm])
                    V(lambda v: v.reciprocal(out=sm[:, 20:20 + ntile], in_=sm[:, 20:20 + ntile]), reads=[B_sm], writes=[B_sm])
                    for ti in range(ntile):
                        p2 = ti % 2
                        V(lambda v, ti=ti, p2=p2: v.scalar_tensor_tensor(out=VN[p2][:], in0=UV[ti][:, 256:512], scalar=sm[:, 20 + ti:21 + ti], in1=gmlp_g[:],
                                                                         op0=ALU.mult, op1=ALU.mult),
                          reads=[B_uv[ti], B_sm, B_lw], writes=[B_vn[p2]])
                        mbk = 2 + p2

                        def mix(pe, p2=p2, mbk=mbk):
                            last = None
                            for g in range(4):
                                last = pe.matmul(PS[mbk][:, g * 64:(g + 1) * 64], spw[:, g, :], VN[p2][:, g * 64:(g + 1) * 64], start=True, stop=True)
                            return last
                        PE(mix, reads=[B_vn[p2], B_lw], writes=[PB[mbk]])
                        V(lambda v, p2=p2, mbk=mbk: v.tensor_tensor(out=MX[p2][:].rearrange("p (g c) -> p g c", g=4),
                                                                    in0=PS[mbk][:, 0:256].rearrange("p (g c) -> p g c", g=4),
                                                                    in1=spb[:].unsqueeze(2).to_broadcast([128, 4, 64]), op=ALU.add),
                          reads=[PB[mbk], B_lw], writes=[B_mx[p2]])
                        G(lambda g, p2=p2, ti=ti: g.tensor_tensor(out=YTK[p2][:], in0=MX[p2][:], in1=UV[ti][:, 0:256], op=ALU.mult),
                          reads=[B_mx[p2], B_uv[ti]], writes=[B_ytk[p2]])

                        def tr(pe, p2=p2):
                            pe.transpose(pbt[:, p2 * 256:p2 * 256 + 128], YTK[p2][:, 0:128], identb[:])
                            return pe.transpose(pbt[:, p2 * 256 + 128:p2 * 256 + 256], YTK[p2][:, 128:256], identb[:])
                        PE(tr, reads=[B_ytk[p2], B_ld], writes=[PB[7]])
                        A(lambda a, ti=ti, p2=p2: a.copy(out=YA[:, :, ti * 128:(ti + 1) * 128],
                                                         in_=pbt[:, p2 * 256:p2 * 256 + 256].rearrange("p (c t) -> p c t", c=2)),
                          reads=[PB[7]], writes=[B_ya])
                    wout_update(seq, l, b0, nt, YA, B_ya, WO, B_wo, 4)

        def mixer_pool(l, seqs):
            for seq in seqs:
                aa.reset()
                L = seq.T
                W = L + 2 * PADW
                WINB = aa.get([128, 8, 256], BF16); B_w = Buf("winb")
                WO = aa.get([128, 2, 1024], BF16); B_wo = Buf("wob")
                ZB = aa.get([128, 2, W], F32); B_zb = Buf("zb")
                T1 = aa.get([128, W], F32); B_t1 = Buf("t1")
                T2 = aa.get([128, W], F32); B_t2 = Buf("t2")
                DT = aa.get([128, 2, L], BF16); B_dt = Buf("dt")
                ET = aa.get([128, 16], F32); B_et = Buf("et")
                YB = aa.get([128, 2, 512], BF16); B_yb = Buf("yb")
                load_win(l, OFF_B, OFF_CQ, WINB, B_w)
                load_wout(l, 1, WO, B_wo)
                G(lambda g: g.memset(ZB[:, :, 0:PADW], 0.0), writes=[B_zb])
                G(lambda g: g.memset(ZB[:, :, PADW + L:W], 0.0), writes=[B_zb])
                ib = 0
                for (b0, nt) in seq.blocks:
                    hb = seq.HB[blk_of(seq, b0)]
                    for c in range(2):
                        bk = ib % 2
                        ib += 1
                        mm_group(PS[bk][:, 0:nt], [(WINB[:, j, c * 128:(c + 1) * 128], seq.H[:, j, b0:b0 + nt]) for j in range(8)],
                                 reads=[hb, B_w], writes=[PB[bk]])
                        A(lambda a, c=c, bk=bk: a.copy(out=ZB[:, c, PADW + b0:PADW + b0 + nt], in_=PS[bk][:, 0:nt]), reads=[PB[bk]], writes=[B_zb])
                for c in range(2):
                    z = ZB[:, c, :]
                    V(lambda v: v.tensor_tensor(out=T1[:, 1:W], in0=z[:, 0:W - 1], in1=z[:, 1:W], op=ALU.add), reads=[B_zb], writes=[B_t1])
                    if c == 0:
                        lo_src, lo_w = T1, 2
                        G(lambda g: g.tensor_tensor(out=T2[64:128, 2:W - 1], in0=T1[64:128, 1:W - 2], in1=T1[64:128, 3:W], op=ALU.add),
                          reads=[B_t1], writes=[B_t2])
                        hi_w = 4
                    else:
                        V(lambda v: v.tensor_tensor(out=T2[:, 2:W - 1], in0=T1[:, 1:W - 2], in1=T1[:, 3:W], op=ALU.add), reads=[B_t1], writes=[B_t2])
                        V(lambda v: v.tensor_tensor(out=T1[:, 4:W - 3], in0=T2[:, 2:W - 5], in1=T2[:, 6:W - 1], op=ALU.add), reads=[B_t2], writes=[B_t1])
                        G(lambda g: g.tensor_tensor(out=T2[64:128, 8:W - 7], in0=T1[64:128, 4:W - 11], in1=T1[64:128, 12:W - 3], op=ALU.add),
                          reads=[B_t1], writes=[B_t2])
                        lo_w, hi_w = 8, 16
                    for (r0, S, B_s, w) in ((0, T1, B_t1, lo_w), (64, T2, B_t2, hi_w)):
                        rs = slice(r0, r0 + 64)
                        V(lambda v, rs=rs, S=S, w=w, c=c: v.scalar_tensor_tensor(
                            out=DT[rs, c, :], in0=S[rs, PADW:PADW + L], scalar=1.0 / w, in1=ZB[rs, c, PADW:PADW + L], op0=ALU.mult, op1=ALU.subtract),
                          reads=[B_s, B_zb], writes=[B_dt])
                        for (e0, tcol) in ((0, 0), (8, L - 8)):
                            V(lambda v, rs=rs, S=S, e0=e0, tcol=tcol, c=c: v.tensor_tensor(
                                out=ET[rs, e0:e0 + 8], in0=S[rs, PADW + tcol:PADW + tcol + 8], in1=poole[rs, c, e0:e0 + 8], op=ALU.mult),
                              reads=[B_s, B_ld], writes=[B_et])
                            V(lambda v, rs=rs, e0=e0, tcol=tcol, c=c: v.tensor_tensor(
                                out=DT[rs, c, tcol:tcol + 8], in0=ET[rs, e0:e0 + 8], in1=ZB[rs, c, PADW + tcol:PADW + tcol + 8], op=ALU.subtract),
                              reads=[B_et, B_zb], writes=[B_dt])
                for (b0, nt) in seq.blocks:
                    for c in range(2):
                        bk = c
                        mm_group(PS[bk][:, 0:nt], [(bdwp[:, c, :], DT[:, c, b0:b0 + nt])], reads=[B_dt, B_lw], writes=[PB[bk]])
                        A(lambda a, c=c, bk=bk: a.activation(out=YB[:, c, 0:nt], in_=PS[bk][:, 0:nt], func=AF.Identity, scale=pscale[:, c:c + 1]),
                          reads=[PB[bk], B_lw], writes=[B_yb])
                    wout_update(seq, l, b0, nt, YB, B_yb, WO, B_wo, 2)

        def mixer_fourier(l, seqs):
            for seq in seqs:
                aa.reset()
                L = seq.T
                ntile = seq.ntile
                kbw = 512 if seq is lat else TC
                WIND = aa.get([128, 8, 256], BF16); B_w = Buf("wind")
                WO = aa.get([128, 2, 1024], BF16); B_wo = Buf("wod")
                ZD = aa.get([128, ntile, 256], BF16); B_zd = Buf("zd")
                TAB = aa.get([128, 2, ntile, kbw], BF16); B_tab = Buf("tab")
                UT = aa.get([128, 2, 2, 512], BF16); B_ut = Buf("ut")
                YD = aa.get([128, 2, 512], BF16); B_yd = Buf("yd")
                load_win(l, OFF_D, IN_W, WIND, B_w)
                load_wout(l, 3, WO, B_wo)
                for ti in range(ntile):
                    t0 = ti * 128
                    hb = seq.HB[blk_of(seq, t0)]
                    bk = ti % 2
                    mm_group(PS[bk][:, 0:256], [(seq.H[:, j, t0:t0 + 128], WIND[:, j, :]) for j in range(8)], reads=[hb, B_w], writes=[PB[bk]])
                    A(lambda a, ti=ti, bk=bk: a.copy(out=ZD[:, ti, :], in_=PS[bk][:, 0:256]), reads=[PB[bk]], writes=[B_zd])
                for kb, (b0, nt) in enumerate(seq.blocks):
                    if seq is lat:
                        cx.dma("sync", TAB[:, 0], dftc_d[kb], writes=[B_tab])
                        cx.dma("sync", TAB[:, 1], dfts_d[kb], writes=[B_tab])
                    else:
                        cx.dma("sync", TAB[:, 0], dftcc_d, writes=[B_tab])
                        cx.dma("sync", TAB[:, 1], dftcs_d, writes=[B_tab])
                    for csi in range(2):
                        for c in range(2):
                            bk = csi * 2 + c
                            mm_group(PS[bk][:, 0:nt], [(ZD[:, ti, c * 128:(c + 1) * 128], TAB[:, csi, ti, 0:nt]) for ti in range(ntile)],
                                     reads=[B_zd, B_tab], writes=[PB[bk]])
                            if bk % 2 == 0:
                                A(lambda a, csi=csi, c=c, bk=bk: a.copy(out=UT[:, csi, c, 0:nt], in_=PS[bk][:, 0:nt]), reads=[PB[bk]], writes=[B_ut])
                            else:
                                V(lambda v, csi=csi, c=c, bk=bk: v.tensor_copy(out=UT[:, csi, c, 0:nt], in_=PS[bk][:, 0:nt]), reads=[PB[bk]], writes=[B_ut])
                    for dj in range(2):
                        bk = 4 + dj
                        mm_group(PS[bk][:, 0:nt], [(WCS[:, csi, c, dj * 128:(dj + 1) * 128], UT[:, csi, c, 0:nt]) for csi in range(2) for c in range(2)],
                                 reads=[B_ut, B_lw], writes=[PB[bk]])
                        A(lambda a, dj=dj, bk=bk: a.copy(out=YD[:, dj, 0:nt], in_=PS[bk][:, 0:nt]), reads=[PB[bk]], writes=[B_yd])
                    wout_update(seq, l, b0, nt, YD, B_yd, WO, B_wo, 6)

        def ffn_phase(l, seqs, experts):
            moe = experts[0][3] is not None
            SL = 512
            nsl = DFF // SL
            WG = [aa.get([128, 8, SL], BF16) for _ in range(2)]
            WU = [aa.get([128, 8, SL], BF16) for _ in range(2)]
            WD = [aa.get([128, 4, D], BF16) for _ in range(2)]
            B_wgu = [Buf("wgu0"), Buf("wgu1")]
            B_wd = [Buf("wd0"), Buf("wd1")]
            ACT = [aa.get([128, 4, 512], BF16) for _ in range(2)]
            B_act = [Buf("act0"), Buf("act1")]
            SG = [aa.get([128, 512], F32) for _ in range(2)]
            B_sg = [Buf("sg0"), Buf("sg1")]
            PAB = [Buf("pab0"), Buf("pab1")]
            PC = [Buf("pc0"), Buf("pc1")]
            if moe:
                GBC = aa.get([128, T], F32)
                B_gbc = Buf("gbc")
            work = [(ei, si) for ei in range(len(experts)) for si in range(nsl)]

            def issue_load_gu(idx):
                ei, si = work[idx]
                wg, wu, wd, ge = experts[ei]
                s = idx % 2
                f0 = si * SL
                cx.dma("gpsimd", WG[s], wg[:, f0:f0 + SL].rearrange("(j p) n -> p j n", p=128), writes=[B_wgu[s]])
                cx.dma("gpsimd", WU[s], wu[:, f0:f0 + SL].rearrange("(j p) n -> p j n", p=128), writes=[B_wgu[s]])

            def issue_load_d(idx):
                ei, si = work[idx]
                wg, wu, wd, ge = experts[ei]
                s = idx % 2
                f0 = si * SL
                cx.dma("gpsimd", WD[s], wd[f0:f0 + SL, :].rearrange("(c p) n -> p c n", p=128), writes=[B_wd[s]])

            cnt = {"fc": 0, "blk": 0, "c": 0}

            def s1_begin():
                a_s = cnt["blk"] % 2
                cnt["blk"] += 1
                return a_s

            def s1_fc(idx, seq, bi, a_s, fc):
                s = idx % 2
                b0, nt = seq.blocks[bi]
                hb = seq.HB[bi]
                ba = cnt["fc"] % 2
                cnt["fc"] += 1
                pa, pbk = PS[2 * ba], PS[2 * ba + 1]

                def gu(pe):
                    last = None
                    for j in range(8):
                        last = pe.matmul(pa[:, 0:nt], WG[s][:, j, fc * 128:(fc + 1) * 128], seq.H[:, j, b0:b0 + nt], start=(j == 0), stop=(j == 7))
                    for j in range(8):
                        last = pe.matmul(pbk[:, 0:nt], WU[s][:, j, fc * 128:(fc + 1) * 128], seq.H[:, j, b0:b0 + nt], start=(j == 0), stop=(j == 7))
                    return last
                PE(gu, reads=[B_wgu[s], hb], writes=[PAB[ba]])
                A(lambda a: a.activation(out=SG[ba][:, 0:nt], in_=pa[:, 0:nt], func=AF.Silu), reads=[PAB[ba]], writes=[B_sg[ba]])
                if moe:
                    G(lambda g: g.tensor_tensor(out=SG[ba][:, 0:nt], in0=SG[ba][:, 0:nt], in1=GBC[:, b0:b0 + nt], op=ALU.mult),
                      reads=[B_sg[ba], B_gbc], writes=[B_sg[ba]])
                V(lambda v: v.tensor_tensor(out=ACT[a_s][:, fc, 0:nt], in0=SG[ba][:, 0:nt], in1=pbk[:, 0:nt], op=ALU.mult),
                  reads=[B_sg[ba], PAB[ba]], writes=[B_act[a_s]])

            def s2_dp(idx, seq, bi, a_s, dp):
                s = idx % 2
                b0, nt = seq.blocks[bi]
                xb = seq.XB[bi]
                n = seq.n
                pc = cnt["c"] % 2
                cnt["c"] += 1
                banks = (PS[4 + 2 * pc], PS[5 + 2 * pc])

                def dn(pe):
                    last = None
                    for k2 in range(2):
                        dj = dp * 2 + k2
                        for fc in range(4):
                            last = pe.matmul(banks[k2][:, 0:nt], WD[s][:, fc, dj * 128:(dj + 1) * 128], ACT[a_s][:, fc, 0:nt],
                                             start=(fc == 0), stop=(fc == 3))
                    return last
                PE(dn, reads=[B_wd[s], B_act[a_s]], writes=[PC[pc]])
                for k2 in range(2):
                    dj = dp * 2 + k2
                    V(lambda v, dj=dj, bank=banks[k2]: v.scalar_tensor_tensor(
                        out=seq.X[:, dj, b0:b0 + nt], in0=bank[:, 0:nt], scalar=MOD[:, l, 5, dj, n:n + 1],
                        in1=seq.X[:, dj, b0:b0 + nt], op0=ALU.mult, op1=ALU.add),
                      reads=[PC[pc], B_mod, xb], writes=[xb])

            def stage2(idx, seq, bi, a_s):
                for dp in range(4):
                    s2_dp(idx, seq, bi, a_s, dp)

            issue_load_gu(0)
            issue_load_d(0)
            pending = None
            for idx, (ei, si) in enumerate(work):
                ge = experts[ei][3]
                if idx + 1 < len(work):
                    issue_load_gu(idx + 1)
                if moe and si == 0:
                    for (b0, nt) in lat.blocks:
                        mm_group(PS[6][:, 0:nt], [(sel8[0:8, ge, :], gateT_g[0][0:8, b0:b0 + nt])], reads=[B_ld, gateT_g[1]], writes=[PC[1]])
                        V(lambda v, b0=b0: v.tensor_copy(out=GBC[:, b0:b0 + nt], in_=PS[6][:, 0:nt]), reads=[PC[1]], writes=[B_gbc])
                first = True
                for seq in seqs:
                    for bi in range(len(seq.blocks)):
                        a_s = s1_begin()
                        for q4 in range(4):
                            s1_fc(idx, seq, bi, a_s, q4)
                            if pending is not None:
                                s2_dp(*pending, q4)
                        pending = (idx, seq, bi, a_s)
                        if first and idx + 1 < len(work):
                            issue_load_d(idx + 1)
                        first = False
            stage2(*pending)

        gateT_g = [None, None]

        def main_schedule():
          for b in range(2):
              lat.n = b
              cs.n = 2
              aa.reset()
              chk("ada")
              for bi, (t0, nt) in enumerate(lat.blocks):
                  cx.dma("sync", XT[:, :, t0:t0 + nt], xT_d[b, :, t0:t0 + nt].rearrange("(j p) t -> p j t", p=128), writes=[lat.XB[bi]])
              cx.dma("sync", XC[:], ctxT_d[b].rearrange("(j p) t -> p j t", p=128), writes=[cs.XB[0]])
              for l in range(2):
                  last = (l == 1)
                  load_layer_small(l)
                  norm_phase([cs, lat], l, GM1, 0)
                  if b == 0 and l == 0:
                      debug_dump("h0", HT[:, :, 0:512], lat.HB[0])
                  chk("norm1")
                  both = [lat] if last else [cs, lat]
                  mixer_attention(l, both, do_ctx_q=not last)
                  chk("attn")
                  if b == 0 and l == 0:
                      cx.barrier()
                      debug_dump("x_att", XT[:, :, 0:512], lat.XB[0])
                  mixer_spatial(l, both)
                  chk("spatial")
                  if b == 0 and l == 0:
                      cx.barrier()
                      debug_dump("x_sp", XT[:, :, 0:512], lat.XB[0])
                  mixer_pool(l, both)
                  chk("pool")
                  if b == 0 and l == 0:
                      cx.barrier()
                      debug_dump("x_pool", XT[:, :, 0:512], lat.XB[0])
                  mixer_fourier(l, both)
                  chk("fourier")
                  if b == 0 and l == 0:
                      cx.barrier()
                      debug_dump("x_mix", XT[:, :, 0:512], lat.XB[0])
                      debug_dump("xc_mix", XC[:], cs.XB[0])
                  if not last:
                      norm_phase([cs, lat], l, GM2, 3)
                      aa.reset()
                      ffn_phase(l, [cs, lat], [(fg_d[0], fu_d[0], fd_d[0], None)])
                      chk("ffn0")
                      if b == 0:
                          cx.barrier()
                          debug_dump("x_l0", XT[:, :, 0:512], lat.XB[0])
                          debug_dump("xc_l0", XC[:], cs.XB[0])
                  else:
                      gateT = XC[:].rearrange("p j t -> p (j t)")
                      B_gateT = cs.XB[0]
                      gateT_g[0], gateT_g[1] = gateT, B_gateT
                      norm_phase([lat], l, GM2, 3, router=True, gateT=gateT, B_gateT=B_gateT)
                      if b == 0:
                          debug_dump("gateT", gateT[0:8, 0:512], B_gateT)
                      aa.reset()
                      ffn_phase(l, [lat], [(mg_d[0, e], mu_d[0, e], md_d[0, e], e) for e in range(NE)])
              cx.barrier()
              for bi, (t0, nt) in enumerate(lat.blocks):
                  cx.dma("sync", outT_d[b, :, t0:t0 + nt].rearrange("(j p) t -> p j t", p=128), XT[:, :, t0:t0 + nt], reads=[lat.XB[bi]])

        try:
            main_schedule()
        except _Stop:
            pass
        cx.finish()
        print("instructions (tracked ops):", cx.ninst, "semaphores:", cx.nsem)
    return nc


_CACHE = {}


def _pmajor(v):
    return np.ascontiguousarray(v.reshape(*v.shape[:-1], 8, 128).swapaxes(-1, -2))


def prepare_inputs(inp):
    f = lambda a: np.ascontiguousarray(np.asarray(a, dtype=np.float32))
    L = 2
    shared = dict(make_consts())
    shared["ada_w"] = f(inp["ada_w"])
    shared["ada_b"] = np.ascontiguousarray(f(inp["ada_b"]).reshape(L, 48, 128).transpose(0, 2, 1))
    shared["norm1_g"] = _pmajor(f(inp["norm1_g"]))
    shared["norm2_g"] = _pmajor(f(inp["norm2_g"]))
    shared["w_in"] = f(inp["w_in"])
    bc = lambda v: np.ascontiguousarray(np.broadcast_to(v[:, None, :], (L, 128, v.shape[-1])))
    shared["gmlp_g"] = bc(f(inp["gmlp_norm_g"]))
    shared["spatial_wT"] = np.ascontiguousarray(f(inp["spatial_w"]).transpose(0, 3, 1, 2))
    shared["spatial_bT"] = np.ascontiguousarray(f(inp["spatial_b"]).transpose(0, 2, 1))
    pw = f(inp["pool_w"])
    bd = np.zeros((L, 128, 2, 128), np.float32)
    for c in range(2):
        for h in range(2):
            bd[:, h * 64:(h + 1) * 64, c, h * 64:(h + 1) * 64] = pw[:, 2 * c + h]
    shared["bdwp"] = bd
    shared["pool_scale"] = np.ascontiguousarray(f(inp["pool_scale"]).reshape(L, 2, 128).transpose(0, 2, 1))
    shared["q_norm_g"] = bc(f(inp["q_norm_g"]))
    shared["kv_norm_g"] = bc(f(inp["kv_norm_g"]))
    shared["qk_q_g"] = bc(np.tile(f(inp["qk_q_g"]), (1, 4)))
    shared["qk_k_g"] = bc(np.tile(f(inp["qk_k_g"]), (1, 4)))
    shared["w_uq"] = f(inp["w_uq"])
    shared["w_ukv"] = f(inp["w_ukv"])
    shared["fourier_w"] = f(inp["fourier_w"])
    shared["w_out"] = f(inp["w_out"])
    shared["ffn_w_gate"] = f(inp["ffn_w_gate"])
    shared["ffn_w_up"] = f(inp["ffn_w_up"])
    shared["ffn_w_down"] = f(inp["ffn_w_down"])
    shared["router_w"] = np.ascontiguousarray(f(inp["router_w"])[0].reshape(8, 128, 8).transpose(1, 0, 2))
    shared["moe_w_gate"] = f(inp["moe_w_gate"])
    shared["moe_w_up"] = f(inp["moe_w_up"])
    shared["moe_w_down"] = f(inp["moe_w_down"])
    x = f(inp["x"])
    ctx = f(inp["ctx"])
    c = f(inp["c"])
    cc = f(inp["c_ctx"])
    maps = []
    for i in range(8):
        m = dict(shared)
        m["xT"] = np.ascontiguousarray(x[2 * i:2 * i + 2].transpose(0, 2, 1))
        m["ctxT"] = np.ascontiguousarray(ctx[2 * i:2 * i + 2].transpose(0, 2, 1))
        c3 = np.stack([c[2 * i], c[2 * i + 1], cc], axis=-1)
        m["cT"] = np.ascontiguousarray(c3.reshape(8, 128, 3).transpose(1, 0, 2))
        maps.append(m)
    return maps


def kernel(**inputs):
    if "nc" not in _CACHE:
        _CACHE["nc"] = build_program()
    nc = _CACHE["nc"]
    maps = prepare_inputs(inputs)
    res = run_bass_kernel_spmd(nc, maps, core_ids=list(range(8)))
    out = np.empty((16, T, D), np.float32)
    for i in range(8):
        o = res.results[i]["outT"]
        out[2 * i:2 * i + 2] = o.transpose(0, 2, 1)
    return out
```

```python
import numpy as np
import ml_dtypes
from contextlib import ExitStack
import concourse.bass as bass
import concourse.mybir as mybir
from concourse.bass_utils import run_bass_kernel_spmd

F32 = mybir.dt.float32
BF16 = mybir.dt.bfloat16
U8 = mybir.dt.uint8
AF = mybir.ActivationFunctionType
ALU = mybir.AluOpType
AX = mybir.AxisListType

D = 1024
T = 2048
TC = 256
NK = T + TC
DFF = 3584
NE = 8
EPS = 1e-6
OFF_A, OFF_B, OFF_CQ, OFF_CKV, OFF_CKR, OFF_D, IN_W = 0, 512, 768, 960, 1088, 1120, 1376
PADW = 8
SEM_LIMIT = 8000


class Buf:
    __slots__ = ("w", "r", "name", "dsem", "dcount")

    def __init__(self, name=""):
        self.w = None
        self.r = {}
        self.name = name
        self.dsem = None
        self.dcount = 0


class Eng:
    def __init__(self, name, h):
        self.name = name
        self.h = h
        self.sem = None
        self.count = 0
        self.waited = {}
        self.own = set()


class Ctx:
    def __init__(self, nc, stack):
        self.nc = nc
        self.stack = stack
        self.nsem = 0
        self.engs = {n: Eng(n, getattr(nc, n)) for n in ("tensor", "vector", "scalar", "gpsimd", "sync")}
        self.dma_tokens = {}
        self.ninst = 0

    def new_sem(self, name):
        self.nsem += 1
        return self.stack.enter_context(self.nc.semaphore(f"s{self.nsem}_{name}"))

    def _wait(self, e, deps):
        need = {}
        for d in deps:
            if d is None:
                continue
            s, v = d
            if e.name == "tensor" and s in e.own:
                continue
            if need.get(s, 0) < v:
                need[s] = v
        for s, v in need.items():
            if e.waited.get(s, 0) < v:
                e.h.wait_ge(s, v)
                e.waited[s] = v

    def _deps(self, reads, writes):
        deps = []
        for b in list(reads) + list(writes):
            if isinstance(b.w, list):
                deps.extend(b.w)
            else:
                deps.append(b.w)
        for b in writes:
            deps.extend(b.r.items())
        return deps

    def _commit(self, tok, reads, writes):
        for b in writes:
            b.w = tok
            b.r = {}
        s, v = tok
        for b in reads:
            if b in writes:
                continue
            if b.r.get(s, 0) < v:
                b.r[s] = v

    def _tok(self, e, ins):
        if e.sem is None or e.count >= SEM_LIMIT:
            e.sem = self.new_sem(e.name)
            e.own.add(e.sem)
            e.count = 0
        e.count += 1
        ins.then_inc(e.sem, 1)
        return (e.sem, e.count)

    def op(self, eng, emit, reads=(), writes=()):
        e = self.engs[eng]
        self._wait(e, self._deps(reads, writes))
        ins = emit(e.h)
        tok = self._tok(e, ins)
        self._commit(tok, reads, writes)
        self.ninst += 1
        return tok

    def dma(self, eng, out, in_, reads=(), writes=()):
        e = self.engs[eng]
        self._wait(e, self._deps(reads, writes))
        owner = writes[0] if writes else reads[0]
        kind = "sw" if eng == "gpsimd" else "hw"
        if owner.dsem is None:
            owner.dsem = {}
        if kind not in owner.dsem:
            owner.dsem[kind] = [self.new_sem("d" + kind + owner.name), 0]
        ent = owner.dsem[kind]
        ent[1] += 16
        e.h.dma_start(out=out, in_=in_).then_inc(ent[0], 16)
        tok = (ent[0], ent[1])
        self._commit(tok, reads, writes)
        if writes:
            others = [(v[0], v[1]) for k, v in owner.dsem.items() if k != kind and v[1] > 0]
            if others:
                owner.w = [tok] + others
        self.dma_tokens[ent[0]] = ent[1]
        return tok

    def barrier(self):
        sp = self.engs["sync"]
        deps = list(self.dma_tokens.items())
        for e in self.engs.values():
            if e.sem is not None:
                deps.append((e.sem, e.count))
        self._wait(sp, deps)
        ins = sp.h.nop()
        tok = self._tok(sp, ins)
        for e in self.engs.values():
            if e is not sp:
                self._wait(e, [tok])

    def finish(self):
        self.barrier()


def _bf(a):
    return np.ascontiguousarray(a.astype(ml_dtypes.bfloat16))


def make_consts():
    c = {}
    c["ident"] = np.eye(128, dtype=np.float32)
    t = np.arange(T)
    row = (t // 64).astype(np.float32)
    col = (t % 64).astype(np.float32)
    inv = (np.float32(10000.0) ** (-np.arange(8, dtype=np.float32) / np.float32(8))).astype(np.float32)
    a0 = row[:, None] * inv[None, :]
    a1 = col[:, None] * inv[None, :]
    cos32 = np.concatenate([np.cos(a0), np.cos(a0), np.cos(a1), np.cos(a1)], axis=1).astype(np.float32)
    sin32 = np.concatenate([-np.sin(a0), np.sin(a0), -np.sin(a1), np.sin(a1)], axis=1).astype(np.float32)
    c["rope_cos"] = np.ascontiguousarray(cos32.reshape(16, 128, 32).transpose(1, 0, 2))
    c["rope_sin"] = np.ascontiguousarray(sin32.reshape(16, 128, 32).transpose(1, 0, 2))
    def dft(L):
        lk = (np.arange(L)[:, None] * np.arange(L)[None, :]) % L
        ang = 2.0 * np.pi * lk / L
        return np.cos(ang) / np.sqrt(L), -np.sin(ang) / np.sqrt(L)
    CL, SLn = dft(T)
    def lay(M):
        return _bf(M.reshape(16, 128, 4, 512).transpose(2, 1, 0, 3))
    c["dft_c"] = lay(CL)
    c["dft_s"] = lay(SLn)
    C2, S2n = dft(TC)
    c["dftc_c"] = _bf(C2.reshape(2, 128, TC).transpose(1, 0, 2))
    c["dftc_s"] = _bf(S2n.reshape(2, 128, TC).transpose(1, 0, 2))
    cm = (np.arange(64)[:, None] * np.arange(64)[None, :]) % 64
    C64 = np.cos(2 * np.pi * cm / 64) / 8.0
    S64 = np.sin(2 * np.pi * cm / 64) / 8.0
    bd = np.zeros((2, 128, 128))
    for h in range(2):
        bd[0, h * 64:(h + 1) * 64, h * 64:(h + 1) * 64] = C64
        bd[1, h * 64:(h + 1) * 64, h * 64:(h + 1) * 64] = S64
    c["bd64"] = _bf(bd.transpose(1, 0, 2))
    E = np.zeros((128, 2, 16), np.float32)
    for cch in range(2):
        for p in range(128):
            w = (2, 4, 8, 16)[2 * cch + p // 64]
            for i in range(8):
                E[p, cch, i] = 1.0 / min(i + w // 2, w)
                tt = 8 - i
                E[p, cch, 8 + i] = 1.0 / min(tt + w // 2, w)
    c["pool_e"] = E
    sel = np.zeros((8, 8, 128), np.float32)
    for e in range(8):
        sel[e, e, :] = 1.0
    c["sel8"] = sel
    return c


class _Stop(Exception):
    pass


def build_program(dbg=None, stop=None):
    nc = bass.Bass("TRN2", target_bir_lowering=False)
    dbg = dbg or {}

    def din(name, shape, dt=F32):
        return nc.dram_tensor(name, list(shape), dt, kind="ExternalInput").ap()

    xT_d = din("xT", [2, D, T])
    ctxT_d = din("ctxT", [2, D, TC])
    cT_d = din("cT", [128, 8, 3])
    ada_w_d = din("ada_w", [2, D, 6 * D])
    ada_b_d = din("ada_b", [2, 128, 48])
    n1g_d = din("norm1_g", [2, 128, 8])
    n2g_d = din("norm2_g", [2, 128, 8])
    w_in_d = din("w_in", [2, D, IN_W])
    gmlp_g_d = din("gmlp_g", [2, 128, 256])
    spw_d = din("spatial_wT", [2, 128, 4, 128])
    spb_d = din("spatial_bT", [2, 128, 4])
    bdwp_d = din("bdwp", [2, 128, 2, 128])
    pscale_d = din("pool_scale", [2, 128, 2])
    qng_d = din("q_norm_g", [2, 128, 192])
    kvng_d = din("kv_norm_g", [2, 128, 128])
    qkq_d = din("qk_q_g", [2, 128, 384])
    qkk_d = din("qk_k_g", [2, 128, 384])
    w_uq_d = din("w_uq", [2, 192, 384])
    w_ukv_d = din("w_ukv", [2, 128, 512])
    wf_d = din("fourier_w", [2, 256, 256])
    w_out_d = din("w_out", [2, D, D])
    fg_d = din("ffn_w_gate", [1, D, DFF])
    fu_d = din("ffn_w_up", [1, D, DFF])
    fd_d = din("ffn_w_down", [1, DFF, D])
    rw_d = din("router_w", [128, 8, 8])
    mg_d = din("moe_w_gate", [1, NE, D, DFF])
    mu_d = din("moe_w_up", [1, NE, D, DFF])
    md_d = din("moe_w_down", [1, NE, DFF, D])
    ident_d = din("ident", [128, 128])
    rcos_d = din("rope_cos", [128, 16, 32])
    rsin_d = din("rope_sin", [128, 16, 32])
    dftc_d = din("dft_c", [4, 128, 16, 512], BF16)
    dfts_d = din("dft_s", [4, 128, 16, 512], BF16)
    dftcc_d = din("dftc_c", [128, 2, TC], BF16)
    dftcs_d = din("dftc_s", [128, 2, TC], BF16)
    bd64_d = din("bd64", [128, 2, 128], BF16)
    poole_d = din("pool_e", [128, 2, 16])
    sel8_d = din("sel8", [8, 8, 128])
    outT_d = nc.dram_tensor("outT", [2, D, T], F32, kind="ExternalOutput").ap()
    dbg_d = {k: nc.dram_tensor("dbg_" + k, list(shp), F32, kind="ExternalOutput").ap() for k, shp in dbg.items()}

    with ExitStack() as st:
        cx = Ctx(nc, st)

        def sb(name, shape, dt):
            return st.enter_context(nc.sbuf_tensor("sb_" + name, list(shape), dt))

        XT = sb("XT", [128, 8, T], F32)
        XC = sb("XC", [128, 8, TC], F32)
        HT = sb("HT", [128, 8, T], BF16)
        HC = sb("HC", [128, 8, TC], BF16)
        ARENA_BYTES = 72 * 1024
        arena = sb("arena", [128, ARENA_BYTES], U8)
        ident = sb("ident", [128, 128], F32)
        identb = sb("identb", [128, 128], BF16)
        onesb = sb("onesb", [128, 128], BF16)
        onesf = sb("onesf", [128, 128], F32)
        epsb = sb("epsb", [128, 1], F32)
        cact = sb("cact", [128, 8, 3], F32)
        MOD = sb("MOD", [128, 2, 6, 8, 3], F32)
        GM1 = sb("GM1", [128, 2, 8, 3], F32)
        GM2 = sb("GM2", [128, 2, 8, 3], F32)
        adab = sb("adab", [128, 2, 48], F32)
        n1g = sb("n1g", [128, 2, 8], F32)
        n2g = sb("n2g", [128, 2, 8], F32)
        rcos = sb("rcos", [128, 16, 32], F32)
        rsin = sb("rsin", [128, 16, 32], F32)
        bd64 = sb("bd64", [128, 2, 128], BF16)
        poole = sb("poole", [128, 2, 16], F32)
        sel8 = sb("sel8", [8, 8, 128], F32)
        rwf = sb("rwf", [128, 8, 8], F32)
        gmlp_g = sb("gmlp_g", [128, 256], F32)
        spw = sb("spw", [128, 4, 128], BF16)
        spb = sb("spb", [128, 4], F32)
        bdwp = sb("bdwp", [128, 2, 128], BF16)
        pscale = sb("pscale", [128, 2], F32)
        qng = sb("qng", [128, 192], F32)
        kvng = sb("kvng", [128, 128], F32)
        qkq = sb("qkq", [128, 384], F32)
        qkk = sb("qkk", [128, 384], F32)
        wuq = sb("wuq", [128, 2, 384], BF16)
        wukv = sb("wukv", [128, 512], BF16)
        wfb = sb("wfb", [128, 2, 256], BF16)
        WCS = sb("WCS", [128, 2, 2, 256], BF16)
        small = sb("small", [128, 64], F32)
        small2 = [sb("small_a", [128, 32], F32), sb("small_b", [128, 32], F32)]

        PS = [st.enter_context(nc.psum_tensor(f"ps{i}", [128, 512], F32)) for i in range(8)]
        PB = [Buf(f"ps{i}") for i in range(8)]

        class AA:
            def __init__(self):
                self.off = 0

            def reset(self):
                cx.barrier()
                self.off = 0

            def get(self, shape, dt):
                esz = 4 if dt == F32 else 2
                n = int(np.prod(shape[1:]))
                nbytes = n * esz
                self.off = (self.off + 31) // 32 * 32
                assert self.off + nbytes <= ARENA_BYTES, (self.off, nbytes)
                ap = arena[:, self.off:self.off + nbytes]
                if dt != U8:
                    ap = ap.bitcast(dt)
                self.off += nbytes
                if len(shape) == 2:
                    return ap
                names = " ".join(f"d{i}" for i in range(len(shape) - 1))
                kw = {f"d{i}": int(shape[i + 1]) for i in range(len(shape) - 2)}
                return ap.rearrange(f"p ({names}) -> p {names}", **kw)

        aa = AA()

        def chk(name):
            if stop == name:
                raise _Stop()
        B_const = Buf("const")
        B_mod = Buf("mod")
        B_lw = Buf("lw")
        B_small = Buf("small")

        def V(fn, reads=(), writes=()):
            return cx.op("vector", fn, reads, writes)

        def A(fn, reads=(), writes=()):
            return cx.op("scalar", fn, reads, writes)

        def G(fn, reads=(), writes=()):
            return cx.op("gpsimd", fn, reads, writes)

        def PE(fn, reads=(), writes=()):
            return cx.op("tensor", fn, reads, writes)

        def mm_group(out_ap, pairs, reads, writes):
            def emit(pe):
                n = len(pairs)
                last = None
                for i, (l, r) in enumerate(pairs):
                    last = pe.matmul(out_ap, l, r, start=(i == 0), stop=(i == n - 1))
                return last
            return PE(emit, reads, writes)

        def debug_dump(name, ap, buf):
            if name in dbg_d:
                cx.dma("gpsimd", dbg_d[name], ap, reads=[buf])

        V(lambda v: v.memset(onesb[:], 1.0), writes=[B_const])
        V(lambda v: v.memset(onesf[:], 1.0), writes=[B_const])
        V(lambda v: v.memset(epsb[:], EPS), writes=[B_const])
        B_ld = Buf("cload")
        for dst, src in ((ident[:], ident_d), (cact[:], cT_d), (rcos[:], rcos_d), (rsin[:], rsin_d),
                         (poole[:], poole_d), (sel8[:], sel8_d), (rwf[:], rw_d), (bd64[:], bd64_d),
                         (adab[:], ada_b_d.rearrange("l p j -> p l j")),
                         (n1g[:], n1g_d.rearrange("l p j -> p l j")), (n2g[:], n2g_d.rearrange("l p j -> p l j"))):
            cx.dma("sync", dst, src, writes=[B_ld])
        cx.dma("gpsimd", identb[:], ident_d, writes=[B_ld])
        A(lambda a: a.activation(out=cact[:], in_=cact[:], func=AF.Silu), reads=[B_ld], writes=[B_ld])

        aa.reset()
        AW = [aa.get([128, 8, 1024], F32) for _ in range(2)]
        AWB = [Buf("aw0"), Buf("aw1")]
        k = 0
        for l in range(2):
            for which in range(6):
                s = k % 2
                k += 1
                cx.dma("sync", AW[s], ada_w_d[l, :, which * 1024:(which + 1) * 1024].rearrange("(j p) n -> p j n", p=128),
                       writes=[AWB[s]])
                pb = PB[s]
                ps = PS[s]

                def emit(pe, s=s, ps=ps):
                    last = None
                    for j in range(8):
                        for kk in range(8):
                            last = pe.matmul(ps[:, j * 3:(j + 1) * 3], AW[s][:, kk, j * 128:(j + 1) * 128], cact[:, kk, :],
                                             start=(kk == 0), stop=(kk == 7))
                    return last
                PE(emit, reads=[AWB[s], B_ld], writes=[pb])
                V(lambda v, l=l, which=which, ps=ps: v.tensor_tensor(
                    out=MOD[:, l, which], in0=ps[:, 0:24].rearrange("p (j n) -> p j n", n=3),
                    in1=adab[:, l, which * 8:(which + 1) * 8].unsqueeze(2).to_broadcast([128, 8, 3]), op=ALU.add),
                  reads=[pb, B_ld], writes=[B_mod])
        for l in range(2):
            for (GM, ng, which) in ((GM1, n1g, 1), (GM2, n2g, 4)):
                V(lambda v, GM=GM, l=l, which=which: v.tensor_scalar(out=GM[:, l], in0=MOD[:, l, which], scalar1=1.0, scalar2=None, op0=ALU.add),
                  reads=[B_mod], writes=[B_mod])
                V(lambda v, GM=GM, l=l, ng=ng: v.tensor_tensor(out=GM[:, l], in0=GM[:, l], in1=ng[:, l].unsqueeze(2).to_broadcast([128, 8, 3]), op=ALU.mult),
                  reads=[B_mod, B_ld], writes=[B_mod])

        class Seq:
            pass

        lat = Seq()
        lat.name = "lat"; lat.T = T; lat.X = XT; lat.H = HT; lat.blocks = [(i * 512, 512) for i in range(4)]
        lat.XB = [Buf(f"xb{i}") for i in range(4)]; lat.HB = [Buf(f"hb{i}") for i in range(4)]
        lat.ntile = 16; lat.tok0 = TC; lat.rope = True
        cs = Seq()
        cs.name = "ctx"; cs.T = TC; cs.X = XC; cs.H = HC; cs.blocks = [(0, TC)]
        cs.XB = [Buf("xcb")]; cs.HB = [Buf("hcb")]
        cs.ntile = 2; cs.tok0 = 0; cs.rope = False

        def blk_of(seq, t0):
            return t0 // 512

        def norm_phase(seqs, l, GM, shift_which, router=False, gateT=None, B_gateT=None):
            aa.reset()
            SQ2 = [aa.get([128, 8, 512], BF16) for _ in range(2)]
            RS2 = [aa.get([128, 512], F32) for _ in range(2)]
            NTMP = 8
            TMP = [aa.get([128, 512], F32) for _ in range(NTMP)]
            B_sq2, B_rs2 = [Buf("sq0"), Buf("sq1")], [Buf("rs0"), Buf("rs1")]
            nblk = 0
            B_tmp = [Buf(f"tmp{i}") for i in range(NTMP)]
            if router:
                H32 = aa.get([128, 8, 512], F32)
                B_h32 = Buf("h32")
                LG = aa.get([128, 16], F32)
                B_lg = Buf("lg")
            it = 0
            allblk = [(seq, bi, t0, nt) for seq in seqs for bi, (t0, nt) in enumerate(seq.blocks)]

            def squares(k):
                seq, bi, t0, nt = allblk[k]
                SQ, B_sq = SQ2[k % 2], B_sq2[k % 2]
                for j in range(8):
                    if j % 2 == 1 and not router:
                        G(lambda g, j=j: g.tensor_tensor(out=SQ[:, j, 0:nt], in0=seq.X[:, j, t0:t0 + nt], in1=seq.X[:, j, t0:t0 + nt], op=ALU.mult),
                          reads=[seq.XB[bi]], writes=[B_sq])
                    else:
                        A(lambda a, j=j: a.activation(out=SQ[:, j, 0:nt], in_=seq.X[:, j, t0:t0 + nt], func=AF.Square),
                          reads=[seq.XB[bi]], writes=[B_sq])
                mm_group(PS[k % 2][:, 0:nt], [(onesb[:], SQ[:, j, 0:nt]) for j in range(8)], reads=[B_sq, B_const], writes=[PB[k % 2]])

            squares(0)
            for k, (seq, bi, t0, nt) in enumerate(allblk):
                    n = seq.n
                    xb, hb = seq.XB[bi], seq.HB[bi]
                    RS, B_rs = RS2[k % 2], B_rs2[k % 2]
                    pbn = k % 2
                    if k + 1 < len(allblk):
                        squares(k + 1)
                    A(lambda a: a.activation(out=RS[:, 0:nt], in_=PS[pbn][:, 0:nt], func=AF.Sqrt, scale=1.0 / D, bias=epsb[:, 0:1]),
                      reads=[PB[pbn], B_const], writes=[B_rs])
                    V(lambda v: v.reciprocal(out=RS[:, 0:nt], in_=RS[:, 0:nt]), reads=[B_rs], writes=[B_rs])
                    for j in range(8):
                        s = it % NTMP
                        it += 1
                        V(lambda v, j=j, s=s: v.tensor_tensor(out=TMP[s][:, 0:nt], in0=seq.X[:, j, t0:t0 + nt], in1=RS[:, 0:nt], op=ALU.mult),
                          reads=[xb, B_rs], writes=[B_tmp[s]])
                        if router:
                            A(lambda a, j=j, s=s: a.activation(out=H32[:, j, 0:nt], in_=TMP[s][:, 0:nt], func=AF.Identity,
                                                               scale=GM[:, l, j, n:n + 1], bias=MOD[:, l, shift_which, j, n:n + 1]),
                              reads=[B_tmp[s], B_mod], writes=[B_h32])
                            G(lambda g, j=j: g.tensor_copy(out=seq.H[:, j, t0:t0 + nt], in_=H32[:, j, 0:nt]), reads=[B_h32], writes=[hb])
                        else:
                            A(lambda a, j=j, s=s: a.activation(out=seq.H[:, j, t0:t0 + nt], in_=TMP[s][:, 0:nt], func=AF.Identity,
                                                               scale=GM[:, l, j, n:n + 1], bias=MOD[:, l, shift_which, j, n:n + 1]),
                              reads=[B_tmp[s], B_mod], writes=[hb])
                    if router:
                        for ti in range(nt // 128):
                            c0 = ti * 128
                            mm_group(PS[3][:, 0:8], [(H32[:, j, c0:c0 + 128], rwf[:, j, :]) for j in range(8)],
                                     reads=[B_h32, B_ld], writes=[PB[3]])
                            lg = LG[:, 0:8]
                            w2 = LG[:, 8:16]
                            sm = small
                            V(lambda v: v.tensor_copy(out=lg, in_=PS[3][:, 0:8]), reads=[PB[3]], writes=[B_lg])
                            V(lambda v: v.reduce_max(out=sm[:, 0:1], in_=lg, axis=AX.X), reads=[B_lg], writes=[B_small])
                            V(lambda v: v.tensor_scalar(out=w2, in0=lg, scalar1=sm[:, 0:1], scalar2=-1e30, op0=ALU.is_equal, op1=ALU.mult),
                              reads=[B_lg, B_small], writes=[B_lg])
                            V(lambda v: v.tensor_tensor(out=w2, in0=w2, in1=lg, op=ALU.add), reads=[B_lg], writes=[B_lg])
                            V(lambda v: v.reduce_max(out=sm[:, 1:2], in_=w2, axis=AX.X), reads=[B_lg], writes=[B_small])
                            V(lambda v: v.tensor_scalar(out=w2, in0=lg, scalar1=sm[:, 1:2], scalar2=None, op0=ALU.is_ge),
                              reads=[B_lg, B_small], writes=[B_lg])
                            V(lambda v: v.tensor_scalar(out=sm[:, 2:3], in0=sm[:, 0:1], scalar1=-1.0, scalar2=None, op0=ALU.mult),
                              reads=[B_small], writes=[B_small])
                            A(lambda a: a.activation(out=lg, in_=lg, func=AF.Exp, bias=sm[:, 2:3], scale=1.0), reads=[B_lg, B_small], writes=[B_lg])
                            V(lambda v: v.tensor_tensor(out=lg, in0=lg, in1=w2, op=ALU.mult), reads=[B_lg], writes=[B_lg])
                            V(lambda v: v.reduce_sum(out=sm[:, 3:4], in_=lg, axis=AX.X), reads=[B_lg], writes=[B_small])
                            V(lambda v: v.reciprocal(out=sm[:, 3:4], in_=sm[:, 3:4]), reads=[B_small], writes=[B_small])
                            V(lambda v: v.tensor_scalar(out=lg, in0=lg, scalar1=sm[:, 3:4], scalar2=None, op0=ALU.mult),
                              reads=[B_lg, B_small], writes=[B_lg])
                            PE(lambda pe: pe.transpose(PS[2][0:8, 0:128], lg, ident[:]), reads=[B_lg, B_ld], writes=[PB[2]])
                            V(lambda v, c0=c0: v.tensor_copy(out=gateT[0:8, t0 + c0:t0 + c0 + 128], in_=PS[2][0:8, 0:128]),
                              reads=[PB[2]], writes=[B_gateT])

        def wout_update(seq, l, t0, nt, YT, B_y, WO, B_wo, bank0):
            xb = seq.XB[blk_of(seq, t0)]
            n = seq.n
            for dj in range(8):
                bk = bank0 + dj % 2
                mm_group(PS[bk][:, 0:nt], [(WO[:, c, dj * 128:(dj + 1) * 128], YT[:, c, 0:nt]) for c in range(2)],
                         reads=[B_y, B_wo], writes=[PB[bk]])
                V(lambda v, dj=dj, bk=bk: v.scalar_tensor_tensor(
                    out=seq.X[:, dj, t0:t0 + nt], in0=PS[bk][:, 0:nt], scalar=MOD[:, l, 2, dj, n:n + 1],
                    in1=seq.X[:, dj, t0:t0 + nt], op0=ALU.mult, op1=ALU.add),
                  reads=[PB[bk], B_mod, xb], writes=[xb])

        def load_wout(l, m, WO, B_wo):
            cx.dma("gpsimd", WO, w_out_d[l, m * 256:(m + 1) * 256, :].rearrange("(c p) n -> p c n", p=128), writes=[B_wo])

        def load_win(l, c0, c1, W, B_w):
            cx.dma("gpsimd", W, w_in_d[l, :, c0:c1].rearrange("(j p) n -> p j n", p=128), writes=[B_w])

        def load_layer_small(l):
            for dst, src, q in ((gmlp_g[:], gmlp_g_d[l], "sync"), (spw[:], spw_d[l], "gpsimd"), (spb[:], spb_d[l], "sync"),
                                (bdwp[:], bdwp_d[l], "gpsimd"), (pscale[:], pscale_d[l], "sync"), (qng[:], qng_d[l], "sync"),
                                (kvng[:], kvng_d[l], "sync"), (qkq[:], qkq_d[l], "sync"), (qkk[:], qkk_d[l], "sync"),
                                (wuq[:, 0, :], w_uq_d[l, 0:128, :], "gpsimd"), (wuq[0:64, 1, :], w_uq_d[l, 128:192, :], "gpsimd"),
                                (wukv[:], w_ukv_d[l], "gpsimd"),
                                (wfb[:], wf_d[l].rearrange("(c p) n -> p c n", p=128), "gpsimd")):
                cx.dma(q, dst, src, writes=[B_lw])
            for csi in range(2):
                for c in range(2):
                    mm_group(PS[0][:, 0:256], [(bd64[:, csi, :], wfb[:, c, :])], reads=[B_ld, B_lw], writes=[PB[0]])
                    V(lambda v, csi=csi, c=c: v.tensor_copy(out=WCS[:, csi, c, :], in_=PS[0][:, 0:256]), reads=[PB[0]], writes=[B_lw])

        def mixer_attention(l, seqs_q, do_ctx_q):
            aa.reset()
            WINC = aa.get([128, 8, 352], BF16); B_w = Buf("winc")
            WO = aa.get([128, 2, 1024], BF16); B_wo = Buf("woc")
            KT = aa.get([128, 4, NK], BF16); B_kt = Buf("kt")
            VP = aa.get([128, 18, 2, 192], BF16); B_vp = Buf("vp")

            class _S:
                pass

            def mk(i):
                o = _S()
                o.TM = aa.get([128, 352], F32); o.B_tm = Buf(f"tm{i}")
                o.CN = aa.get([128, 320], BF16); o.B_cn = Buf(f"cn{i}")
                o.CNT = aa.get([128, 3, 128], BF16); o.B_cnt = Buf(f"cnt{i}")
                o.QF = aa.get([128, 4, 96], F32); o.B_qf = Buf(f"qf{i}")
                o.KF = o.QF; o.B_kf = o.B_qf
                o.QB = aa.get([128, 4, 96], BF16); o.B_qb = Buf(f"qb{i}")
                o.KB = o.QB; o.B_kb = o.B_qb
                if i < 2:
                    o.sm = small2[i]
                else:
                    o.sm = aa.get([128, 32], F32)
                o.B_small = Buf(f"small{i}")
                o.pb = i
                o.ub = 4 + i
                return o
            SS = [mk(0), mk(1)]
            R1 = aa.get([128, 4, 32], F32); B_r1 = Buf("r1")
            SQS = aa.get([128, 384], F32); B_sqs = Buf("sqs")
            SQA = aa.get([128, 192], BF16); B_sqa = Buf("sqa")
            off_q = aa.off
            SS += [mk(2), mk(3)]
            end_extra = aa.off
            aa.off = off_q
            QT2 = [aa.get([128, 4, 512], BF16) for _ in range(2)]; B_qt2 = [Buf("qt0"), Buf("qt1")]
            PT = [aa.get([128, 512], BF16) for _ in range(3)]; B_pt = [Buf(f"pt{i}") for i in range(3)]
            RD = aa.get([128, 512], F32); B_rd = Buf("rd")
            BC = aa.get([128, 512], F32); B_bc = Buf("bc")
            AT = aa.get([128, 2, 512], BF16); B_at = Buf("at")
            aa.off = max(aa.off, end_extra)
            for o in SS:
                o.R1 = R1; o.B_r1 = B_r1; o.SQS = SQS; o.B_sqs = B_sqs
            load_win(l, OFF_CQ, OFF_D, WINC, B_w)
            load_wout(l, 2, WO, B_wo)
            V(lambda v: v.memset(VP.rearrange("p a b c -> p (a b c)"), 0.0), writes=[B_vp])
            for pr_ in range(2):
                V(lambda v, pr_=pr_: v.memset(VP[:, :, pr_, 64:65], 1.0), writes=[B_vp])

            def rms_scale(S, src_ap, width, col):
                A(lambda a: a.activation(out=SQA[:, 0:width], in_=src_ap, func=AF.Square, accum_out=S.sm[:, col:col + 1]),
                  reads=[S.B_tm], writes=[B_sqa, S.B_small])
                A(lambda a: a.activation(out=S.sm[:, col:col + 1], in_=S.sm[:, col:col + 1], func=AF.Ln, scale=1.0 / width, bias=epsb[:, 0:1]),
                  reads=[S.B_small, B_const], writes=[S.B_small])
                A(lambda a: a.activation(out=S.sm[:, col:col + 1], in_=S.sm[:, col:col + 1], func=AF.Exp, scale=-0.5),
                  reads=[S.B_small], writes=[S.B_small])

            def head_norm(S, src, B_src, gains, dst, B_dst, colbase):
                V(lambda v: v.tensor_tensor(out=S.SQS[:].rearrange("p (h d) -> p h d", h=4), in0=src, in1=src, op=ALU.mult),
                  reads=[B_src], writes=[S.B_sqs])
                V(lambda v: v.reduce_sum(out=S.sm[:, colbase:colbase + 4], in_=S.SQS[:].rearrange("p (h d) -> p h d", h=4), axis=AX.X),
                  reads=[S.B_sqs], writes=[S.B_small])
                yield
                A(lambda a: a.activation(out=S.sm[:, colbase:colbase + 4], in_=S.sm[:, colbase:colbase + 4], func=AF.Ln, scale=1.0 / 96, bias=epsb[:, 0:1]),
                  reads=[S.B_small, B_const], writes=[S.B_small])
                A(lambda a: a.activation(out=S.sm[:, colbase:colbase + 4], in_=S.sm[:, colbase:colbase + 4], func=AF.Exp, scale=-0.5),
                  reads=[S.B_small], writes=[S.B_small])
                yield
                V(lambda v: v.tensor_tensor(out=dst, in0=src, in1=S.sm[:, colbase:colbase + 4].unsqueeze(2).to_broadcast([128, 4, 96]), op=ALU.mult),
                  reads=[B_src, S.B_small], writes=[B_dst])
                V(lambda v: v.tensor_tensor(out=dst, in0=dst, in1=gains.rearrange("p (h d) -> p h d", h=4), op=ALU.mult),
                  reads=[B_dst, B_lw], writes=[B_dst])

            def rope(S, src, B_src, ti):
                xr = src[:, :, 64:96]
                cosb = rcos[:, ti, :].unsqueeze(1).to_broadcast([128, 4, 32])
                x5 = xr.rearrange("p h (a b f) -> p h a b f", a=2, b=2)
                r5 = S.R1[:].rearrange("p h (a b f) -> p h a b f", a=2, b=2)
                s5 = rsin[:, ti, :].rearrange("p (a b f) -> p a b f", a=2, b=2)
                for bsel in range(2):
                    V(lambda v, bsel=bsel: v.tensor_tensor(
                        out=r5[:, :, :, bsel, :], in0=x5[:, :, :, 1 - bsel, :],
                        in1=s5[:, :, bsel, :].unsqueeze(1).to_broadcast([128, 4, 2, 8]), op=ALU.mult),
                      reads=[B_src, B_ld], writes=[S.B_r1])
                V(lambda v: v.tensor_tensor(out=xr, in0=xr, in1=cosb, op=ALU.mult), reads=[B_src, B_ld], writes=[B_src])
                V(lambda v: v.tensor_tensor(out=xr, in0=xr, in1=S.R1[:], op=ALU.add), reads=[B_src, S.B_r1], writes=[B_src])

            def run_window(jobs, sets, make):
                jobs = list(jobs)
                free = list(sets)
                active = []
                while jobs or active:
                    while jobs and free:
                        si = free.pop(0)
                        active.append((make(jobs.pop(0), si), si))
                    for item in list(active):
                        g, si = item
                        try:
                            next(g)
                        except StopIteration:
                            active.remove(item)
                            free.append(si)
                        yield

            def kv_tile(seq, ti, si):
                S = SS[si]
                P, PBp = PS[S.pb], PB[S.pb]
                U, PBu = PS[S.ub], PB[S.ub]
                pbt = P[:].bitcast(BF16)
                t0 = ti * 128
                g0 = seq.tok0 + t0
                kt_i = g0 // 128
                hb = seq.HB[blk_of(seq, t0)]
                mm_group(P[:, 192:352], [(seq.H[:, j, t0:t0 + 128], WINC[:, j, 192:352]) for j in range(8)], reads=[hb, B_w], writes=[PBp])
                yield
                A(lambda a: a.copy(out=S.TM[:, 192:352], in_=P[:, 192:352]), reads=[PBp], writes=[S.B_tm])
                rms_scale(S, S.TM[:, 192:320], 128, 1)
                yield
                V(lambda v: v.scalar_tensor_tensor(out=S.CN[:, 192:320], in0=S.TM[:, 192:320], scalar=S.sm[:, 1:2], in1=kvng[:], op0=ALU.mult, op1=ALU.mult),
                  reads=[S.B_tm, S.B_small, B_lw], writes=[S.B_cn])
                yield
                PE(lambda pe: pe.transpose(pbt[:, 0:128], S.CN[:, 192:320], identb[:]), reads=[S.B_cn, B_ld], writes=[PBp])
                yield
                V(lambda v: v.tensor_copy(out=S.CNT[:, 2, :], in_=pbt[:, 0:128]), reads=[PBp], writes=[S.B_cnt])
                yield
                mm_group(U[:, 0:512], [(S.CNT[:, 2, :], wukv[:])], reads=[S.B_cnt, B_lw], writes=[PBu])
                yield
                kv4 = U[:, 0:512].rearrange("p (h d) -> p h d", h=4)
                for e in range(2):
                    A(lambda a, e=e: a.copy(out=VP[:, kt_i, :, e * 128:e * 128 + 64], in_=kv4[:, e:4:2, 64:128]),
                      reads=[PBu], writes=[B_vp])
                A(lambda a: a.copy(out=S.KF[:, :, 0:64], in_=kv4[:, :, 0:64]), reads=[PBu], writes=[S.B_kf])
                V(lambda v: v.tensor_copy(out=S.KF[:, :, 64:96], in_=S.TM[:, 320:352].unsqueeze(1).to_broadcast([128, 4, 32])),
                  reads=[S.B_tm], writes=[S.B_kf])
                yield
                for _ in head_norm(S, S.KF[:], S.B_kf, qkk[:], S.KF[:], S.B_kf, 8):
                    yield
                if seq.rope:
                    rope(S, S.KF[:], S.B_kf, ti)
                V(lambda v: v.tensor_copy(out=S.KB[:], in_=S.KF[:]), reads=[S.B_kf], writes=[S.B_kb])
                yield

                def trk(pe):
                    last = None
                    for h in range(4):
                        last = pe.transpose(pbt[0:96, h * 128:(h + 1) * 128], S.KB[:, h, :], identb[:])
                    return last
                PE(trk, reads=[S.B_kb, B_ld], writes=[PBp])
                yield
                V(lambda v: v.tensor_copy(out=KT[0:96, :, g0:g0 + 128], in_=pbt[0:96, 0:512].rearrange("p (h t) -> p h t", h=4)),
                  reads=[PBp], writes=[B_kt])

            def q_tile(seq, ti, qc, qs, si):
                S = SS[si]
                P, PBp = PS[S.pb], PB[S.pb]
                pbt = P[:].bitcast(BF16)
                t0 = ti * 128
                hb = seq.HB[blk_of(seq, t0)]
                QT = QT2[qs]; B_qt = B_qt2[qs]
                mm_group(P[:, 0:192], [(seq.H[:, j, t0:t0 + 128], WINC[:, j, 0:192]) for j in range(8)], reads=[hb, B_w], writes=[PBp])
                yield
                V(lambda v: v.tensor_copy(out=S.TM[:, 0:192], in_=P[:, 0:192]), reads=[PBp], writes=[S.B_tm])
                rms_scale(S, S.TM[:, 0:192], 192, 0)
                yield
                V(lambda v: v.scalar_tensor_tensor(out=S.CN[:, 0:192], in0=S.TM[:, 0:192], scalar=S.sm[:, 0:1], in1=qng[:], op0=ALU.mult, op1=ALU.mult),
                  reads=[S.B_tm, S.B_small, B_lw], writes=[S.B_cn])
                yield
                yield

                def tr(pe):
                    pe.transpose(pbt[:, 0:128], S.CN[:, 0:128], identb[:])
                    return pe.transpose(pbt[0:64, 128:256], S.CN[:, 128:192], identb[:])
                PE(tr, reads=[S.B_cn, B_ld], writes=[PBp])
                yield
                V(lambda v: v.tensor_copy(out=S.CNT[:, 0, :], in_=pbt[:, 0:128]), reads=[PBp], writes=[S.B_cnt])
                V(lambda v: v.tensor_copy(out=S.CNT[0:64, 1, :], in_=pbt[0:64, 128:256]), reads=[PBp], writes=[S.B_cnt])
                yield
                yield
                mm_group(P[:, 128:512], [(S.CNT[:, 0, :], wuq[:, 0, :]), (S.CNT[0:64, 1, :], wuq[0:64, 1, :])],
                         reads=[S.B_cnt, B_lw], writes=[PBp])
                yield
                V(lambda v: v.tensor_copy(out=S.QF[:].rearrange("p h d -> p (h d)"), in_=P[:, 128:512]), reads=[PBp], writes=[S.B_qf])
                yield
                for _ in head_norm(S, S.QF[:], S.B_qf, qkq[:], S.QF[:], S.B_qf, 12):
                    yield
                if seq.rope:
                    rope(S, S.QF[:], S.B_qf, ti)
                V(lambda v: v.tensor_copy(out=S.QB[:], in_=S.QF[:]), reads=[S.B_qf], writes=[S.B_qb])
                yield
                yield
                yield

                def trq(pe):
                    last = None
                    for h in range(4):
                        last = pe.transpose(pbt[0:96, h * 128:(h + 1) * 128], S.QB[:, h, :], identb[:])
                    return last
                PE(trq, reads=[S.B_qb, B_ld], writes=[PBp])
                yield
                V(lambda v: v.tensor_copy(out=QT[0:96, :, qc:qc + 128], in_=pbt[0:96, 0:512].rearrange("p (h t) -> p h t", h=4)),
                  reads=[PBp], writes=[B_qt])

            kv_jobs = [(seq, ti) for seq in (cs, lat) for ti in range(seq.ntile)]
            for _ in run_window(kv_jobs, [0, 1, 2, 3], lambda job, si: kv_tile(job[0], job[1], si)):
                pass
            chk("attn_kv")
            cx.barrier()
            SS[1].pb = 3

            scale = 96 ** -0.5
            it = [0]
            blks = [(seq, t0, nt) for seq in seqs_q for (t0, nt) in seq.blocks]

            def q_jobs(bi):
                seq, t0, nt = blks[bi]
                jobs = [(seq, t0 // 128 + tq, tq * 128, bi % 2) for tq in range(nt // 128)]
                return run_window(jobs, [0, 1], lambda job, si: q_tile(job[0], job[1], job[2], job[3], si))

            for _ in q_jobs(0):
                pass
            for bi, (seq, t0, nt) in enumerate(blks):
                    nkt = 18 if seq is lat else 2
                    QT = QT2[bi % 2]; B_qt = B_qt2[bi % 2]
                    nxt = q_jobs(bi + 1) if bi + 1 < len(blks) else iter(())
                    items = [(h, kt_i) for h in range(4) for kt_i in range(nkt)]
                    slot_of = {}

                    def do_S(i):
                        h, kt_i = items[i]
                        s = it[0] % 3
                        sbk = 1 + it[0] % 2
                        it[0] += 1
                        slot_of[i] = s
                        mm_group(PS[sbk][:, 0:nt], [(KT[0:96, h, kt_i * 128:(kt_i + 1) * 128], QT[0:96, h, 0:nt])],
                                 reads=[B_kt, B_qt], writes=[PB[sbk]])
                        A(lambda a: a.activation(out=PT[s][:, 0:nt], in_=PS[sbk][:, 0:nt], func=AF.Exp, scale=scale),
                          reads=[PB[sbk]], writes=[B_pt[s]])

                    def do_PV(i):
                        h, kt_i = items[i]
                        s = slot_of[i]
                        pr, odd = h // 2, h % 2
                        accb = 4 + h % 2
                        acc = PS[accb]
                        lhs = VP[:, kt_i, pr, 64:192] if odd else VP[:, kt_i, pr, 0:65]
                        M = 128 if odd else 65
                        PE(lambda pe: pe.matmul(acc[0:M, 0:nt], lhs, PT[s][:, 0:nt], start=(kt_i == 0), stop=(kt_i == nkt - 1)),
                           reads=[B_vp, B_pt[s]], writes=[PB[accb]])
                        if kt_i == nkt - 1:
                            dr = 0 if odd else 64
                            V(lambda v: v.reciprocal(out=RD[dr:dr + 1, 0:nt], in_=acc[dr:dr + 1, 0:nt]), reads=[PB[accb]], writes=[B_rd])

                    def do_norm(h, stage):
                        pr, odd = h // 2, h % 2
                        accb = 4 + h % 2
                        acc = PS[accb]
                        dr = 0 if odd else 64
                        if stage == 0:
                            mm_group(PS[6][:, 0:nt], [(onesf[dr:dr + 1, :], RD[dr:dr + 1, 0:nt])], reads=[B_rd, B_const], writes=[PB[6]])
                        elif stage == 1:
                            V(lambda v: v.tensor_copy(out=BC[:, 0:nt], in_=PS[6][:, 0:nt]), reads=[PB[6]], writes=[B_bc])
                        else:
                            r0 = 64 if odd else 0
                            V(lambda v: v.tensor_tensor(out=AT[r0:r0 + 64, pr, 0:nt], in0=acc[r0:r0 + 64, 0:nt], in1=BC[r0:r0 + 64, 0:nt], op=ALU.mult),
                              reads=[PB[accb], B_bc], writes=[B_at])

                    n_it = len(items)
                    LA = 2
                    last_step = n_it + LA - 1
                    offs = (8, 10, 12) if nkt >= 16 else (2, 2, 2)
                    due = {}
                    for i in range(n_it + LA):
                        if i < n_it:
                            do_S(i)
                        j = i - LA
                        if j >= 0:
                            do_PV(j)
                            h, kt_i = items[j]
                            if kt_i == nkt - 1:
                                for stage in range(3):
                                    due.setdefault(min(i + offs[stage], last_step), []).append((h, stage))
                        for (h, stage) in due.pop(i, []):
                            do_norm(h, stage)
                        next(nxt, None)
                    assert not due
                    for _ in nxt:
                        pass
                    wout_update(seq, l, t0, nt, AT, B_at, WO, B_wo, 6)

        def mixer_spatial(l, seqs):
            aa.reset()
            WINA = aa.get([128, 8, 512], BF16); B_w = Buf("wina")
            WO = aa.get([128, 2, 1024], BF16); B_wo = Buf("woa")
            UV = [aa.get([128, 512], F32) for _ in range(4)]; B_uv = [Buf(f"uv{i}") for i in range(4)]
            VN = [aa.get([128, 256], BF16) for _ in range(2)]; B_vn = [Buf(f"vn{i}") for i in range(2)]
            SQS = aa.get([128, 256], F32); B_sqs = Buf("sqsa")
            YTK = [aa.get([128, 256], BF16) for _ in range(2)]; B_ytk = [Buf(f"ytk{i}") for i in range(2)]
            MX = [aa.get([128, 256], F32) for _ in range(2)]; B_mx = [Buf(f"mx{i}") for i in range(2)]
            YA = aa.get([128, 2, 512], BF16); B_ya = Buf("ya")
            load_win(l, OFF_A, OFF_B, WINA, B_w)
            load_wout(l, 0, WO, B_wo)
            sm = small
            B_sm = Buf("sm_sp")
            pbt = PS[7][:].bitcast(BF16)
            for seq in seqs:
                for (b0, nt) in seq.blocks:
                    ntile = nt // 128
                    for ti in range(ntile):
                        t0 = b0 + ti * 128
                        hb = seq.HB[blk_of(seq, t0)]
                        bk = ti % 2
                        mm_group(PS[bk][:, 0:512], [(seq.H[:, j, t0:t0 + 128], WINA[:, j, :]) for j in range(8)], reads=[hb, B_w], writes=[PB[bk]])
                        A(lambda a, ti=ti, bk=bk: a.activation(out=UV[ti][:], in_=PS[bk][:, 0:512], func=AF.Gelu), reads=[PB[bk]], writes=[B_uv[ti]])
                        V(lambda v, ti=ti: v.tensor_tensor(out=SQS[:], in0=UV[ti][:, 256:512], in1=UV[ti][:, 256:512], op=ALU.mult),
                          reads=[B_uv[ti]], writes=[B_sqs])
                        V(lambda v, ti=ti: v.reduce_sum(out=sm[:, 20 + ti:21 + ti], in_=SQS[:], axis=AX.X), reads=[B_sqs], writes=[B_sm])
                    A(lambda a: a.activation(out=sm[:, 20:20 + ntile], in_=sm[:, 20:20 + ntile], func=AF.Sqrt, scale=1.0 / 256, bias=epsb[:, 0:1]),
                      reads=[B_sm, B_const], writes=[B_sm])
                    V(lambda v: v.reciprocal(out=sm[:, 20:20 + ntile], in_=sm[:, 20:20 + ntile]), reads=[B_sm], writes=[B_sm])
                    for ti in range(ntile):
                        p2 = ti % 2
                        V(lambda v, ti=ti, p2=p2: v.scalar_tensor_tensor(out=VN[p2][:], in0=UV[ti][:, 256:512], scalar=sm[:, 20 + ti:21 + ti], in1=gmlp_g[:],
                                                                         op0=ALU.mult, op1=ALU.mult),
                          reads=[B_uv[ti], B_sm, B_lw], writes=[B_vn[p2]])
                        mbk = 2 + p2

                        def mix(pe, p2=p2, mbk=mbk):
                            last = None
                            for g in range(4):
                                last = pe.matmul(PS[mbk][:, g * 64:(g + 1) * 64], spw[:, g, :], VN[p2][:, g * 64:(g + 1) * 64], start=True, stop=True)
                            return last
                        PE(mix, reads=[B_vn[p2], B_lw], writes=[PB[mbk]])
                        V(lambda v, p2=p2, mbk=mbk: v.tensor_tensor(out=MX[p2][:].rearrange("p (g c) -> p g c", g=4),
                                                                    in0=PS[mbk][:, 0:256].rearrange("p (g c) -> p g c", g=4),
                                                                    in1=spb[:].unsqueeze(2).to_broadcast([128, 4, 64]), op=ALU.add),
                          reads=[PB[mbk], B_lw], writes=[B_mx[p2]])
                        G(lambda g, p2=p2, ti=ti: g.tensor_tensor(out=YTK[p2][:], in0=MX[p2][:], in1=UV[ti][:, 0:256], op=ALU.mult),
                          reads=[B_mx[p2], B_uv[ti]], writes=[B_ytk[p2]])

                        def tr(pe, p2=p2):
                            pe.transpose(pbt[:, p2 * 256:p2 * 256 + 128], YTK[p2][:, 0:128], identb[:])
                            return pe.transpose(pbt[:, p2 * 256 + 128:p2 * 256 + 256], YTK[p2][:, 128:256], identb[:])
                        PE(tr, reads=[B_ytk[p2], B_ld], writes=[PB[7]])
                        A(lambda a, ti=ti, p2=p2: a.copy(out=YA[:, :, ti * 128:(ti + 1) * 128],
                                                         in_=pbt[:, p2 * 256:p2 * 256 + 256].rearrange("p (c t) -> p c t", c=2)),
                          reads=[PB[7]], writes=[B_ya])
                    wout_update(seq, l, b0, nt, YA, B_ya, WO, B_wo, 4)

        def mixer_pool(l, seqs):
            for seq in seqs:
                aa.reset()
                L = seq.T
                W = L + 2 * PADW
                WINB = aa.get([128, 8, 256], BF16); B_w = Buf("winb")
                WO = aa.get([128, 2, 1024], BF16); B_wo = Buf("wob")
                ZB = aa.get([128, 2, W], F32); B_zb = Buf("zb")
                T1 = aa.get([128, W], F32); B_t1 = Buf("t1")
                T2 = aa.get([128, W], F32); B_t2 = Buf("t2")
                DT = aa.get([128, 2, L], BF16); B_dt = Buf("dt")
                ET = aa.get([128, 16], F32); B_et = Buf("et")
                YB = aa.get([128, 2, 512], BF16); B_yb = Buf("yb")
                load_win(l, OFF_B, OFF_CQ, WINB, B_w)
                load_wout(l, 1, WO, B_wo)
                G(lambda g: g.memset(ZB[:, :, 0:PADW], 0.0), writes=[B_zb])
                G(lambda g: g.memset(ZB[:, :, PADW + L:W], 0.0), writes=[B_zb])
                ib = 0
                for (b0, nt) in seq.blocks:
                    hb = seq.HB[blk_of(seq, b0)]
                    for c in range(2):
                        bk = ib % 2
                        ib += 1
                        mm_group(PS[bk][:, 0:nt], [(WINB[:, j, c * 128:(c + 1) * 128], seq.H[:, j, b0:b0 + nt]) for j in range(8)],
                                 reads=[hb, B_w], writes=[PB[bk]])
                        A(lambda a, c=c, bk=bk: a.copy(out=ZB[:, c, PADW + b0:PADW + b0 + nt], in_=PS[bk][:, 0:nt]), reads=[PB[bk]], writes=[B_zb])
                for c in range(2):
                    z = ZB[:, c, :]
                    V(lambda v: v.tensor_tensor(out=T1[:, 1:W], in0=z[:, 0:W - 1], in1=z[:, 1:W], op=ALU.add), reads=[B_zb], writes=[B_t1])
                    if c == 0:
                        lo_src, lo_w = T1, 2
                        G(lambda g: g.tensor_tensor(out=T2[64:128, 2:W - 1], in0=T1[64:128, 1:W - 2], in1=T1[64:128, 3:W], op=ALU.add),
                          reads=[B_t1], writes=[B_t2])
                        hi_w = 4
                    else:
                        V(lambda v: v.tensor_tensor(out=T2[:, 2:W - 1], in0=T1[:, 1:W - 2], in1=T1[:, 3:W], op=ALU.add), reads=[B_t1], writes=[B_t2])
                        V(lambda v: v.tensor_tensor(out=T1[:, 4:W - 3], in0=T2[:, 2:W - 5], in1=T2[:, 6:W - 1], op=ALU.add), reads=[B_t2], writes=[B_t1])
                        G(lambda g: g.tensor_tensor(out=T2[64:128, 8:W - 7], in0=T1[64:128, 4:W - 11], in1=T1[64:128, 12:W - 3], op=ALU.add),
                          reads=[B_t1], writes=[B_t2])
                        lo_w, hi_w = 8, 16
                    for (r0, S, B_s, w) in ((0, T1, B_t1, lo_w), (64, T2, B_t2, hi_w)):
                        rs = slice(r0, r0 + 64)
                        V(lambda v, rs=rs, S=S, w=w, c=c: v.scalar_tensor_tensor(
                            out=DT[rs, c, :], in0=S[rs, PADW:PADW + L], scalar=1.0 / w, in1=ZB[rs, c, PADW:PADW + L], op0=ALU.mult, op1=ALU.subtract),
                          reads=[B_s, B_zb], writes=[B_dt])
                        for (e0, tcol) in ((0, 0), (8, L - 8)):
                            V(lambda v, rs=rs, S=S, e0=e0, tcol=tcol, c=c: v.tensor_tensor(
                                out=ET[rs, e0:e0 + 8], in0=S[rs, PADW + tcol:PADW + tcol + 8], in1=poole[rs, c, e0:e0 + 8], op=ALU.mult),
                              reads=[B_s, B_ld], writes=[B_et])
                            V(lambda v, rs=rs, e0=e0, tcol=tcol, c=c: v.tensor_tensor(
                                out=DT[rs, c, tcol:tcol + 8], in0=ET[rs, e0:e0 + 8], in1=ZB[rs, c, PADW + tcol:PADW + tcol + 8], op=ALU.subtract),
                              reads=[B_et, B_zb], writes=[B_dt])
                for (b0, nt) in seq.blocks:
                    for c in range(2):
                        bk = c
                        mm_group(PS[bk][:, 0:nt], [(bdwp[:, c, :], DT[:, c, b0:b0 + nt])], reads=[B_dt, B_lw], writes=[PB[bk]])
                        A(lambda a, c=c, bk=bk: a.activation(out=YB[:, c, 0:nt], in_=PS[bk][:, 0:nt], func=AF.Identity, scale=pscale[:, c:c + 1]),
                          reads=[PB[bk], B_lw], writes=[B_yb])
                    wout_update(seq, l, b0, nt, YB, B_yb, WO, B_wo, 2)

        def mixer_fourier(l, seqs):
            for seq in seqs:
                aa.reset()
                L = seq.T
                ntile = seq.ntile
                kbw = 512 if seq is lat else TC
                WIND = aa.get([128, 8, 256], BF16); B_w = Buf("wind")
                WO = aa.get([128, 2, 1024], BF16); B_wo = Buf("wod")
                ZD = aa.get([128, ntile, 256], BF16); B_zd = Buf("zd")
                TAB = aa.get([128, 2, ntile, kbw], BF16); B_tab = Buf("tab")
                UT = aa.get([128, 2, 2, 512], BF16); B_ut = Buf("ut")
                YD = aa.get([128, 2, 512], BF16); B_yd = Buf("yd")
                load_win(l, OFF_D, IN_W, WIND, B_w)
                load_wout(l, 3, WO, B_wo)
                for ti in range(ntile):
                    t0 = ti * 128
                    hb = seq.HB[blk_of(seq, t0)]
                    bk = ti % 2
                    mm_group(PS[bk][:, 0:256], [(seq.H[:, j, t0:t0 + 128], WIND[:, j, :]) for j in range(8)], reads=[hb, B_w], writes=[PB[bk]])
                    A(lambda a, ti=ti, bk=bk: a.copy(out=ZD[:, ti, :], in_=PS[bk][:, 0:256]), reads=[PB[bk]], writes=[B_zd])
                for kb, (b0, nt) in enumerate(seq.blocks):
                    if seq is lat:
                        cx.dma("sync", TAB[:, 0], dftc_d[kb], writes=[B_tab])
                        cx.dma("sync", TAB[:, 1], dfts_d[kb], writes=[B_tab])
                    else:
                        cx.dma("sync", TAB[:, 0], dftcc_d, writes=[B_tab])
                        cx.dma("sync", TAB[:, 1], dftcs_d, writes=[B_tab])
                    for csi in range(2):
                        for c in range(2):
                            bk = csi * 2 + c
                            mm_group(PS[bk][:, 0:nt], [(ZD[:, ti, c * 128:(c + 1) * 128], TAB[:, csi, ti, 0:nt]) for ti in range(ntile)],
                                     reads=[B_zd, B_tab], writes=[PB[bk]])
                            if bk % 2 == 0:
                                A(lambda a, csi=csi, c=c, bk=bk: a.copy(out=UT[:, csi, c, 0:nt], in_=PS[bk][:, 0:nt]), reads=[PB[bk]], writes=[B_ut])
                            else:
                                V(lambda v, csi=csi, c=c, bk=bk: v.tensor_copy(out=UT[:, csi, c, 0:nt], in_=PS[bk][:, 0:nt]), reads=[PB[bk]], writes=[B_ut])
                    for dj in range(2):
                        bk = 4 + dj
                        mm_group(PS[bk][:, 0:nt], [(WCS[:, csi, c, dj * 128:(dj + 1) * 128], UT[:, csi, c, 0:nt]) for csi in range(2) for c in range(2)],
                                 reads=[B_ut, B_lw], writes=[PB[bk]])
                        A(lambda a, dj=dj, bk=bk: a.copy(out=YD[:, dj, 0:nt], in_=PS[bk][:, 0:nt]), reads=[PB[bk]], writes=[B_yd])
                    wout_update(seq, l, b0, nt, YD, B_yd, WO, B_wo, 6)

        def ffn_phase(l, seqs, experts):
            moe = experts[0][3] is not None
            SL = 512
            nsl = DFF // SL
            WG = [aa.get([128, 8, SL], BF16) for _ in range(2)]
            WU = [aa.get([128, 8, SL], BF16) for _ in range(2)]
            WD = [aa.get([128, 4, D], BF16) for _ in range(2)]
            B_wgu = [Buf("wgu0"), Buf("wgu1")]
            B_wd = [Buf("wd0"), Buf("wd1")]
            ACT = [aa.get([128, 4, 512], BF16) for _ in range(2)]
            B_act = [Buf("act0"), Buf("act1")]
            SG = [aa.get([128, 512], F32) for _ in range(2)]
            B_sg = [Buf("sg0"), Buf("sg1")]
            PAB = [Buf("pab0"), Buf("pab1")]
            PC = [Buf("pc0"), Buf("pc1")]
            if moe:
                GBC = aa.get([128, T], F32)
                B_gbc = Buf("gbc")
            work = [(ei, si) for ei in range(len(experts)) for si in range(nsl)]

            def issue_load_gu(idx):
                ei, si = work[idx]
                wg, wu, wd, ge = experts[ei]
                s = idx % 2
                f0 = si * SL
                cx.dma("gpsimd", WG[s], wg[:, f0:f0 + SL].rearrange("(j p) n -> p j n", p=128), writes=[B_wgu[s]])
                cx.dma("gpsimd", WU[s], wu[:, f0:f0 + SL].rearrange("(j p) n -> p j n", p=128), writes=[B_wgu[s]])

            def issue_load_d(idx):
                ei, si = work[idx]
                wg, wu, wd, ge = experts[ei]
                s = idx % 2
                f0 = si * SL
                cx.dma("gpsimd", WD[s], wd[f0:f0 + SL, :].rearrange("(c p) n -> p c n", p=128), writes=[B_wd[s]])

            cnt = {"fc": 0, "blk": 0, "c": 0}

            def s1_begin():
                a_s = cnt["blk"] % 2
                cnt["blk"] += 1
                return a_s

            def s1_fc(idx, seq, bi, a_s, fc):
                s = idx % 2
                b0, nt = seq.blocks[bi]
                hb = seq.HB[bi]
                ba = cnt["fc"] % 2
                cnt["fc"] += 1
                pa, pbk = PS[2 * ba], PS[2 * ba + 1]

                def gu(pe):
                    last = None
                    for j in range(8):
                        last = pe.matmul(pa[:, 0:nt], WG[s][:, j, fc * 128:(fc + 1) * 128], seq.H[:, j, b0:b0 + nt], start=(j == 0), stop=(j == 7))
                    for j in range(8):
                        last = pe.matmul(pbk[:, 0:nt], WU[s][:, j, fc * 128:(fc + 1) * 128], seq.H[:, j, b0:b0 + nt], start=(j == 0), stop=(j == 7))
                    return last
                PE(gu, reads=[B_wgu[s], hb], writes=[PAB[ba]])
                A(lambda a: a.activation(out=SG[ba][:, 0:nt], in_=pa[:, 0:nt], func=AF.Silu), reads=[PAB[ba]], writes=[B_sg[ba]])
                if moe:
                    G(lambda g: g.tensor_tensor(out=SG[ba][:, 0:nt], in0=SG[ba][:, 0:nt], in1=GBC[:, b0:b0 + nt], op=ALU.mult),
                      reads=[B_sg[ba], B_gbc], writes=[B_sg[ba]])
                V(lambda v: v.tensor_tensor(out=ACT[a_s][:, fc, 0:nt], in0=SG[ba][:, 0:nt], in1=pbk[:, 0:nt], op=ALU.mult),
                  reads=[B_sg[ba], PAB[ba]], writes=[B_act[a_s]])

            def s2_dp(idx, seq, bi, a_s, dp):
                s = idx % 2
                b0, nt = seq.blocks[bi]
                xb = seq.XB[bi]
                n = seq.n
                pc = cnt["c"] % 2
                cnt["c"] += 1
                banks = (PS[4 + 2 * pc], PS[5 + 2 * pc])

                def dn(pe):
                    last = None
                    for k2 in range(2):
                        dj = dp * 2 + k2
                        for fc in range(4):
                            last = pe.matmul(banks[k2][:, 0:nt], WD[s][:, fc, dj * 128:(dj + 1) * 128], ACT[a_s][:, fc, 0:nt],
                                             start=(fc == 0), stop=(fc == 3))
                    return last
                PE(dn, reads=[B_wd[s], B_act[a_s]], writes=[PC[pc]])
                for k2 in range(2):
                    dj = dp * 2 + k2
                    V(lambda v, dj=dj, bank=banks[k2]: v.scalar_tensor_tensor(
                        out=seq.X[:, dj, b0:b0 + nt], in0=bank[:, 0:nt], scalar=MOD[:, l, 5, dj, n:n + 1],
                        in1=seq.X[:, dj, b0:b0 + nt], op0=ALU.mult, op1=ALU.add),
                      reads=[PC[pc], B_mod, xb], writes=[xb])

            def stage2(idx, seq, bi, a_s):
                for dp in range(4):
                    s2_dp(idx, seq, bi, a_s, dp)

            issue_load_gu(0)
            issue_load_d(0)
            pending = None
            for idx, (ei, si) in enumerate(work):
                ge = experts[ei][3]
                if idx + 1 < len(work):
                    issue_load_gu(idx + 1)
                if moe and si == 0:
                    for (b0, nt) in lat.blocks:
                        mm_group(PS[6][:, 0:nt], [(sel8[0:8, ge, :], gateT_g[0][0:8, b0:b0 + nt])], reads=[B_ld, gateT_g[1]], writes=[PC[1]])
                        V(lambda v, b0=b0: v.tensor_copy(out=GBC[:, b0:b0 + nt], in_=PS[6][:, 0:nt]), reads=[PC[1]], writes=[B_gbc])
                first = True
                for seq in seqs:
                    for bi in range(len(seq.blocks)):
                        a_s = s1_begin()
                        for q4 in range(4):
                            s1_fc(idx, seq, bi, a_s, q4)
                            if pending is not None:
                                s2_dp(*pending, q4)
                        pending = (idx, seq, bi, a_s)
                        if first and idx + 1 < len(work):
                            issue_load_d(idx + 1)
                        first = False
            stage2(*pending)

        gateT_g = [None, None]

        def main_schedule():
          for b in range(2):
              lat.n = b
              cs.n = 2
              aa.reset()
              chk("ada")
              for bi, (t0, nt) in enumerate(lat.blocks):
                  cx.dma("sync", XT[:, :, t0:t0 + nt], xT_d[b, :, t0:t0 + nt].rearrange("(j p) t -> p j t", p=128), writes=[lat.XB[bi]])
              cx.dma("sync", XC[:], ctxT_d[b].rearrange("(j p) t -> p j t", p=128), writes=[cs.XB[0]])
              for l in range(2):
                  last = (l == 1)
                  load_layer_small(l)
                  norm_phase([cs, lat], l, GM1, 0)
                  if b == 0 and l == 0:
                      debug_dump("h0", HT[:, :, 0:512], lat.HB[0])
                  chk("norm1")
                  both = [lat] if last else [cs, lat]
                  mixer_attention(l, both, do_ctx_q=not last)
                  chk("attn")
                  if b == 0 and l == 0:
                      cx.barrier()
                      debug_dump("x_att", XT[:, :, 0:512], lat.XB[0])
                  mixer_spatial(l, both)
                  chk("spatial")
                  if b == 0 and l == 0:
                      cx.barrier()
                      debug_dump("x_sp", XT[:, :, 0:512], lat.XB[0])
                  mixer_pool(l, both)
                  chk("pool")
                  if b == 0 and l == 0:
                      cx.barrier()
                      debug_dump("x_pool", XT[:, :, 0:512], lat.XB[0])
                  mixer_fourier(l, both)
                  chk("fourier")
                  if b == 0 and l == 0:
                      cx.barrier()
                      debug_dump("x_mix", XT[:, :, 0:512], lat.XB[0])
                      debug_dump("xc_mix", XC[:], cs.XB[0])
                  if not last:
                      norm_phase([cs, lat], l, GM2, 3)
                      aa.reset()
                      ffn_phase(l, [cs, lat], [(fg_d[0], fu_d[0], fd_d[0], None)])
                      chk("ffn0")
                      if b == 0:
                          cx.barrier()
                          debug_dump("x_l0", XT[:, :, 0:512], lat.XB[0])
                          debug_dump("xc_l0", XC[:], cs.XB[0])
                  else:
                      gateT = XC[:].rearrange("p j t -> p (j t)")
                      B_gateT = cs.XB[0]
                      gateT_g[0], gateT_g[1] = gateT, B_gateT
                      norm_phase([lat], l, GM2, 3, router=True, gateT=gateT, B_gateT=B_gateT)
                      if b == 0:
                          debug_dump("gateT", gateT[0:8, 0:512], B_gateT)
                      aa.reset()
                      ffn_phase(l, [lat], [(mg_d[0, e], mu_d[0, e], md_d[0, e], e) for e in range(NE)])
              cx.barrier()
              for bi, (t0, nt) in enumerate(lat.blocks):
                  cx.dma("sync", outT_d[b, :, t0:t0 + nt].rearrange("(j p) t -> p j t", p=128), XT[:, :, t0:t0 + nt], reads=[lat.XB[bi]])

        try:
            main_schedule()
        except _Stop:
            pass
        cx.finish()
        print("instructions (tracked ops):", cx.ninst, "semaphores:", cx.nsem)
    return nc


_CACHE = {}


def _pmajor(v):
    return np.ascontiguousarray(v.reshape(*v.shape[:-1], 8, 128).swapaxes(-1, -2))


def prepare_inputs(inp):
    f = lambda a: np.ascontiguousarray(np.asarray(a, dtype=np.float32))
    L = 2
    shared = dict(make_consts())
    shared["ada_w"] = f(inp["ada_w"])
    shared["ada_b"] = np.ascontiguousarray(f(inp["ada_b"]).reshape(L, 48, 128).transpose(0, 2, 1))
    shared["norm1_g"] = _pmajor(f(inp["norm1_g"]))
    shared["norm2_g"] = _pmajor(f(inp["norm2_g"]))
    shared["w_in"] = f(inp["w_in"])
    bc = lambda v: np.ascontiguousarray(np.broadcast_to(v[:, None, :], (L, 128, v.shape[-1])))
    shared["gmlp_g"] = bc(f(inp["gmlp_norm_g"]))
    shared["spatial_wT"] = np.ascontiguousarray(f(inp["spatial_w"]).transpose(0, 3, 1, 2))
    shared["spatial_bT"] = np.ascontiguousarray(f(inp["spatial_b"]).transpose(0, 2, 1))
    pw = f(inp["pool_w"])
    bd = np.zeros((L, 128, 2, 128), np.float32)
    for c in range(2):
        for h in range(2):
            bd[:, h * 64:(h + 1) * 64, c, h * 64:(h + 1) * 64] = pw[:, 2 * c + h]
    shared["bdwp"] = bd
    shared["pool_scale"] = np.ascontiguousarray(f(inp["pool_scale"]).reshape(L, 2, 128).transpose(0, 2, 1))
    shared["q_norm_g"] = bc(f(inp["q_norm_g"]))
    shared["kv_norm_g"] = bc(f(inp["kv_norm_g"]))
    shared["qk_q_g"] = bc(np.tile(f(inp["qk_q_g"]), (1, 4)))
    shared["qk_k_g"] = bc(np.tile(f(inp["qk_k_g"]), (1, 4)))
    shared["w_uq"] = f(inp["w_uq"])
    shared["w_ukv"] = f(inp["w_ukv"])
    shared["fourier_w"] = f(inp["fourier_w"])
    shared["w_out"] = f(inp["w_out"])
    shared["ffn_w_gate"] = f(inp["ffn_w_gate"])
    shared["ffn_w_up"] = f(inp["ffn_w_up"])
    shared["ffn_w_down"] = f(inp["ffn_w_down"])
    shared["router_w"] = np.ascontiguousarray(f(inp["router_w"])[0].reshape(8, 128, 8).transpose(1, 0, 2))
    shared["moe_w_gate"] = f(inp["moe_w_gate"])
    shared["moe_w_up"] = f(inp["moe_w_up"])
    shared["moe_w_down"] = f(inp["moe_w_down"])
    x = f(inp["x"])
    ctx = f(inp["ctx"])
    c = f(inp["c"])
    cc = f(inp["c_ctx"])
    maps = []
    for i in range(8):
        m = dict(shared)
        m["xT"] = np.ascontiguousarray(x[2 * i:2 * i + 2].transpose(0, 2, 1))
        m["ctxT"] = np.ascontiguousarray(ctx[2 * i:2 * i + 2].transpose(0, 2, 1))
        c3 = np.stack([c[2 * i], c[2 * i + 1], cc], axis=-1)
        m["cT"] = np.ascontiguousarray(c3.reshape(8, 128, 3).transpose(1, 0, 2))
        maps.append(m)
    return maps


def kernel(**inputs):
    if "nc" not in _CACHE:
        _CACHE["nc"] = build_program()
    nc = _CACHE["nc"]
    maps = prepare_inputs(inputs)
    res = run_bass_kernel_spmd(nc, maps, core_ids=list(range(8)))
    out = np.empty((16, T, D), np.float32)
    for i in range(8):
        o = res.results[i]["outT"]
        out[2 * i:2 * i + 2] = o.transpose(0, 2, 1)
    return out
```
